# Optimizing a Trainium2 kernel written in Bass

```python
import math
import jax
import jax.numpy as jnp
from jax import lax
import numpy as np


D_MODEL = 1024
BATCH = 1
SEQ = 16384
DEPTH = 1

HEAD_DIM = 64
NSA_HEADS = 8
NSA_GROUPS = 2
NSA_REP = NSA_HEADS // NSA_GROUPS
CMP_BLOCK = 32
CMP_STRIDE = 16
CMP_HIDDEN = 256
SEL_BLOCK = 64
SEL_TOPK = 16
NSA_WINDOW = 512
NSA_QBLOCK = 128
SWA_HEADS = 8
SWA_KV_HEADS = 2
SWA_REP = SWA_HEADS // SWA_KV_HEADS
SWA_WINDOW = 128
SWA_QBLOCK = 128
D_FF = 4 * D_MODEL
NUM_BUCKETS = 32
MAX_DISTANCE = 1024
N_BIAS_HEADS = NSA_HEADS + SWA_HEADS
RMS_EPS = 1e-6

NSA_Q_DIM = NSA_HEADS * HEAD_DIM
NSA_KV_DIM = NSA_GROUPS * HEAD_DIM
SWA_Q_DIM = SWA_HEADS * HEAD_DIM
SWA_KV_DIM = SWA_KV_HEADS * HEAD_DIM
IN_SIZES = (NSA_Q_DIM,) + (NSA_KV_DIM,) * 6 + (3 * NSA_HEADS, SWA_Q_DIM, SWA_KV_DIM, SWA_KV_DIM, D_MODEL, D_MODEL)
IN_DIM = int(sum(IN_SIZES))
IN_OFFSETS = tuple(int(o) for o in np.cumsum(IN_SIZES)[:-1])

kernel_name = 'hybrid_nsa_swa_sink_gated_block'


def rms_norm(x, g):
    xf = x.astype(jnp.float32)
    y = xf * lax.rsqrt(jnp.mean(xf * xf, axis=-1, keepdims=True) + RMS_EPS)
    return (y * g.astype(jnp.float32)).astype(x.dtype)


def t5_bucket(dist):
    max_exact = NUM_BUCKETS // 2
    d = jnp.maximum(dist, 0)
    df = jnp.maximum(d, 1).astype(jnp.float32)
    large = max_exact + (jnp.log(df / max_exact) / math.log(MAX_DISTANCE / max_exact)
                         * (NUM_BUCKETS - max_exact)).astype(jnp.int32)
    large = jnp.minimum(large, NUM_BUCKETS - 1)
    return jnp.where(d < max_exact, d, large)


def masked_softmax(s, mask):
    s = jnp.where(mask, s.astype(jnp.float32), -jnp.inf)
    m = jnp.max(s, axis=-1, keepdims=True)
    m = jnp.where(jnp.isfinite(m), m, 0.0)
    e = jnp.exp(s - m)
    return e / jnp.maximum(jnp.sum(e, axis=-1, keepdims=True), 1e-30)


def compress_kv(kv, pos, w1, w2):
    b, s, g, d = kv.shape
    n_cmp = (s - CMP_BLOCK) // CMP_STRIDE + 1
    kb = kv.reshape(b, s // CMP_STRIDE, CMP_STRIDE, g, d)
    blocks = jnp.concatenate([kb[:, o:o + n_cmp] for o in range(CMP_BLOCK // CMP_STRIDE)], axis=2)
    blocks = blocks + pos[None, None, :, None, :]
    flat = blocks.transpose(0, 1, 3, 2, 4).reshape(b, n_cmp, g, CMP_BLOCK * d)
    return jax.nn.gelu(flat @ w1) @ w2


def nsa_attention(q, kc, vc, k_slc, v_slc, k_win, v_win, gates, bias_tab):
    b, s, g, r, d = q.shape
    scale = d ** -0.5
    n_cmp = kc.shape[1]
    n_sel = s // SEL_BLOCK
    topk = min(SEL_TOPK, n_sel)
    n_chunks = s // NSA_QBLOCK
    ratio = SEL_BLOCK // CMP_STRIDE
    pad_front = CMP_BLOCK // CMP_STRIDE - 1
    pad_back = n_sel * ratio - n_cmp
    ks_blk = k_slc.reshape(b, n_sel, SEL_BLOCK, g, d).transpose(0, 3, 1, 2, 4)
    vs_blk = v_slc.reshape(b, n_sel, SEL_BLOCK, g, d).transpose(0, 3, 1, 2, 4)
    kw = jnp.pad(k_win, ((0, 0), (NSA_WINDOW, 0), (0, 0), (0, 0)))
    vw = jnp.pad(v_win, ((0, 0), (NSA_WINDOW, 0), (0, 0), (0, 0)))
    cmp_end = jnp.arange(n_cmp) * CMP_STRIDE + CMP_BLOCK - 1
    b_idx = jnp.arange(b)[:, None, None, None]
    g_idx = jnp.arange(g)[None, :, None, None]
    win_off = jnp.arange(NSA_QBLOCK + NSA_WINDOW) - NSA_WINDOW
    sel_off = jnp.arange(SEL_BLOCK)
    blk_ids = jnp.arange(n_sel)

    def chunk(args):
        qc, gc, ci = args
        t = ci * NSA_QBLOCK + jnp.arange(NSA_QBLOCK)
        sc = jnp.einsum('bqgrd,bngd->bgrqn', qc, kc).astype(jnp.float32) * scale
        pc = masked_softmax(sc, cmp_end[None, :] <= t[:, None])
        oc = jnp.einsum('bgrqn,bngd->bqgrd', pc.astype(vc.dtype), vc)
        pg = jnp.pad(pc.sum(axis=2), ((0, 0), (0, 0), (0, 0), (pad_front, pad_back)))
        ps = sum(pg[..., o:o + n_sel * ratio:ratio] for o in range(ratio + pad_front))
        cur = t // SEL_BLOCK
        valid = blk_ids[None, :] <= cur[:, None]
        forced = (blk_ids[None, :] == 0) | (blk_ids[None, :] == cur[:, None]) | (blk_ids[None, :] == cur[:, None] - 1)
        score = jnp.where(valid, jnp.where(forced, jnp.inf, ps), -jnp.inf)
        _, idx = lax.top_k(score, topk)
        kg = ks_blk[b_idx, g_idx, idx]
        vg = vs_blk[b_idx, g_idx, idx]
        pos = idx[..., None] * SEL_BLOCK + sel_off
        tq = t[None, None, :, None, None]
        bias_s = bias_tab[t5_bucket(tq - pos), g_idx[..., None]]
        ss = jnp.einsum('bqgrd,bgqjkd->bgrqjk', qc, kg).astype(jnp.float32) * scale + jnp.moveaxis(bias_s, -1, 2)
        n_keys = topk * SEL_BLOCK
        mask_s = (pos <= tq).reshape(b, g, 1, NSA_QBLOCK, n_keys)
        p_s = masked_softmax(ss.reshape(b, g, r, NSA_QBLOCK, n_keys), mask_s)
        o_s = jnp.einsum('bgrqn,bgqnd->bqgrd', p_s.astype(vg.dtype), vg.reshape(b, g, NSA_QBLOCK, n_keys, d))
        kwc = lax.dynamic_slice_in_dim(kw, ci * NSA_QBLOCK, NSA_QBLOCK + NSA_WINDOW, axis=1)
        vwc = lax.dynamic_slice_in_dim(vw, ci * NSA_QBLOCK, NSA_QBLOCK + NSA_WINDOW, axis=1)
        spos = ci * NSA_QBLOCK + win_off
        dist = t[:, None] - spos[None, :]
        mask_w = (dist >= 0) & (dist < NSA_WINDOW) & (spos[None, :] >= 0)
        bias_w = jnp.transpose(bias_tab[t5_bucket(dist)], (2, 3, 0, 1))
        sw = jnp.einsum('bqgrd,bkgd->bgrqk', qc, kwc).astype(jnp.float32) * scale + bias_w
        p_w = masked_softmax(sw, mask_w)
        o_w = jnp.einsum('bgrqk,bkgd->bqgrd', p_w.astype(vwc.dtype), vwc)
        gs = jax.nn.sigmoid(gc.astype(jnp.float32)).astype(qc.dtype)
        return gs[..., 0:1] * oc + gs[..., 1:2] * o_s + gs[..., 2:3] * o_w

    q_ch = q.reshape(b, n_chunks, NSA_QBLOCK, g, r, d).swapaxes(0, 1)
    g_ch = gates.reshape(b, n_chunks, NSA_QBLOCK, g, r, 3).swapaxes(0, 1)
    out = lax.map(chunk, (q_ch, g_ch, jnp.arange(n_chunks)))
    return out.swapaxes(0, 1).reshape(b, s, g * r * d)


def swa_sink_attention(q, k, v, sinks, bias_tab):
    b, s, g, r, d = q.shape
    scale = d ** -0.5
    nb = s // SWA_QBLOCK
    qb = q.reshape(b, nb, SWA_QBLOCK, g, r, d)
    kb = k.reshape(b, nb, SWA_QBLOCK, g, d)
    vb = v.reshape(b, nb, SWA_QBLOCK, g, d)
    pad = ((0, 0), (1, 0), (0, 0), (0, 0), (0, 0))
    kk = jnp.concatenate([jnp.pad(kb, pad)[:, :-1], kb], axis=2)
    vv = jnp.concatenate([jnp.pad(vb, pad)[:, :-1], vb], axis=2)
    qi = jnp.arange(SWA_QBLOCK)[:, None]
    ki = jnp.arange(2 * SWA_QBLOCK)[None, :]
    dist = qi - ki + SWA_QBLOCK
    band = (dist >= 0) & (dist < SWA_WINDOW)
    first = (jnp.arange(nb) > 0)[:, None, None] | (ki >= SWA_QBLOCK)[None]
    mask = (band[None] & first)[None, :, None, None]
    bias = jnp.transpose(bias_tab[t5_bucket(dist)], (2, 3, 0, 1))
    sc = jnp.einsum('bnqgrd,bnkgd->bngrqk', qb, kk).astype(jnp.float32) * scale + bias
    sc = jnp.where(mask, sc, -jnp.inf)
    sink = sinks.astype(jnp.float32)[None, None, :, :, None, None]
    m = jnp.maximum(jnp.max(sc, axis=-1, keepdims=True), sink)
    e = jnp.exp(sc - m)
    p = e / (jnp.sum(e, axis=-1, keepdims=True) + jnp.exp(sink - m))
    o = jnp.einsum('bngrqk,bnkgd->bnqgrd', p.astype(vv.dtype), vv)
    return o.reshape(b, s, g * r * d)


def setup_inputs(seed: int = 0) -> dict:
    key = jax.random.key(seed)
    ks = jax.random.split(key, 20)
    nrm = lambda k, shape, scale: jax.random.normal(k, shape, jnp.float32) * scale
    gain = lambda k: 1.0 + nrm(k, (DEPTH, D_MODEL), 0.05)
    cin = CMP_BLOCK * HEAD_DIM
    return {
        'x': nrm(ks[0], (BATCH, SEQ, D_MODEL), 1.0),
        'norm_mix_pre': gain(ks[1]),
        'norm_mix_post': gain(ks[2]),
        'norm_mlp_pre': gain(ks[3]),
        'norm_mlp_post': gain(ks[4]),
        'w_in': nrm(ks[5], (DEPTH, D_MODEL, IN_DIM), D_MODEL ** -0.5),
        'cmp_pos_k': nrm(ks[6], (DEPTH, CMP_BLOCK, HEAD_DIM), 0.5),
        'cmp_w1_k': nrm(ks[7], (DEPTH, cin, CMP_HIDDEN), cin ** -0.5),
        'cmp_w2_k': nrm(ks[8], (DEPTH, CMP_HIDDEN, HEAD_DIM), CMP_HIDDEN ** -0.5),
        'cmp_pos_v': nrm(ks[9], (DEPTH, CMP_BLOCK, HEAD_DIM), 0.5),
        'cmp_w1_v': nrm(ks[10], (DEPTH, cin, CMP_HIDDEN), cin ** -0.5),
        'cmp_w2_v': nrm(ks[11], (DEPTH, CMP_HIDDEN, HEAD_DIM), CMP_HIDDEN ** -0.5),
        'attn_sinks': nrm(ks[12], (DEPTH, SWA_HEADS), 0.5),
        'rel_bias': nrm(ks[13], (NUM_BUCKETS, N_BIAS_HEADS), 0.5),
        'w_up_nsa': nrm(ks[14], (DEPTH, NSA_Q_DIM, D_MODEL), NSA_Q_DIM ** -0.5),
        'w_up_swa': nrm(ks[15], (DEPTH, SWA_Q_DIM, D_MODEL), SWA_Q_DIM ** -0.5),
        'w_out': nrm(ks[16], (DEPTH, D_MODEL, D_MODEL), D_MODEL ** -0.5),
        'w_mlp_in': nrm(ks[17], (DEPTH, D_MODEL, D_FF), D_MODEL ** -0.5),
        'w_mlp_out': nrm(ks[18], (DEPTH, D_FF, D_MODEL), D_FF ** -0.5),
    }


def reference(x, norm_mix_pre, norm_mix_post, norm_mlp_pre, norm_mlp_post, w_in,
              cmp_pos_k, cmp_w1_k, cmp_w2_k, cmp_pos_v, cmp_w1_v, cmp_w2_v,
              attn_sinks, rel_bias, w_up_nsa, w_up_swa, w_out, w_mlp_in, w_mlp_out):
    b, s, _ = x.shape
    bias_nsa = rel_bias[:, :NSA_HEADS].reshape(NUM_BUCKETS, NSA_GROUPS, NSA_REP)
    bias_swa = rel_bias[:, NSA_HEADS:].reshape(NUM_BUCKETS, SWA_KV_HEADS, SWA_REP)
    h = x
    for l in range(DEPTH):
        u = rms_norm(h, norm_mix_pre[l])
        (q_n, kc_n, vc_n, ks_n, vs_n, kw_n, vw_n, g_n,
         q_s, k_s, v_s, gate_a, gate_b) = jnp.split(u @ w_in[l], IN_OFFSETS, axis=-1)
        kvr = lambda t_: t_.reshape(b, s, NSA_GROUPS, HEAD_DIM)
        kc = compress_kv(kvr(kc_n), cmp_pos_k[l], cmp_w1_k[l], cmp_w2_k[l])
        vc = compress_kv(kvr(vc_n), cmp_pos_v[l], cmp_w1_v[l], cmp_w2_v[l])
        o_a = nsa_attention(q_n.reshape(b, s, NSA_GROUPS, NSA_REP, HEAD_DIM), kc, vc,
                            kvr(ks_n), kvr(vs_n), kvr(kw_n), kvr(vw_n),
                            g_n.reshape(b, s, NSA_GROUPS, NSA_REP, 3), bias_nsa)
        o_b = swa_sink_attention(q_s.reshape(b, s, SWA_KV_HEADS, SWA_REP, HEAD_DIM),
                                 k_s.reshape(b, s, SWA_KV_HEADS, HEAD_DIM),
                                 v_s.reshape(b, s, SWA_KV_HEADS, HEAD_DIM),
                                 attn_sinks[l].reshape(SWA_KV_HEADS, SWA_REP), bias_swa)
        y = jax.nn.sigmoid(gate_a) * (o_a @ w_up_nsa[l]) + jax.nn.sigmoid(gate_b) * (o_b @ w_up_swa[l])
        h = h + rms_norm(y @ w_out[l], norm_mix_post[l])
        u = rms_norm(h, norm_mlp_pre[l])
        f = jnp.square(jax.nn.relu(u @ w_mlp_in[l])) @ w_mlp_out[l]
        h = h + rms_norm(f, norm_mlp_post[l])
    return h
```

```python
import contextlib
import os
import numpy as np
import ml_dtypes
import concourse.bass as bass
import concourse.mybir as mybir
from concourse.bass_utils import run_bass_kernel_spmd

F32 = mybir.dt.float32
BF16 = mybir.dt.bfloat16
ALU = mybir.AluOpType
AF = mybir.ActivationFunctionType
AX = mybir.AxisListType

NCORES = 8
S = 16384
D = 1024
NCH = S // 128
NOWN = 16
EPS = 1e-6
NEG = -30000.0
SCALE = 0.125


class Sch:
    def __init__(self, nc, es):
        self.nc, self.es = nc, es
        self.eng = {"pe": nc.tensor, "dve": nc.vector, "act": nc.scalar, "pool": nc.gpsimd, "sp": nc.sync}
        self.nsem = 0
        self.psem, self.pcnt = {}, {}
        for e in self.eng:
            self._new_psem(e)
        self.waited = {}
        self.lastw = {}
        self.readers = {}
        self.dsem = {}
        self.ninst = 0

    def _sem(self, name):
        self.nsem += 1
        return self.es.enter_context(self.nc.semaphore(f"{name}_{self.nsem}"))

    def _new_psem(self, e):
        self.psem[e] = self._sem("p_" + e)
        self.pcnt[e] = 0

    def _deps(self, r, w):
        toks = []
        for k in r:
            if k in self.lastw:
                toks.append(self.lastw[k])
        for k in w:
            if k in self.lastw:
                toks.append(self.lastw[k])
            toks.extend(self.readers.get(k, {}).values())
        return toks

    def _wait(self, e, toks):
        for (sem, val, src) in toks:
            if src == "pe" and e == "pe":
                continue
            k = (e, id(sem))
            if self.waited.get(k, 0) >= val:
                continue
            self.eng[e].wait_ge(sem, val)
            self.waited[k] = val

    def _record(self, tok, r, w):
        for k in r:
            d = self.readers.setdefault(k, {})
            d[id(tok[0])] = tok
        for k in w:
            self.lastw[k] = tok
            self.readers[k] = {}

    def op(self, e, meth, *args, r=(), w=(), **kw):
        self._wait(e, self._deps(r, w))
        ins = getattr(self.eng[e], meth)(*args, **kw)
        if self.pcnt[e] >= 8000:
            self._new_psem(e)
        self.pcnt[e] += 1
        ins.then_inc(self.psem[e], 1)
        tok = (self.psem[e], self.pcnt[e], e)
        self._record(tok, r, w)
        self.ninst += 1
        return tok

    def dma(self, q, out, in_, r=(), w=(), key=None):
        self._wait(q, self._deps(r, w))
        key = key if key is not None else (w[0] if w else ("rd", r[0]))
        ent = self.dsem.get(key)
        if ent is None or ent[1] >= 30000:
            ent = [self._sem("d"), 0]
            self.dsem[key] = ent
        ins = self.eng[q].dma_start(out=out, in_=in_)
        ent[1] += 16
        ins.then_inc(ent[0], 16)
        tok = (ent[0], ent[1], "dma")
        self._record(tok, r, w)
        self.ninst += 1
        return tok

    def wait_all(self, e, keys):
        toks = []
        for k in keys:
            if k in self.lastw:
                toks.append(self.lastw[k])
            toks.extend(self.readers.get(k, {}).values())
        self._wait(e, toks)


def own_chunks(c):
    out = []
    for k in range(8):
        out.append(16 * k + c)
        out.append(16 * k + 15 - c)
    return out


def t5_bucket_np(dist):
    d = np.maximum(dist, 0)
    df = np.maximum(d, 1).astype(np.float32)
    large = 16 + (np.log(df / 16) / np.float32(np.log(1024 / 16)) * 16).astype(np.int32)
    large = np.minimum(large, 31)
    return np.where(d < 16, d, large)


def build(dbg=None):
    dbg = dbg or set()
    nc = bass.Bass("TRN2", target_bir_lowering=False)
    I, O = {}, {}

    def inp(name, shape, dt=F32):
        I[name] = nc.dram_tensor(name, list(shape), dt, kind="ExternalInput")
        return I[name]

    def outp(name, shape, dt=F32):
        O[name] = nc.dram_tensor(name, list(shape), dt, kind="ExternalOutput")
        return O[name]

    def scratch(name, shape, dt=BF16):
        return nc.dram_tensor(name, list(shape), dt)

    xT_all = inp("xT_all", [D, S])
    xT_own = inp("xT_own", [D, 2048])
    xT_kv2 = inp("xT_kv2", [D, 16, 640])
    hv_in = inp("hv", [128, 16, 5])
    w_A = inp("w_A", [D, 512])
    w_Q = inp("w_Q", [D, 1024])
    w_ga = inp("w_ga", [D, 1024])
    w_gb = inp("w_gb", [D, 1024])
    w_kv2 = inp("w_kv2", [D, 512])
    w_gn = inp("w_gn", [D, 24])
    gains = inp("gains", [128, 4, 8])
    indic = inp("indic", [63, S], BF16)
    w1c = [inp("w1k", [2048, 256]), inp("w1v", [2048, 256])]
    w2c = [inp("w2k", [256, 64]), inp("w2v", [256, 64])]
    posc_in = [inp("posk", [128, 16]), inp("posv", [128, 16])]
    nt_sel = inp("nt_sel", [2, 15, 2, 128, 512])
    nt_win = inp("nt_win", [5, 2, 128, 512])
    nt_swa = inp("nt_swa", [2, 2, 128, 512])
    b31_in = inp("b31row", [1, 2, 512])
    sinks_in = inp("sinks", [128, 8])
    atab_in = inp("atab", [128, 16, 310], BF16)
    trow_in = inp("trow", [128, 16, 128])
    cend_in = inp("cend", [128, 8])
    band_in = inp("band", [128, 8, 256], BF16)
    ident_in = inp("ident", [128, 128], BF16)
    identf_in = inp("identf", [128, 128])
    w_upA = inp("w_upA", [512, 1024])
    w_upB = inp("w_upB", [512, 1024])
    w_out = inp("w_out", [D, D])
    w_m1 = inp("w_m1", [D, 4096])
    w_m2 = inp("w_m2", [4096, D])

    kcnT = scratch("kcnT", [128, S])
    vcnT = scratch("vcnT", [128, S])
    qn_d = scratch("qn_d", [64, 16, 8, 128])
    qs_d = scratch("qs_d", [64, 16, 8, 128])
    gate_d = [scratch("ga_d", [128, 8, 2048]), scratch("gb_d", [128, 8, 2048])]
    kw_d = scratch("kw_d", [64, 16, 2, 640])
    vw_d = scratch("vw_d", [128, 16, 5, 2, 65])
    k2_d = scratch("k2_d", [64, 16, 2, 256])
    v2_d = scratch("v2_d", [128, 16, 2, 2, 65])
    oat_d = scratch("oat_d", [128, 4, 2048])
    obt_d = scratch("obt_d", [128, 4, 2048])
    ht_d = scratch("ht_d", [128, 8, 2048], F32)
    EB = 256

    out = outp("out", [D, NOWN * 128])
    dbo = {}

    def dbgout(name, shape, dt=F32):
        dbo[name] = outp(name, shape, dt)
        return dbo[name]

    es = contextlib.ExitStack()
    with es:
        sc = Sch(nc, es)

        uid = [0]

        def mk(stack):
            def sbx(name, shape, dt):
                uid[0] += 1
                return stack.enter_context(nc.sbuf_tensor(f"{name}_{uid[0]}", list(shape), dt))

            def psx(name, shape=(128, 512), dt=F32):
                uid[0] += 1
                return stack.enter_context(nc.psum_tensor(f"{name}_{uid[0]}", list(shape), dt))
            return sbx, psx

        sb, ps = mk(es)

        def barrier():
            toks = [(sc.psem[e], sc.pcnt[e], e) for e in sc.eng if sc.pcnt[e] > 0]
            toks += [(ent[0], ent[1], "dma") for ent in sc.dsem.values() if ent[1] > 0]
            for e in sc.eng:
                for (sem, val, src) in toks:
                    if src == e:
                        continue
                    k = (e, id(sem))
                    if sc.waited.get(k, 0) >= val:
                        continue
                    sc.eng[e].wait_ge(sem, val)
                    sc.waited[k] = val

        ones = sb("ones", [128, 128], BF16)
        gn = sb("gn", [128, 4, 8], F32)
        GS = sb("GS", [128, 16, 24], F32)
        sc.op("pool", "memset", ones[:], 1.0, w=["ones"])
        sc.dma("sp", gn[:], gains.ap(), w=["gn"])

        def norm_cols(xt_ap, xkey, sq_ap, sqkey, pstat, pkey, rs_ap, rskey, u_ap, ukey, gidx, n, split=True,
                      do_sq=True, do_rest=True):
            if do_sq:
                sc.op("act", "activation", sq_ap[:], xt_ap[:], AF.Square, r=[xkey], w=[sqkey])
            if not do_rest:
                return
            for c0 in range(0, n, 512):
                c1 = min(n, c0 + 512)
                for kc in range(8):
                    sc.op("pe", "matmul", pstat[:, 0:c1 - c0], ones[:], sq_ap[:, kc, c0:c1], start=(kc == 0), stop=(kc == 7),
                          r=[sqkey, "ones"], w=[pkey])
                sc.op("dve", "tensor_scalar", rs_ap[:, c0:c1], pstat[:, 0:c1 - c0], 1.0 / D, EPS, ALU.mult, ALU.add,
                      r=[pkey], w=[rskey])
            sc.op("act", "activation", rs_ap[:, 0:n], rs_ap[:, 0:n], AF.Sqrt, r=[rskey], w=[rskey])
            sc.op("dve", "reciprocal", rs_ap[:, 0:n], rs_ap[:, 0:n], r=[rskey], w=[rskey])
            if u_ap is not None:
                for (e, k0, k1) in ((("dve", 0, 5), ("pool", 5, 8)) if split else (("dve", 0, 8),)):
                    sc.op(e, "tensor_tensor", u_ap[:, k0:k1, :], xt_ap[:, k0:k1, :],
                          rs_ap[:, 0:n].unsqueeze(1).broadcast_to([128, k1 - k0, n]), ALU.mult,
                          r=[xkey, rskey], w=[ukey])

        def fold_gain(W, wkey, gidx):
            for kc in range(8):
                sc.op("dve", "tensor_scalar", W[:, kc, :], W[:, kc, :], gn[:, gidx, kc:kc + 1], None, ALU.mult,
                      r=[wkey, "gn"], w=[wkey])

        with contextlib.ExitStack() as esP:
            sbP, psP = mk(esP)
            KT = sbP("KT", [128, 2, S], BF16)
            VAf = sbP("VAf", [128, NCH * 130 + 64], BF16)
            VA = VAf[:, 0:NCH * 130].rearrange("p (t g e) -> p t g e", g=2, e=65)
            KC = sbP("KC", [128, 2, 1024], BF16)
            VCf = sbP("VCf", [128, 8 * 130 + 64], BF16)
            VC = VCf[:, 0:8 * 130].rearrange("p (t g e) -> p t g e", g=2, e=65)
            sc.op("pool", "memset", VAf[:], 0.0, w=["VA"])
            sc.op("pool", "memset", VA[:, :, :, 64:65], 1.0, w=["VA"])
            sc.op("pool", "memset", KC[:], 0.0, w=["KC"])
            sc.op("pool", "memset", VCf[:], 0.0, w=["VC"])
            sc.op("pool", "memset", VC[:, :, :, 64:65], 1.0, w=["VC"])
            for g in range(2):
                sc.dma("pool", KT[64:127, g, :], indic.ap(), w=[("KTc", g)])

            with contextlib.ExitStack() as esA:
                sbA, psA = mk(esA)
                WA = sbA("WA", [128, 8, 512], BF16)
                xt = [sbA(f"xt{i}", [128, 8, 512], F32) for i in range(3)]
                sq = [sbA(f"sq{i}", [128, 8, 512], BF16) for i in range(2)]
                uT = [sbA(f"uT{i}", [128, 8, 512], BF16) for i in range(2)]
                rs = [sbA(f"rs{i}", [128, 512], F32) for i in range(2)]
                stg = [sbA(f"stg{i}", [128, 2, 512], BF16) for i in range(2)]
                p_stat = [psA(f"p_stat{i}") for i in range(2)]
                p_kcn = psA("p_kcn")
                p_vcn = psA("p_vcn")
                p_ks = psA("p_ks")
                p_v = [psA(f"p_v{i}", [128, 4, 128]) for i in range(2)]

                sc.dma("pool", WA[:], w_A.ap().rearrange("(kc p) n -> p kc n", p=128), w=["WA"])
                fold_gain(WA, "WA", 0)
                xsrc = xT_all.ap().rearrange("(kc p) t -> p kc t", p=128)
                NB = 32 if "Ashort" not in dbg else 2
                kcn_v = kcnT.ap().rearrange("p (s n) -> p s n", s=16)
                vcn_v = vcnT.ap().rearrange("p (s n) -> p s n", s=16)

                def loadA(tb):
                    sc.dma("sp", xt[tb % 3][:], xsrc[:, :, tb * 512:(tb + 1) * 512], w=[("xt", tb % 3)])

                def sqA(tb):
                    s = tb % 2
                    norm_cols(xt[tb % 3], ("xt", tb % 3), sq[s], ("sq", s), p_stat[s], ("p_stat", s), rs[s], ("rs", s),
                              uT[s], ("uT", s), 0, 512, do_rest=False)

                def restA(tb):
                    s = tb % 2
                    norm_cols(xt[tb % 3], ("xt", tb % 3), sq[s], ("sq", s), p_stat[s], ("p_stat", s), rs[s], ("rs", s),
                              uT[s], ("uT", s), 0, 512, do_sq=False)

                def projA(tb):
                    s = tb % 2
                    cs = slice(tb * 512, (tb + 1) * 512)
                    for (pp, name, c0) in ((p_kcn, "p_kcn", 0), (p_vcn, "p_vcn", 128), (p_ks, "p_ks", 256)):
                        for kc in range(8):
                            sc.op("pe", "matmul", pp[:], WA[:, kc, c0:c0 + 128], uT[s][:, kc, :],
                                  start=(kc == 0), stop=(kc == 7), r=[("uT", s), "WA"], w=[name])
                    for t4 in range(4):
                        for kc in range(8):
                            sc.op("pe", "matmul", p_v[s][:, t4, :], uT[s][:, kc, t4 * 128:(t4 + 1) * 128],
                                  WA[:, kc, 384:512], start=(kc == 0), stop=(kc == 7),
                                  r=[("uT", s), "WA"], w=[("p_v", s)])
                    sc.op("act", "activation", stg[s][:, 0, :].rearrange("p (s n) -> p s n", s=16),
                          p_kcn[:].rearrange("p (n s) -> p s n", s=16), AF.Copy, r=["p_kcn"], w=[("stg", s)])
                    sc.op("act", "activation", stg[s][:, 1, :].rearrange("p (s n) -> p s n", s=16),
                          p_vcn[:].rearrange("p (n s) -> p s n", s=16), AF.Copy, r=["p_vcn"], w=[("stg", s)])
                    sc.dma("pool", kcn_v[:, :, tb * 32:(tb + 1) * 32], stg[s][:, 0, :].rearrange("p (s n) -> p s n", s=16),
                           r=[("stg", s)], w=["kcnT"])
                    sc.dma("pool", vcn_v[:, :, tb * 32:(tb + 1) * 32], stg[s][:, 1, :].rearrange("p (s n) -> p s n", s=16),
                           r=[("stg", s)], w=["vcnT"])
                    sc.op("dve", "tensor_copy", KT[0:64, 0, cs], p_ks[0:64, :], r=["p_ks"], w=[("KT", 0)])
                    sc.op("dve", "tensor_copy", KT[0:64, 1, cs], p_ks[64:128, :], r=["p_ks"], w=[("KT", 1)])
                    sc.op("dve", "tensor_copy", VA[:, tb * 4:(tb + 1) * 4, :, 0:64],
                          p_v[s][:].rearrange("p t (g d) -> p t g d", g=2), r=[("p_v", s)], w=["VA"])

                for tb in range(min(3, NB)):
                    loadA(tb)
                sqA(0)
                restA(0)
                if NB > 1:
                    sqA(1)
                for tb in range(NB):
                    if tb + 1 < NB:
                        restA(tb + 1)
                    if tb + 2 < NB:
                        sqA(tb + 2)
                    projA(tb)
                    if tb + 3 < NB:
                        loadA(tb + 3)
                barrier()

            with contextlib.ExitStack() as esB:
                sbB, psB = mk(esB)
                X2s = [sbB(f"X2{i}", [128, 16, 1024], BF16) for i in range(2)]
                W1 = [sbB(f"W1_{i}", [128, 16, 256], BF16) for i in range(2)]
                W2 = [sbB(f"W2_{i}", [128, 2, 64], BF16) for i in range(2)]
                posc = [sbB(f"posc{i}", [128, 16], BF16) for i in range(2)]
                cb = [sbB(f"cb{i}", [128, 2], F32) for i in range(2)]
                hbs = [sbB(f"hb{i}", [128, 512], F32) for i in range(2)]
                t1s = [sbB(f"t1{i}", [128, 512], F32) for i in range(2)]
                sgs = [sbB(f"sg{i}", [128, 512], F32) for i in range(2)]
                G = [sbB(f"G{i}", [128, 512], BF16) for i in range(2)]
                p_h = [psB(f"p_h{i}") for i in range(2)]
                p_c = psB("p_c", [128, 2])
                p_o = psB("p_o")
                p_vo = psB("p_vo", [128, 4, 64])
                for i in range(2):
                    sc.dma("pool", W1[i][:], w1c[i].ap().rearrange("(jp p) n -> p jp n", p=128), w=[("W1", i)])
                    sc.dma("pool", W2[i][:], w2c[i].ap().rearrange("(hc p) d -> p hc d", p=128), w=[("W2", i)])
                    sc.dma("pool", posc[i][:], posc_in[i].ap(), w=[("posc", i)])
                sc.op("pool", "memset", G[0][:], 0.0, w=[("G", 0)])
                sc.op("pool", "memset", G[1][:], 0.0, w=[("G", 1)])
                for i in range(2):
                    sc.op("pool", "memset", X2s[i][:], 0.0, w=[("X2", i)])
                def loadX2(kv_, g_):
                    xi_ = (kv_ * 2 + g_) % 2
                    src_ = kcnT if kv_ == 0 else vcnT
                    key_ = "kcnT" if kv_ == 0 else "vcnT"
                    srcv = src_.ap().rearrange("p (s n) -> p s n", s=16)[g_ * 64:(g_ + 1) * 64]
                    sc.dma("sp", X2s[xi_][0:64, :, :], srcv, r=[key_], w=[("X2", xi_)])
                    sc.dma("act", X2s[xi_][64:128, 0:15, :], srcv[:, 1:16, :], r=[key_], w=[("X2", xi_)])
                    sc.dma("act", X2s[xi_][64:128, 15, 0:1023], srcv[:, 0, 1:1024], r=[key_], w=[("X2", xi_)])

                for kv in range(2):
                    for hc in range(2):
                        for jp in range(16):
                            sc.op("pe", "matmul", p_c[:, hc:hc + 1], W1[kv][:, jp, hc * 128:(hc + 1) * 128],
                                  posc[kv][:, jp:jp + 1], start=(jp == 0), stop=(jp == 15),
                                  r=[("W1", kv), ("posc", kv)], w=["p_c"])
                    sc.op("dve", "tensor_copy", cb[kv][:], p_c[:], r=["p_c"], w=[("cb", kv)])
                    src = kcnT if kv == 0 else vcnT
                    srckey = "kcnT" if kv == 0 else "vcnT"
                    for g in range(2):
                        xi = (kv * 2 + g) % 2
                        X2 = X2s[xi]
                        if kv == 0 and g == 0:
                            loadX2(0, 0)
                        if kv * 2 + g + 1 < 4:
                            loadX2((kv * 2 + g + 1) // 2, (kv * 2 + g + 1) % 2)
                        for nb in range(2):
                            n0 = nb * 512
                            ncols = 512 if nb == 0 else 511
                            for hc in range(2):
                                for jp in range(16):
                                    if jp < 8:
                                        rhs = X2[:, 2 * jp, n0:n0 + ncols]
                                    else:
                                        rhs = X2[:, 2 * jp - 16, n0 + 1:n0 + 1 + ncols]
                                    sc.op("pe", "matmul", p_h[hc][:, 0:ncols], W1[kv][:, jp, hc * 128:(hc + 1) * 128], rhs,
                                          start=(jp == 0), stop=(jp == 15), r=[("W1", kv), ("X2", xi)], w=[("p_h", hc)])
                                hb, t1, sg = hbs[hc], t1s[hc], sgs[hc]
                                hbk, t1k, sgk = ("hb", hc), ("t1", hc), ("sg", hc)
                                sc.op("act", "activation", hb[:, 0:ncols], p_h[hc][:, 0:ncols], AF.Identity,
                                      bias=cb[kv][:, hc:hc + 1], r=[("p_h", hc), ("cb", kv)], w=[hbk])
                                sc.op("dve", "tensor_tensor", t1[:, 0:ncols], hb[:, 0:ncols], hb[:, 0:ncols], ALU.mult,
                                      r=[hbk], w=[t1k])
                                sc.op("dve", "tensor_scalar", t1[:, 0:ncols], t1[:, 0:ncols], 0.044715, 1.0, ALU.mult, ALU.add,
                                      r=[t1k], w=[t1k])
                                sc.op("dve", "tensor_tensor", t1[:, 0:ncols], t1[:, 0:ncols], hb[:, 0:ncols], ALU.mult,
                                      r=[t1k, hbk], w=[t1k])
                                sc.op("act", "activation", sg[:, 0:ncols], t1[:, 0:ncols], AF.Sigmoid, scale=1.5957691216,
                                      r=[t1k], w=[sgk])
                                sc.op("dve", "tensor_tensor", G[hc][:, 0:ncols], hb[:, 0:ncols], sg[:, 0:ncols], ALU.mult,
                                      r=[hbk, sgk], w=[("G", hc)])
                            if kv == 0:
                                for hc in range(2):
                                    sc.op("pe", "matmul", p_o[0:64, 0:ncols], W2[0][:, hc, :], G[hc][:, 0:ncols],
                                          start=(hc == 0), stop=(hc == 1), r=[("W2", 0), ("G", hc)], w=["p_o"])
                                sc.op("act", "activation", KC[0:64, g, n0:n0 + ncols], p_o[0:64, 0:ncols], AF.Copy,
                                      r=["p_o"], w=["KC"])
                            else:
                                for t4 in range(4):
                                    for hc in range(2):
                                        sc.op("pe", "matmul", p_vo[:, t4, :], G[hc][:, t4 * 128:(t4 + 1) * 128],
                                              W2[1][:, hc, :], start=(hc == 0), stop=(hc == 1),
                                              r=[("W2", 1), ("G", hc)], w=["p_vo"])
                                sc.op("dve", "tensor_copy", VC[:, nb * 4:(nb + 1) * 4, g, 0:64], p_vo[:],
                                      r=["p_vo"], w=["VC"])
                if "B" in dbg:
                    d_kc = dbgout("d_kc", [128, 2, 1024], BF16)
                    d_vc = dbgout("d_vc", [128, 8, 2, 65], BF16)
                    sc.dma("sp", d_kc.ap(), KC[:], r=["KC"], w=["d_kc"])
                    sc.dma("sp", d_vc.ap(), VC[:], r=["VC"], w=["d_vc"])
                barrier()

            with contextlib.ExitStack() as esC:
                sbC, psC = mk(esC)
                Wbs = [sbC(f"Wb{i}", [128, 8, 1024], BF16) for i in range(2)]
                Wb = Wbs[0]
                Wgn = sbC("Wgn", [128, 8, 24], BF16)
                uTo = sbC("uTo", [128, 8, 2048], BF16)
                xos = [sbC(f"xo{i}", [128, 8, 256], F32) for i in range(2)]
                sqo = sbC("sqo", [128, 8, 256], BF16)
                rso = sbC("rso", [128, 256], F32)
                qst1 = sbC("qst", [64, 4, 8, 128], BF16)
                qst = [qst1, qst1]
                gst = [sbC(f"gst{i}", [128, 512], BF16) for i in range(2)]
                p_st = psC("p_st")
                p_q = [psC(f"p_q{i}") for i in range(3)]
                p_gn = psC("p_gn", [128, 24])
                xosrc = xT_own.ap().rearrange("(kc p) t -> p kc t", p=128)
                sc.dma("pool", Wb[:], w_Q.ap().rearrange("(kc p) n -> p kc n", p=128), w=[("Wb", 0)])
                sc.dma("pool", Wgn[:], w_gn.ap().rearrange("(kc p) n -> p kc n", p=128), w=["Wgn"])
                fold_gain(Wb, ("Wb", 0), 0)
                fold_gain(Wgn, "Wgn", 0)
                sc.dma("pool", Wbs[1][:], w_ga.ap().rearrange("(kc p) n -> p kc n", p=128), w=[("Wb", 1)])
                fold_gain(Wbs[1], ("Wb", 1), 0)
                sc.dma("sp", xos[0][:], xosrc[:, :, 0:256], w=[("xo", 0)])
                for ob in range(8):
                    cs = slice(ob * 256, (ob + 1) * 256)
                    if ob + 1 < 8:
                        sc.dma("sp", xos[(ob + 1) % 2][:], xosrc[:, :, (ob + 1) * 256:(ob + 2) * 256], w=[("xo", (ob + 1) % 2)])
                    norm_cols(xos[ob % 2], ("xo", ob % 2), sqo, "sqo", p_st, "p_st", rso, "rso", uTo[:, :, cs],
                              ("uTo", ob // 2), 0, 256)
                cnt = 0
                for ob in range(4):
                    cs = slice(ob * 512, (ob + 1) * 512)
                    for h16 in range(16):
                        b = cnt % 3
                        cnt += 1
                        for kc in range(8):
                            sc.op("pe", "matmul", p_q[b][0:64, :], Wb[:, kc, h16 * 64:(h16 + 1) * 64], uTo[:, kc, cs],
                                  start=(kc == 0), stop=(kc == 7), r=[("Wb", 0), ("uTo", ob)], w=[("p_q", b)])
                        sc.op("dve", "tensor_scalar", qst[h16 // 8][:, :, h16 % 8, :],
                              p_q[b][0:64, :].rearrange("p (j q) -> p j q", j=4), SCALE, None, ALU.mult,
                              r=[("p_q", b)], w=["qst"])
                        if h16 == 7:
                            sc.dma("sp", qn_d.ap()[:, ob * 4:(ob + 1) * 4], qst[0][:], r=["qst"], w=["qn_d"])
                    sc.dma("sp", qs_d.ap()[:, ob * 4:(ob + 1) * 4], qst[1][:], r=["qst"], w=["qs_d"])
                for j in range(16):
                    for kc in range(8):
                        sc.op("pe", "matmul", p_gn[:], uTo[:, kc, j * 128:(j + 1) * 128], Wgn[:, kc, :],
                              start=(kc == 0), stop=(kc == 7), r=[("uTo", j // 4), "Wgn"], w=["p_gn"])
                    sc.op("act", "activation", GS[:, j, :], p_gn[:], AF.Sigmoid, r=["p_gn"], w=["GS"])
                cnt = 0
                for gi, wsrc in enumerate((w_ga, w_gb)):
                    Wb = Wbs[1 - gi]
                    wkey = ("Wb", 1 - gi)
                    if gi == 1:
                        sc.dma("pool", Wb[:], wsrc.ap().rearrange("(kc p) n -> p kc n", p=128), r=[], w=[wkey])
                        fold_gain(Wb, wkey, 0)
                    for ob in range(4):
                        cs = slice(ob * 512, (ob + 1) * 512)
                        for fc in range(8):
                            b = cnt % 3
                            s2 = cnt % 2
                            cnt += 1
                            for kc in range(8):
                                sc.op("pe", "matmul", p_q[b][:], Wb[:, kc, fc * 128:(fc + 1) * 128], uTo[:, kc, cs],
                                      start=(kc == 0), stop=(kc == 7), r=[wkey, ("uTo", ob)], w=[("p_q", b)])
                            sc.op("act", "activation", gst[s2][:], p_q[b][:], AF.Sigmoid, r=[("p_q", b)], w=[("gst", s2)])
                            sc.dma("act", gate_d[gi].ap()[:, fc, cs], gst[s2][:], r=[("gst", s2)], w=[("gate_d", gi)])
                if "C" in dbg:
                    d_gs = dbgout("d_gs", [128, 16, 24])
                    sc.dma("sp", d_gs.ap(), GS[:], r=["GS"], w=["d_gs"])
                    d_qn = dbgout("d_qn", [64, 16, 8, 128], BF16)
                    sc.dma("sp", d_qn.ap(), qn_d.ap(), r=["qn_d"], w=["d_qn"])
                    d_ga = dbgout("d_ga", [128, 8, 2048], BF16)
                    sc.dma("sp", d_ga.ap(), gate_d[0].ap(), r=[("gate_d", 0)], w=["d_ga"])
                barrier()

            with contextlib.ExitStack() as esC:
                sbC, psC = mk(esC)
                Wk = sbC("Wk", [128, 8, 512], BF16)
                hvs = sbC("hvs", [128, 16, 5], F32)
                xks = [sbC(f"xk{i}", [128, 8, 640], F32) for i in range(2)]
                sqk = sbC("sqk", [128, 8, 640], BF16)
                rsks = [sbC(f"rsk{i}", [128, 640], F32) for i in range(2)]
                uTk = [sbC(f"uTk{i}", [128, 8, 640], BF16) for i in range(2)]
                kst = [sbC(f"kst{i}", [64, 2, 640], BF16) for i in range(2)]
                vst = [sbC(f"vst{i}", [128, 5, 2, 65], BF16) for i in range(2)]
                k2st = [sbC(f"k2st{i}", [64, 2, 256], BF16) for i in range(2)]
                v2st = [sbC(f"v2st{i}", [128, 2, 2, 65], BF16) for i in range(2)]
                p_st = psC("p_st")
                p_kw = [psC(f"p_kw{i}") for i in range(2)]
                p_vw = [psC(f"p_vw{i}", [128, 4, 128]) for i in range(2)]
                p_k2 = psC("p_k2", [128, 256])
                p_v2 = psC("p_v2", [128, 2, 128])
                sc.dma("pool", Wk[:], w_kv2.ap().rearrange("(kc p) n -> p kc n", p=128), w=["Wk"])
                fold_gain(Wk, "Wk", 0)
                sc.dma("sp", hvs[:], hv_in.ap(), w=["hvs"])
                xksrc = xT_kv2.ap().rearrange("(kc p) j t -> p kc j t", p=128)
                def loadK(j):
                    sc.dma("sp", xks[j % 2][:], xksrc[:, :, j, :], w=[("xk", j % 2)])

                def normK(j):
                    s = j % 2
                    norm_cols(xks[s], ("xk", s), sqk, "sqk", p_st, "p_st", rsks[s], ("rsk", s), uTk[s], ("uTk", s), 0, 640)

                loadK(0)
                loadK(1)
                normK(0)
                for j in range(16):
                    s = j % 2
                    if j + 1 < 16:
                        normK(j + 1)
                    for (c0, c1, pi) in ((0, 512, 0), (512, 640, 1)):
                        for kc in range(8):
                            sc.op("pe", "matmul", p_kw[pi][:, 0:c1 - c0], Wk[:, kc, 0:128], uTk[s][:, kc, c0:c1],
                                  start=(kc == 0), stop=(kc == 7), r=["Wk", ("uTk", s)], w=[("p_kw", pi)])
                        sc.op("dve", "tensor_copy", kst[s][0:64, 0, c0:c1], p_kw[pi][0:64, 0:c1 - c0],
                              r=[("p_kw", pi)], w=[("kst", s)])
                        sc.op("dve", "tensor_copy", kst[s][0:64, 1, c0:c1], p_kw[pi][64:128, 0:c1 - c0],
                              r=[("p_kw", pi)], w=[("kst", s)])
                    for t5 in range(5):
                        pv = p_vw[0][:, t5, :] if t5 < 4 else p_vw[1][:, 0, :]
                        for kc in range(8):
                            sc.op("pe", "matmul", pv, uTk[s][:, kc, t5 * 128:(t5 + 1) * 128], Wk[:, kc, 128:256],
                                  start=(kc == 0), stop=(kc == 7), r=["Wk", ("uTk", s)], w=[("p_vw", 0 if t5 < 4 else 1)])
                    sc.op("dve", "tensor_copy", vst[s][:, 0:4, :, 0:64], p_vw[0][:].rearrange("p t (g d) -> p t g d", g=2),
                          r=[("p_vw", 0)], w=[("vst", s)])
                    sc.op("dve", "tensor_copy", vst[s][:, 4, :, 0:64], p_vw[1][:, 0, :].rearrange("p (g d) -> p g d", g=2),
                          r=[("p_vw", 1)], w=[("vst", s)])
                    sc.op("dve", "tensor_copy", vst[s][:, :, :, 64],
                          hvs[:, j, :].unsqueeze(2).broadcast_to([128, 5, 2]), r=["hvs"], w=[("vst", s)])
                    for kc in range(8):
                        sc.op("pe", "matmul", p_k2[:], Wk[:, kc, 256:384], uTk[s][:, kc, 384:640],
                              start=(kc == 0), stop=(kc == 7), r=["Wk", ("uTk", s)], w=["p_k2"])
                    sc.op("dve", "tensor_copy", k2st[s][0:64, 0, :], p_k2[0:64, :], r=["p_k2"], w=[("k2st", s)])
                    sc.op("dve", "tensor_copy", k2st[s][0:64, 1, :], p_k2[64:128, :], r=["p_k2"], w=[("k2st", s)])
                    for t2 in range(2):
                        for kc in range(8):
                            sc.op("pe", "matmul", p_v2[:, t2, :], uTk[s][:, kc, (3 + t2) * 128:(4 + t2) * 128],
                                  Wk[:, kc, 384:512], start=(kc == 0), stop=(kc == 7), r=["Wk", ("uTk", s)], w=["p_v2"])
                    sc.op("dve", "tensor_copy", v2st[s][:, :, :, 0:64], p_v2[:].rearrange("p t (g d) -> p t g d", g=2),
                          r=["p_v2"], w=[("v2st", s)])
                    sc.op("dve", "tensor_copy", v2st[s][:, :, :, 64],
                          hvs[:, j, 3:5].unsqueeze(2).broadcast_to([128, 2, 2]), r=["hvs"], w=[("v2st", s)])
                    sc.dma("pool", kw_d.ap()[:, j], kst[s][:], r=[("kst", s)], w=["kw_d"])
                    sc.dma("pool", vw_d.ap()[:, j], vst[s][:], r=[("vst", s)], w=["vw_d"])
                    sc.dma("pool", k2_d.ap()[:, j], k2st[s][:], r=[("k2st", s)], w=["k2_d"])
                    sc.dma("pool", v2_d.ap()[:, j], v2st[s][:], r=[("v2st", s)], w=["v2_d"])
                    if j + 2 < 16:
                        loadK(j + 2)
                if "C" in dbg:
                    d_kw = dbgout("d_kw", [64, 16, 2, 640], BF16)
                    sc.dma("sp", d_kw.ap(), kw_d.ap(), r=["kw_d"], w=["d_kw"])
                    d_vw = dbgout("d_vw", [128, 16, 5, 2, 65], BF16)
                    sc.dma("sp", d_vw.ap(), vw_d.ap(), r=["vw_d"], w=["d_vw"])
                barrier()

            with contextlib.ExitStack() as esD:
                sbD, psD = mk(esD)
                QA = [[sbD(f"QA{p}{g}", [128, 7, 512], BF16) for g in range(2)] for p in range(2)]
                QS = [sbD(f"QS{p}", [64, 2, 512], BF16) for p in range(2)]
                KWj = [sbD(f"KWj{p}", [64, 2, 640], BF16) for p in range(2)]
                VWj = [sbD(f"VWj{p}", [128, 5 * 130 + 64], BF16) for p in range(2)]
                K2j = [sbD(f"K2j{p}", [64, 2, 256], BF16) for p in range(2)]
                V2j = [sbD(f"V2j{p}", [128, 2 * 130 + 64], BF16) for p in range(2)]
                atj = [sbD(f"atj{p}", [128, 310], BF16) for p in range(2)]
                trj = [sbD(f"trj{p}", [128, 128], F32) for p in range(2)]
                cend = sbD("cend", [128, 8], F32)
                band = sbD("band", [128, 8, 256], BF16)
                ident = sbD("ident", [128, 128], BF16)
                identf = sbD("identf", [128, 128], F32)
                esink = sbD("esink", [128, 8], F32)
                b31s = sbD("b31s", [1, 2, 512], F32)
                b31b = sbD("b31b", [1, 2, 512], BF16)
                cm = [sbD(f"cm{p}", [128, 8, 128], BF16) for p in range(2)]
                PT = [sbD(f"PT{i}", [128, 1024], BF16) for i in range(3)]
                NT = [sbD(f"NT{i}", [128, 512], BF16) for i in range(6)]
                score = sbD("score", [128, 310], F32)
                work = sbD("work", [128, 310], F32)
                work2 = sbD("work2", [128, 310], F32)
                top = sbD("top", [128, 16], F32)
                thr = sbD("thr", [128, 1], F32)
                rs4 = sbD("rs4", [128, 4], F32)
                fac = sbD("fac", [128, 4], F32)
                Mtok = [[sbD(f"Mtok{p}{g}", [128, 5, 128], BF16) for g in range(2)] for p in range(2)]
                rs4s = sbD("rs4s", [128, 4], F32)
                OT = [sbD(f"OT{i}", [128, 512], BF16) for i in range(10)]
                oacc = [[sbD(f"oa{p}", [128, 2, 4, 64], F32), sbD(f"ob{p}", [128, 2, 4, 64], F32)] for p in range(2)]
                oab = [sbD(f"oab{i}", [128, 512], BF16) for i in range(2)]
                oTs = [sbD(f"oTs{i}", [128, 4, 128], BF16) for i in range(2)]
                psS = [psD(f"psS{i}", [128, 2, 512]) for i in range(2)]
                psO = [psD("psO0")]
                psN = psD("psN")
                psM = [psD(f"psM{i}") for i in range(2)]

                sc.dma("sp", cend[:], cend_in.ap(), w=["cend"])
                sc.dma("sp", band[:], band_in.ap(), w=["band"])
                sc.dma("sp", ident[:], ident_in.ap(), w=["ident"])
                sc.dma("sp", identf[:], identf_in.ap(), w=["identf"])
                sc.dma("sp", esink[:], sinks_in.ap(), w=["esink"])
                sc.op("act", "activation", esink[:], esink[:], AF.Exp, r=["esink"], w=["esink"])
                sc.dma("sp", b31s[:], b31_in.ap(), w=["b31s"])
                sc.op("dve", "tensor_copy", b31b[:], b31s[:], r=["b31s"], w=["b31b"])
                sc.op("pool", "memset", score[:], 0.0, w=["score"])
                for p in range(2):
                    for g in range(2):
                        sc.op("pool", "memset", QA[p][g][:], 0.0, w=[("QA", p, g)])
                        sc.op("pool", "memset", Mtok[p][g][:], 0.0, w=[("Mtok", p, g)])
                        for slot in range(5):
                            sc.dma("sp", QA[p][g][126:127, slot, :], b31b[0:1, g, :], r=["b31b"], w=[("QA", p, g)])

                for p in range(2):
                    sc.op("pool", "memset", VWj[p][:], 0.0, w=[("VWj", p)])
                    sc.op("pool", "memset", V2j[p][:], 0.0, w=[("V2j", p)])
                rot = {"S": 0, "O": 0, "PT": 0, "tmp": 0, "NT": 0, "OT": 0, "M": 0}

                def nxt(name, n):
                    v = rot[name]
                    rot[name] = (v + 1) % n
                    return v

                def chunk_info(j):
                    k, typ = j // 2, j % 2
                    Bsel = 16 * k + (8 if typ == 0 else 16)
                    base = 16 * k - 7 if typ == 0 else 16 * k + 1
                    near = {kt: kt - base for kt in range(max(base, 0), min(base + 15, Bsel))}
                    nseg = (2 * Bsel + 61) // 62
                    nsegs = sorted(set((2 * kt) // 62 for kt in near))
                    far_segs = sorted(set((2 * kt) // 62 for kt in range(Bsel) if kt not in near))
                    return k, typ, Bsel, near, nseg, nsegs, far_segs

                def load_misc(j):
                    p = j % 2
                    sc.dma("sp", atj[p][:], atab_in.ap()[:, j, :], w=[("atj", p)])
                    sc.dma("sp", trj[p][:], trow_in.ap()[:, j, :], w=[("trj", p)])
                    sc.dma("sp", KWj[p][:], kw_d.ap()[:, j], r=["kw_d"], w=[("KWj", p)])
                    sc.dma("sp", VWj[p][:, 0:650], vw_d.ap()[:, j].rearrange("p t g e -> p (t g e)"), r=["vw_d"], w=[("VWj", p)])
                    sc.dma("sp", K2j[p][:], k2_d.ap()[:, j], r=["k2_d"], w=[("K2j", p)])
                    sc.dma("sp", V2j[p][:, 0:260], v2_d.ap()[:, j].rearrange("p t g e -> p (t g e)"), r=["v2_d"], w=[("V2j", p)])
                    for g in range(2):
                        sc.dma("sp", QS[p][:, g, :].rearrange("p (r q) -> p r q", r=4), qs_d.ap()[:, j, 4 * g:4 * g + 4, :],
                               r=["qs_d"], w=[("QS", p)])

                def load_QA(j, g):
                    p = j % 2
                    k, typ, Bsel, near, nseg, nsegs, far_segs = chunk_info(j)
                    slots = list(far_segs) + [5 + (sg_ - nsegs[0]) for sg_ in nsegs]
                    for slot in slots:
                        sc.dma("sp", QA[p][g][0:64, slot, :].rearrange("p (r q) -> p r q", r=4),
                               qn_d.ap()[:, j, 4 * g:4 * g + 4, :], r=["qn_d"], w=[("QA", p, g)])

                def load_chunk(j):
                    load_misc(j)
                    load_QA(j, 0)
                    load_QA(j, 1)

                def evac(bO):
                    o = nxt("OT", 10)
                    sc.op("act", "activation", OT[o][0:65, :], psO[bO][0:65, :], AF.Copy, r=[("psO", bO)], w=[("OT", o)])
                    return o

                def normalize(j, g, o, kind):
                    trv = psN[:].bitcast(BF16)[:, 0:264].rearrange("p (h e) -> p h e", h=4)
                    for h in range(4):
                        sc.op("pe", "transpose", trv[:, h, 0:65], OT[o][0:65, h * 128:(h + 1) * 128], ident[0:65, 0:65],
                              r=[("OT", o), "ident"], w=["psN"])
                    if kind == 3:
                        sc.op("dve", "tensor_tensor", rs4[:], trv[:, :, 64], esink[:, 4 * g:4 * g + 4], ALU.add,
                              r=["psN", "esink"], w=["rs4"])
                    else:
                        sc.op("dve", "tensor_scalar", rs4[:], trv[:, :, 64], 1e-30, None, ALU.max, r=["psN"], w=["rs4"])
                    sc.op("dve", "reciprocal", fac[:], rs4[:], r=["rs4"], w=["fac"])
                    if kind != 3:
                        gv = GS[:, j, :].rearrange("p (g r b) -> p g r b", g=2, r=4)[:, g, :, kind]
                        sc.op("dve", "tensor_tensor", fac[:], fac[:], gv, ALU.mult, r=["fac", "GS"], w=["fac"])
                    ab = 1 if kind == 3 else 0
                    acc = oacc[j % 2][ab]
                    akey = ("oacc", j % 2, ab)
                    for h in range(4):
                        if kind in (0, 3):
                            sc.op("dve", "tensor_scalar", acc[:, g, h, :], trv[:, h, 0:64], fac[:, h:h + 1], None, ALU.mult,
                                  r=["psN", "fac"], w=[akey])
                        else:
                            sc.op("dve", "scalar_tensor_tensor", acc[:, g, h, :], trv[:, h, 0:64], fac[:, h:h + 1],
                                  acc[:, g, h, :], ALU.mult, ALU.add, r=["psN", "fac", akey], w=[akey])

                def attn_tiles(p, g, tiles, qrhs, qkeys):
                    bO = 0
                    n = len(tiles)
                    npair = (n + 1) // 2
                    state = {}
                    ntbuf = {}
                    LOOK = 4

                    def issue_nt(i):
                        t = tiles[i]
                        if t["nt"] is not None:
                            nti = nxt("NT", 6)
                            sc.dma("pool", NT[nti][:], t["nt"], w=[("NT", nti)])
                            ntbuf[i] = nti

                    def issue_S(pi):
                        b = nxt("S", 2)
                        for h2 in range(2):
                            i = 2 * pi + h2
                            if i >= n:
                                break
                            t = tiles[i]
                            nti = ntbuf.get(i)
                            sc.op("pe", "matmul", psS[b][:, h2, :], t["lhsT"], qrhs(t), start=True, stop=(nti is None),
                                  r=t["lkeys"] + qkeys, w=[("psS", b, h2)])
                            if nti is not None:
                                sc.op("pe", "matmul", psS[b][:, h2, :], ident[:], NT[nti][:], start=False, stop=True,
                                      r=["ident", ("NT", nti)], w=[("psS", b, h2)])
                        state[pi] = b

                    def issue_rest(pi):
                        b = state[pi]
                        w2 = min(2, n - 2 * pi)
                        pt = nxt("PT", 3)
                        sc.op("act", "activation", PT[pt][:, 0:w2 * 512], psS[b][:, 0:w2, :].rearrange("p a c -> p (a c)"),
                              AF.Exp, r=[("psS", b, h2) for h2 in range(w2)], w=[("PT", pt)])
                        for h2 in range(w2):
                            t = tiles[2 * pi + h2]
                            if t.get("post") is not None:
                                t["post"](PT[pt][:, h2 * 512:(h2 + 1) * 512], ("PT", pt))
                        for h2 in range(w2):
                            i = 2 * pi + h2
                            t = tiles[i]
                            sc.op("pe", "matmul", psO[bO][:], t["v"], PT[pt][:, h2 * 512:(h2 + 1) * 512],
                                  start=(i == 0), stop=(i == n - 1), r=t["vkeys"] + [("PT", pt)], w=[("psO", bO)])
                        for h2 in range(w2):
                            i = 2 * pi + h2
                            t = tiles[i]
                            if t.get("extra") is not None:
                                t["extra"](PT[pt][:, h2 * 512:(h2 + 1) * 512], ("PT", pt), i, n)

                    for i in range(min(LOOK, n)):
                        issue_nt(i)
                    issue_S(0)
                    for pi in range(npair):
                        for i in range(2 * pi + LOOK, min(2 * pi + LOOK + 2, n)):
                            issue_nt(i)
                        if pi + 1 < npair:
                            issue_S(pi + 1)
                        issue_rest(pi)
                        between()
                    return bO

                pend = []
                tick = [0]

                def between():
                    tick[0] += 1
                    if tick[0] % 3 == 0 and pend:
                        normalize(*pend.pop(0))

                def drain():
                    while pend:
                        normalize(*pend.pop(0))

                def cmp_sel(j, g):
                    p = j % 2
                    k, typ, Bsel, near, nseg, nsegs, far_segs = chunk_info(j)
                    NCT = k + 1
                    slotq = far_segs[0] if far_segs else 5
                    tiles = []
                    for ct in range(NCT):
                        def post(pta, ptk, ct=ct):
                            sc.op("dve", "tensor_tensor", pta.rearrange("p (h q) -> p h q", h=4),
                                  pta.rearrange("p (h q) -> p h q", h=4),
                                  cm[p][:, ct, :].unsqueeze(1).broadcast_to([128, 4, 128]), ALU.mult,
                                  r=[ptk, ("cm", p)], w=[ptk])

                        def extra(pta, ptk, i, n, ct=ct):
                            for h in range(4):
                                dst = psM[h // 2][:].rearrange("p (a c) -> p a c", a=2)[:, h % 2, :]
                                sc.op("pe", "matmul", dst, pta[:, h * 128:(h + 1) * 128], band[:, ct, :],
                                      start=(i == 0 and h % 2 == 0), stop=(i == n - 1 and h % 2 == 1), r=[ptk, "band"],
                                      w=[("psM", h // 2)])
                        tiles.append(dict(lhsT=KC[0:64, g, ct * 128:(ct + 1) * 128], lkeys=["KC"], nt=None,
                                          v=VCf[:, (ct * 2 + g) * 65:(ct * 2 + g) * 65 + 128], vkeys=["VC"],
                                          post=post, extra=extra))
                    bO = attn_tiles(p, g, tiles, lambda t: QA[p][g][0:64, slotq, :], [("QA", p, g)])
                    pend.append((j, g, evac(bO), 0))
                    for i2 in range(2):
                        sc.op("dve", "tensor_scalar", rs4s[:, 2 * i2:2 * i2 + 2],
                              psM[i2][:].rearrange("p (a c) -> p a c", a=2)[:, :, 0], 1e-30, None, ALU.max,
                              r=[("psM", i2)], w=["rs4s"])
                    sc.op("dve", "reciprocal", rs4s[:], rs4s[:], r=["rs4s"], w=["rs4s"])
                    for h in range(4):
                        src = psM[h // 2][:, (h % 2) * 256:(h % 2) * 256 + 256]
                        if h == 0:
                            sc.op("dve", "tensor_scalar", score[:, 0:256], src, rs4s[:, 0:1], None, ALU.mult,
                                  r=[("psM", 0), "rs4s"], w=["score"])
                        else:
                            sc.op("dve", "scalar_tensor_tensor", score[:, 0:256], src, rs4s[:, h:h + 1], score[:, 0:256],
                                  ALU.mult, ALU.add, r=[("psM", h // 2), "rs4s", "score"], w=["score"])
                    sc.op("dve", "tensor_tensor", work[:], score[:], atj[p][:], ALU.add, r=["score", ("atj", p)], w=["work"])
                    sc.op("dve", "max", top[:, 0:8], work[:], r=["work"], w=["top"])
                    sc.op("dve", "match_replace", work2[:], top[:, 0:8], work[:], -1e9, r=["top", "work"], w=["work2"])
                    sc.op("dve", "max", top[:, 8:16], work2[:], r=["work2"], w=["top"])
                    sc.op("dve", "tensor_scalar", thr[:], top[:, 15:16], -4096.0, None, ALU.max, r=["top"], w=["thr"])
                    sc.op("dve", "tensor_scalar", Mtok[p][g][:, :, 64:126], work[:].rearrange("p (m c) -> p m c", m=5),
                          thr[:, 0:1], NEG, ALU.is_lt, ALU.mult, r=["work", "thr"], w=[("Mtok", p, g)])

                def make_cm(j):
                    p = j % 2
                    for ct in range(j // 2 + 1):
                        sc.op("dve", "tensor_scalar", cm[p][:, ct, :], trj[p][:], cend[:, ct:ct + 1], None, ALU.is_ge,
                              r=[("trj", p), "cend"], w=[("cm", p)])

                def drain_upto(jmax):
                    keep = []
                    while pend:
                        e = pend.pop(0)
                        if e[0] <= jmax:
                            normalize(*e)
                        else:
                            keep.append(e)
                    pend.extend(keep)

                def trbqa(j):
                    p = j % 2
                    k, typ, Bsel, near, nseg, nsegs, far_segs = chunk_info(j)
                    for g in range(2):
                        trb = psM[g][:].bitcast(BF16).rearrange("p (m c) -> p m c", m=8)
                        for m in range(nseg):
                            sc.op("pe", "transpose", trb[:, m, :], Mtok[p][g][:, m, :], ident[:],
                                  r=[("Mtok", p, g), "ident"], w=[("psM", g)])
                        if far_segs:
                            f0, f1 = far_segs[0], far_segs[-1] + 1
                            sc.op("dve", "tensor_copy",
                                  QA[p][g][64:126, f0:f1, :].rearrange("p s (h q) -> p s h q", h=4),
                                  trb[64:126, f0:f1, :].unsqueeze(2).broadcast_to([62, f1 - f0, 4, 128]),
                                  r=[("psM", g)], w=[("QA", p, g)])
                        if nsegs:
                            n0_, n1_ = nsegs[0], nsegs[-1] + 1
                            sc.op("dve", "tensor_copy",
                                  QA[p][g][64:126, 5:5 + n1_ - n0_, :].rearrange("p s (h q) -> p s h q", h=4),
                                  trb[64:126, n0_:n1_, :].unsqueeze(2).broadcast_to([62, n1_ - n0_, 4, 128]),
                                  r=[("psM", g)], w=[("QA", p, g)])

                def winswa(j, g):
                    p = j % 2
                    k, typ, Bsel, near, nseg, nsegs, far_segs = chunk_info(j)
                    slotq = far_segs[0] if far_segs else 5
                    tiles = []
                    for i5 in range(5):
                        tiles.append(dict(lhsT=KWj[p][0:64, g, i5 * 128:(i5 + 1) * 128], lkeys=[("KWj", p)],
                                          nt=nt_win.ap()[i5, g], v=VWj[p][:, (i5 * 2 + g) * 65:(i5 * 2 + g) * 65 + 128],
                                          vkeys=[("VWj", p)]))
                    bO = attn_tiles(p, g, tiles, lambda t: QA[p][g][0:64, slotq, :], [("QA", p, g)])
                    pend.append((j, g, evac(bO), 2))
                    tiles = []
                    for i2 in range(2):
                        tiles.append(dict(lhsT=K2j[p][0:64, g, i2 * 128:(i2 + 1) * 128], lkeys=[("K2j", p)],
                                          nt=nt_swa.ap()[i2, g], v=V2j[p][:, (i2 * 2 + g) * 65:(i2 * 2 + g) * 65 + 128],
                                          vkeys=[("V2j", p)]))
                    bO = attn_tiles(p, g, tiles, lambda t: QS[p][0:64, g, :], [("QS", p)])
                    pend.append((j, g, evac(bO), 3))

                def selloop(j, g):
                    p = j % 2
                    k, typ, Bsel, near, nseg, nsegs, far_segs = chunk_info(j)
                    tiles = []
                    for kt in range(Bsel):
                        sg_ = (2 * kt) // 62
                        if kt in near:
                            slot = 5 + (sg_ - nsegs[0])
                            nt = nt_sel.ap()[typ, near[kt], g]
                        else:
                            slot = sg_
                            nt = None
                        tiles.append(dict(lhsT=KT[0:127, g, kt * 128:(kt + 1) * 128], lkeys=[("KT", g), ("KTc", g)],
                                          nt=nt, v=VAf[:, (kt * 2 + g) * 65:(kt * 2 + g) * 65 + 128], vkeys=["VA"], slot=slot))
                    bO = attn_tiles(p, g, tiles, lambda t: QA[p][g][0:127, t["slot"], :], [("QA", p, g)])
                    pend.append((j, g, evac(bO), 1))

                def final_copy(j):
                    for ab in range(2):
                        sc.op("dve", "tensor_copy", oab[ab][:], oacc[j % 2][ab][:].rearrange("p g h d -> p (g h d)"),
                              r=[("oacc", j % 2, ab)], w=[("oab", ab)])

                def final_T(j):
                    for ab in range(2):
                        trb = psN[:].bitcast(BF16).rearrange("p (m c) -> p m c", m=8)
                        for c4 in range(4):
                            sc.op("pe", "transpose", trb[:, c4, :], oab[ab][:, c4 * 128:(c4 + 1) * 128], ident[:],
                                  r=[("oab", ab), "ident"], w=["psN"])
                        sc.op("dve", "tensor_copy", oTs[ab][:], trb[:, 0:4, :], r=["psN"], w=[("oTs", ab)])
                        dst = (oat_d if ab == 0 else obt_d).ap()[:, :, j * 128:(j + 1) * 128]
                        sc.dma("sp", dst, oTs[ab][:], r=[("oTs", ab)], w=[("o_d", ab)])

                jlist = list(range(16)) if "Dshort" not in dbg else [0, 1]
                NJ = len(jlist)
                load_chunk(0)
                make_cm(0)
                cmp_sel(0, 0)
                cmp_sel(0, 1)
                trbqa(0)
                if NJ > 1:
                    load_chunk(1)
                    make_cm(1)
                for j in jlist:
                    if j + 1 < NJ:
                        cmp_sel(j + 1, 0)
                    if j > 0:
                        drain_upto(j - 1)
                        final_copy(j - 1)
                    winswa(j, 0)
                    if j + 1 < NJ:
                        cmp_sel(j + 1, 1)
                    winswa(j, 1)
                    if j > 0:
                        final_T(j - 1)
                    if j + 2 < NJ:
                        load_misc(j + 2)
                    selloop(j, 0)
                    if j + 2 < NJ:
                        load_QA(j + 2, 0)
                        make_cm(j + 2)
                    selloop(j, 1)
                    if j + 2 < NJ:
                        load_QA(j + 2, 1)
                    if j + 1 < NJ:
                        trbqa(j + 1)
                drain()
                final_copy(NJ - 1)
                final_T(NJ - 1)
                if "D" in dbg:
                    d_oa = dbgout("d_oa", [128, 4, 2048], BF16)
                    sc.dma("sp", d_oa.ap(), oat_d.ap(), r=[("o_d", 0)], w=["d_oa"])
                    d_ob = dbgout("d_ob", [128, 4, 2048], BF16)
                    sc.dma("sp", d_ob.ap(), obt_d.ap(), r=[("o_d", 1)], w=["d_ob"])
                barrier()

        with contextlib.ExitStack() as esE:
            sbE, psE = mk(esE)
            W1m = sbE("W1m", [128, 8, 4096], BF16)
            W2m = sbE("W2m", [128, 32, 1024], BF16)
            EE = 128
            with contextlib.ExitStack() as esE1:
                sbE1, psE1 = mk(esE1)
                WuA = sbE1("WuA", [128, 4, 1024], BF16)
                WuB = sbE1("WuB", [128, 4, 1024], BF16)
                Wo = sbE1("Wo", [128, 8, 1024], BF16)
                oaT = [sbE1(f"oaT{i}", [128, 4, EE], BF16) for i in range(2)]
                obT = [sbE1(f"obT{i}", [128, 4, EE], BF16) for i in range(2)]
                GA = [sbE1(f"GA{i}", [128, 8, EE], BF16) for i in range(2)]
                GB = [sbE1(f"GB{i}", [128, 8, EE], BF16) for i in range(2)]
                xo = [sbE1(f"xoE{i}", [128, 8, EE], F32) for i in range(3)]
                t1 = [sbE1(f"t1E{i}", [128, EE], F32) for i in range(2)]
                t2 = [sbE1(f"t2E{i}", [128, EE], F32) for i in range(2)]
                t3 = [sbE1(f"t3E{i}", [128, EE], F32) for i in range(2)]
                yTs = [sbE1(f"yT{i}", [128, 8, EE], BF16) for i in range(2)]
                hst = sbE1("hst", [128, 8, EE], F32)
                zTs = [sbE1(f"zT{i}", [128, 8, EE], F32) for i in range(2)]
                zsq = sbE1("zsq", [128, 8, EE], BF16)
                rsE = sbE1("rsE", [128, EE], F32)
                pA = [psE1(f"pA{i}") for i in range(2)]
                pB = [psE1(f"pB{i}") for i in range(2)]
                pZ = [psE1(f"pZ{i}") for i in range(2)]
                pSt = psE1("pSt")
                sc.dma("pool", WuA[:], w_upA.ap().rearrange("(c p) n -> p c n", p=128), w=["WuA"])
                sc.dma("pool", WuB[:], w_upB.ap().rearrange("(c p) n -> p c n", p=128), w=["WuB"])
                sc.dma("pool", Wo[:], w_out.ap().rearrange("(c p) n -> p c n", p=128), w=["Wo"])
                for q4 in range(4):
                    sc.dma("pool", W1m[:, :, q4 * 1024:(q4 + 1) * 1024],
                           w_m1.ap()[:, q4 * 1024:(q4 + 1) * 1024].rearrange("(c p) n -> p c n", p=128), w=["W1m"])
                for q4 in range(4):
                    sc.dma("pool", W2m[:, q4 * 8:(q4 + 1) * 8, :],
                           w_m2.ap()[q4 * 1024:(q4 + 1) * 1024, :].rearrange("(c p) n -> p c n", p=128), w=["W2m"])
                xosrc = xT_own.ap().rearrange("(kc p) t -> p kc t", p=128)
                def loadE(eb):
                    s = eb % 2
                    cs = slice(eb * EE, (eb + 1) * EE)
                    sc.dma("sp", oaT[s][:], oat_d.ap()[:, :, cs], r=[("o_d", 0)], w=[("oaT", s)])
                    sc.dma("sp", obT[s][:], obt_d.ap()[:, :, cs], r=[("o_d", 1)], w=[("obT", s)])
                    sc.dma("sp", GA[s][:], gate_d[0].ap()[:, :, cs], r=[("gate_d", 0)], w=[("GA", s)])
                    sc.dma("sp", GB[s][:], gate_d[1].ap()[:, :, cs], r=[("gate_d", 1)], w=[("GB", s)])
                    sc.dma("sp", xo[eb % 3][:], xosrc[:, :, cs], w=[("xoE", eb % 3)])

                def upE(eb):
                    s = eb % 2
                    yT = yTs[s]
                    for fc in range(8):
                        b = fc % 2
                        for c4 in range(4):
                            sc.op("pe", "matmul", pA[b][:, 0:EE], WuA[:, c4, fc * 128:(fc + 1) * 128], oaT[s][:, c4, :],
                                  start=(c4 == 0), stop=(c4 == 3), r=["WuA", ("oaT", s)], w=[("pA", b)])
                        for c4 in range(4):
                            sc.op("pe", "matmul", pB[b][:, 0:EE], WuB[:, c4, fc * 128:(fc + 1) * 128], obT[s][:, c4, :],
                                  start=(c4 == 0), stop=(c4 == 3), r=["WuB", ("obT", s)], w=[("pB", b)])
                        sc.op("dve", "tensor_tensor", t1[b][:], pA[b][:, 0:EE], GA[s][:, fc, :], ALU.mult,
                              r=[("pA", b), ("GA", s)], w=[("t1E", b)])
                        sc.op("dve", "tensor_tensor", t2[b][:], pB[b][:, 0:EE], GB[s][:, fc, :], ALU.mult,
                              r=[("pB", b), ("GB", s)], w=[("t2E", b)])
                        sc.op("dve", "tensor_tensor", yT[:, fc, :], t1[b][:], t2[b][:], ALU.add,
                              r=[("t1E", b), ("t2E", b)], w=[("yT", s)])

                def outmmE(eb):
                    s = eb % 2
                    yT = yTs[s]
                    zT = zTs[s]
                    for fo in range(8):
                        b = fo % 2
                        for fc in range(8):
                            sc.op("pe", "matmul", pZ[b][:, 0:EE], Wo[:, fc, fo * 128:(fo + 1) * 128], yT[:, fc, :],
                                  start=(fc == 0), stop=(fc == 7), r=["Wo", ("yT", s)], w=[("pZ", b)])
                        sc.op("act", "activation", zT[:, fo, :], pZ[b][:, 0:EE], AF.Copy, r=[("pZ", b)], w=[("zT", s)])

                def finE(eb):
                    s = eb % 2
                    zT = zTs[s]
                    cs = slice(eb * EE, (eb + 1) * EE)
                    norm_cols(zT, ("zT", s), zsq, "zsq", pSt, "pSt", rsE, "rsE", None, None, 1, EE)
                    for fo in range(8):
                        b = fo % 2
                        sc.op("dve", "scalar_tensor_tensor", t3[b][:], zT[:, fo, :], gn[:, 1, fo:fo + 1], rsE[:],
                              ALU.mult, ALU.mult, r=[("zT", s), "rsE", "gn"], w=[("t3E", b)])
                        sc.op("dve", "tensor_tensor", hst[:, fo, :], t3[b][:], xo[eb % 3][:, fo, :], ALU.add,
                              r=[("t3E", b), ("xoE", eb % 3)], w=["hst"])
                    sc.dma("act", ht_d.ap()[:, :, cs], hst[:], r=["hst"], w=["ht_d"])

                NEE = 2048 // EE
                loadE(0)
                loadE(1)
                upE(0)
                for eb in range(NEE):
                    if eb + 1 < NEE:
                        upE(eb + 1)
                    outmmE(eb)
                    if eb >= 1:
                        finE(eb - 1)
                    if eb + 2 < NEE:
                        loadE(eb + 2)
                finE(NEE - 1)
                fold_gain(W1m, "W1m", 2)
                if "E" in dbg:
                    d_h = dbgout("d_h", [128, 8, 2048])
                    sc.dma("sp", d_h.ap(), ht_d.ap(), r=["ht_d"], w=["d_h"])
                barrier()

            with contextlib.ExitStack() as esF:
                sbF, psF = mk(esF)
                hT = [sbF(f"hT{i}", [128, 8, EB], F32) for i in range(3)]
                hsq = sbF("hsq", [128, 8, EB], BF16)
                rsF = sbF("rsF", [128, EB], F32)
                rsO = sbF("rsO", [128, EB], F32)
                u2 = [sbF(f"u2{i}", [128, 8, EB], BF16) for i in range(2)]
                rl = [sbF(f"rl{i}", [128, EB], BF16) for i in range(2)]
                hid = sbF("hid", [128, 32, EB], BF16)
                fT = sbF("fT", [128, 8, EB], F32)
                fsq = sbF("fsq", [128, 8, EB], BF16)
                t1 = [sbF(f"t1F{i}", [128, EB], F32) for i in range(2)]
                pH = [psF(f"pH{i}") for i in range(3)]
                pF = [psF(f"pF{i}") for i in range(2)]
                pSt = psF("pStF")
                pSo = psF("pStO")
                outv = out.ap().rearrange("(c p) t -> p c t", p=128)
                NFB = 2048 // EB

                def loadF(fb):
                    sc.dma("sp", hT[fb % 3][:], ht_d.ap()[:, :, fb * EB:(fb + 1) * EB], r=["ht_d"], w=[("hT", fb % 3)])

                def normF(fb):
                    s = fb % 2
                    norm_cols(hT[fb % 3], ("hT", fb % 3), hsq, "hsq", pSt, "pStF", rsF, "rsF", u2[s], ("u2", s), 2, EB,
                              split=(fb > 0))

                def hiddenF(fb):
                    s = fb % 2
                    for hc in range(32):
                        b = hc % 3
                        for kc in range(8):
                            sc.op("pe", "matmul", pH[b][:, 0:EB], W1m[:, kc, hc * 128:(hc + 1) * 128], u2[s][:, kc, :],
                                  start=(kc == 0), stop=(kc == 7), r=["W1m", ("u2", s)], w=[("pH", b)])
                        sc.op("act", "activation", rl[hc % 2][:], pH[b][:, 0:EB], AF.Relu, r=[("pH", b)], w=[("rl", hc % 2)])
                        sc.op("pool" if (fb > 0 and hc % 2 == 1) else "dve", "tensor_tensor", hid[:, hc, :], rl[hc % 2][:],
                              rl[hc % 2][:], ALU.mult, r=[("rl", hc % 2)], w=["hid"])

                def secondF(fb):
                    for fo in range(8):
                        b = fo % 2
                        for hc in range(32):
                            sc.op("pe", "matmul", pF[b][:, 0:EB], W2m[:, hc, fo * 128:(fo + 1) * 128], hid[:, hc, :],
                                  start=(hc == 0), stop=(hc == 31), r=["W2m", "hid"], w=[("pF", b)])
                        sc.op("act", "activation", fT[:, fo, :], pF[b][:, 0:EB], AF.Copy, r=[("pF", b)], w=["fT"])

                def outF(fb):
                    s = fb % 2
                    cs = slice(fb * EB, (fb + 1) * EB)
                    norm_cols(fT, "fT", fsq, "fsq", pSo, "pStO", rsO, "rsO", None, None, 3, EB)
                    for fo in range(8):
                        b = fo % 2
                        sc.op("dve", "scalar_tensor_tensor", t1[b][:], fT[:, fo, :], gn[:, 3, fo:fo + 1], rsO[:],
                              ALU.mult, ALU.mult, r=["fT", "rsO", "gn"], w=[("t1F", b)])
                        sc.op("dve" if fo % 2 == 0 else "pool", "tensor_tensor", hT[fb % 3][:, fo, :], t1[b][:],
                              hT[fb % 3][:, fo, :], ALU.add, r=[("t1F", b), ("hT", fb % 3)], w=[("hT", fb % 3)])
                    sc.dma("act", outv[:, :, cs], hT[fb % 3][:], r=[("hT", fb % 3)], w=["out"])

                loadF(0)
                loadF(1)
                loadF(2)
                normF(0)
                hiddenF(0)
                for fb in range(NFB):
                    if fb + 1 < NFB:
                        normF(fb + 1)
                    secondF(fb)
                    if fb + 1 < NFB:
                        hiddenF(fb + 1)
                    outF(fb)
                    if fb + 3 < NFB:
                        loadF(fb + 3)

        sc.wait_all("sp", ["out"] + list(dbo.keys()))
    print("instructions:", sc.ninst, "sems:", sc.nsem)
    return nc


IN_SIZES = (512,) + (128,) * 6 + (24, 512, 128, 128, 1024, 1024)
OFF = np.concatenate([[0], np.cumsum(IN_SIZES)]).astype(int)


def _bias_table(relx, delta, heads, lo, hi):
    p = np.arange(128)[:, None]
    q = np.arange(128)[None, :]
    dist = 128 * delta + q - p
    valid = (dist >= lo) & (dist < hi)
    b = np.where(valid, t5_bucket_np(dist), 32)
    out = np.stack([relx[b, h] for h in heads], axis=1)
    return np.ascontiguousarray(out.reshape(128, 512))


def prep_inputs(inputs):
    f = lambda k: np.asarray(inputs[k], np.float32)
    x = f("x")[0]
    w_in = f("w_in")[0]
    xT = np.ascontiguousarray(x.T)
    seg = lambda i: w_in[:, OFF[i]:OFF[i + 1]]
    cat = lambda l: np.ascontiguousarray(np.concatenate(l, axis=1))
    gains = np.zeros((128, 4, 8), np.float32)
    for gi, nm in enumerate(["norm_mix_pre", "norm_mix_post", "norm_mlp_pre", "norm_mlp_post"]):
        gains[:, gi, :] = f(nm)[0].reshape(8, 128).T
    indic = np.zeros((63, S), np.float32)
    blk = np.arange(S) // 64
    indic[blk % 62, np.arange(S)] = 1.0
    indic[62, :] = 1.0
    rel = f("rel_bias")
    relx = np.concatenate([rel, np.full((1, 16), NEG, np.float32)], axis=0)
    b31row = np.zeros((1, 2, 512), np.float32)
    for g in range(2):
        for h in range(4):
            b31row[0, g, h * 128:(h + 1) * 128] = rel[31, 4 * g + h]
    pcol = np.arange(128)[:, None]
    cend = (16 * (np.arange(8)[None, :] * 128 + pcol) + 31).astype(np.float32)
    n = (np.arange(8)[None, :, None] * 128 + pcol[:, :, None])
    jb = np.arange(256)[None, None, :]
    band = ((n >= 4 * jb - 1) & (n <= 4 * jb + 3) & (n <= 1022)).astype(np.float32)
    band[:, :, 0] = 1.0
    nt_win = np.stack([np.stack([_bias_table(relx, 4 - i, [4 * g + h for h in range(4)], 0, 512) for g in range(2)])
                       for i in range(5)])
    nt_swa = np.stack([np.stack([_bias_table(relx, 1 - i, [8 + 4 * g + h for h in range(4)], 0, 128) for g in range(2)])
                       for i in range(2)])
    common = dict(
        xT_all=xT,
        w_A=cat([seg(1), seg(2), seg(3), seg(4)]), w_Q=cat([seg(0), seg(8)]),
        w_ga=np.ascontiguousarray(seg(11)), w_gb=np.ascontiguousarray(seg(12)),
        w_kv2=cat([seg(5), seg(6), seg(9), seg(10)]), w_gn=np.ascontiguousarray(seg(7)),
        gains=gains, indic=indic.astype(ml_dtypes.bfloat16),
        w1k=f("cmp_w1_k")[0], w1v=f("cmp_w1_v")[0], w2k=f("cmp_w2_k")[0], w2v=f("cmp_w2_v")[0],
        posk=np.ascontiguousarray(f("cmp_pos_k")[0].reshape(16, 128).T),
        posv=np.ascontiguousarray(f("cmp_pos_v")[0].reshape(16, 128).T),
        nt_win=nt_win, nt_swa=nt_swa, b31row=b31row,
        sinks=np.ascontiguousarray(np.tile(f("attn_sinks")[0][None, :], (128, 1))),
        cend=cend, band=band.astype(ml_dtypes.bfloat16),
        ident=np.eye(128, dtype=np.float32).astype(ml_dtypes.bfloat16), identf=np.eye(128, dtype=np.float32),
        w_upA=f("w_up_nsa")[0], w_upB=f("w_up_swa")[0], w_out=f("w_out")[0],
        w_m1=f("w_mlp_in")[0], w_m2=f("w_mlp_out")[0],
    )
    in_maps = []
    for c in range(NCORES):
        own = own_chunks(c)
        m = dict(common)
        m["xT_own"] = np.ascontiguousarray(np.concatenate([xT[:, ci * 128:(ci + 1) * 128] for ci in own], axis=1))
        kv2 = np.zeros((D, 16, 640), np.float32)
        hv = np.zeros((128, 16, 5), np.float32)
        atab = np.zeros((128, 16, 310), np.float32)
        trow = np.zeros((128, 16, 128), np.float32)
        bl = np.arange(310)[None, :]
        for j, ci in enumerate(own):
            for i in range(5):
                cc = ci - 4 + i
                if cc >= 0:
                    kv2[:, j, i * 128:(i + 1) * 128] = xT[:, cc * 128:(cc + 1) * 128]
                    hv[:, j, i] = 1.0
            t = 128 * ci + np.arange(128)[:, None]
            cur = t // 64
            valid = (bl <= cur) & (bl < 256)
            forced = (bl == 0) | (bl == cur) | (bl == cur - 1)
            atab[:, j, :] = np.where(valid, np.where(forced, 8192.0, 0.0), -8192.0)
            trow[:, j, :] = 128 * ci + np.arange(128)[None, :]
        m["xT_kv2"] = kv2
        m["hv"] = hv
        m["atab"] = atab.astype(ml_dtypes.bfloat16)
        m["trow"] = trow
        nts = np.zeros((2, 15, 2, 128, 512), np.float32)
        for s in range(15):
            for g in range(2):
                heads = [4 * g + h for h in range(4)]
                nts[0, s, g] = _bias_table(relx, c + 7 - s, heads, 0, 1 << 30)
                nts[1, s, g] = _bias_table(relx, 14 - c - s, heads, 0, 1 << 30)
        m["nt_sel"] = nts
        in_maps.append(m)
    return in_maps


def kernel(**inputs):
    dbg = set(os.environ.get("KDBG", "").split(",")) - {""}
    in_maps = prep_inputs(inputs)
    nc = build(dbg)
    res = run_bass_kernel_spmd(nc, in_maps, core_ids=list(range(NCORES)))
    if dbg:
        kernel.last = res
        kernel.in_maps = in_maps
    outT = np.zeros((D, S), np.float32)
    for c in range(NCORES):
        o = np.asarray(res.results[c]["out"])
        for j, ci in enumerate(own_chunks(c)):
            outT[:, ci * 128:(ci + 1) * 128] = o[:, j * 128:(j + 1) * 128]
    return np.ascontiguousarray(outT.T)[None].astype(np.float32)
```

```python
import contextlib
import os
import numpy as np
import ml_dtypes
import concourse.bass as bass
import concourse.mybir as mybir
from concourse.bass_utils import run_bass_kernel_spmd

F32 = mybir.dt.float32
BF16 = mybir.dt.bfloat16
ALU = mybir.AluOpType
AF = mybir.ActivationFunctionType
AX = mybir.AxisListType

NCORES = 8
S = 16384
D = 1024
NCH = S // 128
NOWN = 16
EPS = 1e-6
NEG = -30000.0
SCALE = 0.125


class Sch:
    def __init__(self, nc, es):
        self.nc, self.es = nc, es
        self.eng = {"pe": nc.tensor, "dve": nc.vector, "act": nc.scalar, "pool": nc.gpsimd, "sp": nc.sync}
        self.nsem = 0
        self.psem, self.pcnt = {}, {}
        for e in self.eng:
            self._new_psem(e)
        self.waited = {}
        self.lastw = {}
        self.readers = {}
        self.dsem = {}
        self.ninst = 0

    def _sem(self, name):
        self.nsem += 1
        return self.es.enter_context(self.nc.semaphore(f"{name}_{self.nsem}"))

    def _new_psem(self, e):
        self.psem[e] = self._sem("p_" + e)
        self.pcnt[e] = 0

    def _deps(self, r, w):
        toks = []
        for k in r:
            if k in self.lastw:
                toks.append(self.lastw[k])
        for k in w:
            if k in self.lastw:
                toks.append(self.lastw[k])
            toks.extend(self.readers.get(k, {}).values())
        return toks

    def _wait(self, e, toks):
        for (sem, val, src) in toks:
            if src == "pe" and e == "pe":
                continue
            k = (e, id(sem))
            if self.waited.get(k, 0) >= val:
                continue
            self.eng[e].wait_ge(sem, val)
            self.waited[k] = val

    def _record(self, tok, r, w):
        for k in r:
            d = self.readers.setdefault(k, {})
            d[id(tok[0])] = tok
        for k in w:
            self.lastw[k] = tok
            self.readers[k] = {}

    def op(self, e, meth, *args, r=(), w=(), **kw):
        self._wait(e, self._deps(r, w))
        ins = getattr(self.eng[e], meth)(*args, **kw)
        if self.pcnt[e] >= 8000:
            self._new_psem(e)
        self.pcnt[e] += 1
        ins.then_inc(self.psem[e], 1)
        tok = (self.psem[e], self.pcnt[e], e)
        self._record(tok, r, w)
        self.ninst += 1
        return tok

    def dma(self, q, out, in_, r=(), w=(), key=None):
        self._wait(q, self._deps(r, w))
        key = key if key is not None else (w[0] if w else ("rd", r[0]))
        ent = self.dsem.get(key)
        if ent is None or ent[1] >= 30000:
            ent = [self._sem("d"), 0]
            self.dsem[key] = ent
        ins = self.eng[q].dma_start(out=out, in_=in_)
        ent[1] += 16
        ins.then_inc(ent[0], 16)
        tok = (ent[0], ent[1], "dma")
        self._record(tok, r, w)
        self.ninst += 1
        return tok

    def wait_all(self, e, keys):
        toks = []
        for k in keys:
            if k in self.lastw:
                toks.append(self.lastw[k])
            toks.extend(self.readers.get(k, {}).values())
        self._wait(e, toks)


def own_chunks(c):
    out = []
    for k in range(8):
        out.append(16 * k + c)
        out.append(16 * k + 15 - c)
    return out


def t5_bucket_np(dist):
    d = np.maximum(dist, 0)
    df = np.maximum(d, 1).astype(np.float32)
    large = 16 + (np.log(df / 16) / np.float32(np.log(1024 / 16)) * 16).astype(np.int32)
    large = np.minimum(large, 31)
    return np.where(d < 16, d, large)


def build(dbg=None):
    dbg = dbg or set()
    nc = bass.Bass("TRN2", target_bir_lowering=False)
    I, O = {}, {}

    def inp(name, shape, dt=F32):
        I[name] = nc.dram_tensor(name, list(shape), dt, kind="ExternalInput")
        return I[name]

    def outp(name, shape, dt=F32):
        O[name] = nc.dram_tensor(name, list(shape), dt, kind="ExternalOutput")
        return O[name]

    def scratch(name, shape, dt=BF16):
        return nc.dram_tensor(name, list(shape), dt)

    xT_all = inp("xT_all", [D, S])
    xT_own = inp("xT_own", [D, 2048])
    xT_kv2 = inp("xT_kv2", [D, 16, 640])
    hv_in = inp("hv", [128, 16, 5])
    w_A = inp("w_A", [D, 512])
    w_Q = inp("w_Q", [D, 1024])
    w_ga = inp("w_ga", [D, 1024])
    w_gb = inp("w_gb", [D, 1024])
    w_kv2 = inp("w_kv2", [D, 512])
    w_gn = inp("w_gn", [D, 24])
    gains = inp("gains", [128, 4, 8])
    indic = inp("indic", [63, S], BF16)
    w1c = [inp("w1k", [2048, 256]), inp("w1v", [2048, 256])]
    w2c = [inp("w2k", [256, 64]), inp("w2v", [256, 64])]
    posc_in = [inp("posk", [128, 16]), inp("posv", [128, 16])]
    nt_sel = inp("nt_sel", [2, 15, 2, 128, 512])
    nt_win = inp("nt_win", [5, 2, 128, 512])
    nt_swa = inp("nt_swa", [2, 2, 128, 512])
    b31_in = inp("b31row", [1, 2, 512])
    sinks_in = inp("sinks", [128, 8])
    atab_in = inp("atab", [128, 16, 310], BF16)
    trow_in = inp("trow", [128, 16, 128])
    cend_in = inp("cend", [128, 8])
    band_in = inp("band", [128, 8, 256], BF16)
    ident_in = inp("ident", [128, 128], BF16)
    identf_in = inp("identf", [128, 128])
    w_upA = inp("w_upA", [512, 1024])
    w_upB = inp("w_upB", [512, 1024])
    w_out = inp("w_out", [D, D])
    w_m1 = inp("w_m1", [D, 4096])
    w_m2 = inp("w_m2", [4096, D])

    kcnT = scratch("kcnT", [128, S])
    vcnT = scratch("vcnT", [128, S])
    qn_d = scratch("qn_d", [64, 16, 8, 128])
    qs_d = scratch("qs_d", [64, 16, 8, 128])
    gate_d = [scratch("ga_d", [128, 8, 2048]), scratch("gb_d", [128, 8, 2048])]
    kw_d = scratch("kw_d", [64, 16, 2, 640])
    vw_d = scratch("vw_d", [128, 16, 5, 2, 65])
    k2_d = scratch("k2_d", [64, 16, 2, 256])
    v2_d = scratch("v2_d", [128, 16, 2, 2, 65])
    oat_d = scratch("oat_d", [128, 4, 2048])
    obt_d = scratch("obt_d", [128, 4, 2048])
    ht_d = scratch("ht_d", [128, 8, 2048], F32)
    EB = 256

    out = outp("out", [D, NOWN * 128])
    dbo = {}

    def dbgout(name, shape, dt=F32):
        dbo[name] = outp(name, shape, dt)
        return dbo[name]

    es = contextlib.ExitStack()
    with es:
        sc = Sch(nc, es)

        uid = [0]

        def mk(stack):
            def sbx(name, shape, dt):
                uid[0] += 1
                return stack.enter_context(nc.sbuf_tensor(f"{name}_{uid[0]}", list(shape), dt))

            def psx(name, shape=(128, 512), dt=F32):
                uid[0] += 1
                return stack.enter_context(nc.psum_tensor(f"{name}_{uid[0]}", list(shape), dt))
            return sbx, psx

        sb, ps = mk(es)

        def barrier():
            toks = [(sc.psem[e], sc.pcnt[e], e) for e in sc.eng if sc.pcnt[e] > 0]
            toks += [(ent[0], ent[1], "dma") for ent in sc.dsem.values() if ent[1] > 0]
            for e in sc.eng:
                for (sem, val, src) in toks:
                    if src == e:
                        continue
                    k = (e, id(sem))
                    if sc.waited.get(k, 0) >= val:
                        continue
                    sc.eng[e].wait_ge(sem, val)
                    sc.waited[k] = val

        ones = sb("ones", [128, 128], BF16)
        gn = sb("gn", [128, 4, 8], F32)
        GS = sb("GS", [128, 16, 24], F32)
        sc.op("pool", "memset", ones[:], 1.0, w=["ones"])
        sc.dma("sp", gn[:], gains.ap(), w=["gn"])

        def norm_cols(xt_ap, xkey, sq_ap, sqkey, pstat, pkey, rs_ap, rskey, u_ap, ukey, gidx, n, split=True,
                      do_sq=True, do_rest=True):
            if do_sq:
                sc.op("act", "activation", sq_ap[:], xt_ap[:], AF.Square, r=[xkey], w=[sqkey])
            if not do_rest:
                return
            for c0 in range(0, n, 512):
                c1 = min(n, c0 + 512)
                for kc in range(8):
                    sc.op("pe", "matmul", pstat[:, 0:c1 - c0], ones[:], sq_ap[:, kc, c0:c1], start=(kc == 0), stop=(kc == 7),
                          r=[sqkey, "ones"], w=[pkey])
                sc.op("dve", "tensor_scalar", rs_ap[:, c0:c1], pstat[:, 0:c1 - c0], 1.0 / D, EPS, ALU.mult, ALU.add,
                      r=[pkey], w=[rskey])
            sc.op("act", "activation", rs_ap[:, 0:n], rs_ap[:, 0:n], AF.Sqrt, r=[rskey], w=[rskey])
            sc.op("dve", "reciprocal", rs_ap[:, 0:n], rs_ap[:, 0:n], r=[rskey], w=[rskey])
            if u_ap is not None:
                for (e, k0, k1) in ((("dve", 0, 5), ("pool", 5, 8)) if split else (("dve", 0, 8),)):
                    sc.op(e, "tensor_tensor", u_ap[:, k0:k1, :], xt_ap[:, k0:k1, :],
                          rs_ap[:, 0:n].unsqueeze(1).broadcast_to([128, k1 - k0, n]), ALU.mult,
                          r=[xkey, rskey], w=[ukey])

        def fold_gain(W, wkey, gidx):
            for kc in range(8):
                sc.op("dve", "tensor_scalar", W[:, kc, :], W[:, kc, :], gn[:, gidx, kc:kc + 1], None, ALU.mult,
                      r=[wkey, "gn"], w=[wkey])

        with contextlib.ExitStack() as esP:
            sbP, psP = mk(esP)
            KT = sbP("KT", [128, 2, S], BF16)
            VAf = sbP("VAf", [128, NCH * 130 + 64], BF16)
            VA = VAf[:, 0:NCH * 130].rearrange("p (t g e) -> p t g e", g=2, e=65)
            KC = sbP("KC", [128, 2, 1024], BF16)
            VCf = sbP("VCf", [128, 8 * 130 + 64], BF16)
            VC = VCf[:, 0:8 * 130].rearrange("p (t g e) -> p t g e", g=2, e=65)
            sc.op("pool", "memset", VAf[:], 0.0, w=["VA"])
            sc.op("pool", "memset", VA[:, :, :, 64:65], 1.0, w=["VA"])
            sc.op("pool", "memset", KC[:], 0.0, w=["KC"])
            sc.op("pool", "memset", VCf[:], 0.0, w=["VC"])
            sc.op("pool", "memset", VC[:, :, :, 64:65], 1.0, w=["VC"])
            for g in range(2):
                sc.dma("pool", KT[64:127, g, :], indic.ap(), w=[("KTc", g)])

            with contextlib.ExitStack() as esA:
                sbA, psA = mk(esA)
                WA = sbA("WA", [128, 8, 512], BF16)
                xt = [sbA(f"xt{i}", [128, 8, 512], F32) for i in range(3)]
                sq = [sbA(f"sq{i}", [128, 8, 512], BF16) for i in range(2)]
                uT = [sbA(f"uT{i}", [128, 8, 512], BF16) for i in range(2)]
                rs = [sbA(f"rs{i}", [128, 512], F32) for i in range(2)]
                stg = [sbA(f"stg{i}", [128, 2, 512], BF16) for i in range(2)]
                p_stat = [psA(f"p_stat{i}") for i in range(2)]
                p_kcn = psA("p_kcn")
                p_vcn = psA("p_vcn")
                p_ks = psA("p_ks")
                p_v = [psA(f"p_v{i}", [128, 4, 128]) for i in range(2)]

                sc.dma("pool", WA[:], w_A.ap().rearrange("(kc p) n -> p kc n", p=128), w=["WA"])
                fold_gain(WA, "WA", 0)
                xsrc = xT_all.ap().rearrange("(kc p) t -> p kc t", p=128)
                NB = 32 if "Ashort" not in dbg else 2
                kcn_v = kcnT.ap().rearrange("p (s n) -> p s n", s=16)
                vcn_v = vcnT.ap().rearrange("p (s n) -> p s n", s=16)

                def loadA(tb):
                    sc.dma("sp", xt[tb % 3][:], xsrc[:, :, tb * 512:(tb + 1) * 512], w=[("xt", tb % 3)])

                def sqA(tb):
                    s = tb % 2
                    norm_cols(xt[tb % 3], ("xt", tb % 3), sq[s], ("sq", s), p_stat[s], ("p_stat", s), rs[s], ("rs", s),
                              uT[s], ("uT", s), 0, 512, do_rest=False)

                def restA(tb):
                    s = tb % 2
                    norm_cols(xt[tb % 3], ("xt", tb % 3), sq[s], ("sq", s), p_stat[s], ("p_stat", s), rs[s], ("rs", s),
                              uT[s], ("uT", s), 0, 512, do_sq=False)

                def projA(tb):
                    s = tb % 2
                    cs = slice(tb * 512, (tb + 1) * 512)
                    for (pp, name, c0) in ((p_kcn, "p_kcn", 0), (p_vcn, "p_vcn", 128), (p_ks, "p_ks", 256)):
                        for kc in range(8):
                            sc.op("pe", "matmul", pp[:], WA[:, kc, c0:c0 + 128], uT[s][:, kc, :],
                                  start=(kc == 0), stop=(kc == 7), r=[("uT", s), "WA"], w=[name])
                    for t4 in range(4):
                        for kc in range(8):
                            sc.op("pe", "matmul", p_v[s][:, t4, :], uT[s][:, kc, t4 * 128:(t4 + 1) * 128],
                                  WA[:, kc, 384:512], start=(kc == 0), stop=(kc == 7),
                                  r=[("uT", s), "WA"], w=[("p_v", s)])
                    sc.op("act", "activation", stg[s][:, 0, :].rearrange("p (s n) -> p s n", s=16),
                          p_kcn[:].rearrange("p (n s) -> p s n", s=16), AF.Copy, r=["p_kcn"], w=[("stg", s)])
                    sc.op("act", "activation", stg[s][:, 1, :].rearrange("p (s n) -> p s n", s=16),
                          p_vcn[:].rearrange("p (n s) -> p s n", s=16), AF.Copy, r=["p_vcn"], w=[("stg", s)])
                    sc.dma("pool", kcn_v[:, :, tb * 32:(tb + 1) * 32], stg[s][:, 0, :].rearrange("p (s n) -> p s n", s=16),
                           r=[("stg", s)], w=["kcnT"])
                    sc.dma("pool", vcn_v[:, :, tb * 32:(tb + 1) * 32], stg[s][:, 1, :].rearrange("p (s n) -> p s n", s=16),
                           r=[("stg", s)], w=["vcnT"])
                    sc.op("dve", "tensor_copy", KT[0:64, 0, cs], p_ks[0:64, :], r=["p_ks"], w=[("KT", 0)])
                    sc.op("dve", "tensor_copy", KT[0:64, 1, cs], p_ks[64:128, :], r=["p_ks"], w=[("KT", 1)])
                    sc.op("dve", "tensor_copy", VA[:, tb * 4:(tb + 1) * 4, :, 0:64],
                          p_v[s][:].rearrange("p t (g d) -> p t g d", g=2), r=[("p_v", s)], w=["VA"])

                for tb in range(min(3, NB)):
                    loadA(tb)
                sqA(0)
                restA(0)
                if NB > 1:
                    sqA(1)
                for tb in range(NB):
                    if tb + 1 < NB:
                        restA(tb + 1)
                    if tb + 2 < NB:
                        sqA(tb + 2)
                    projA(tb)
                    if tb + 3 < NB:
                        loadA(tb + 3)
                barrier()

            with contextlib.ExitStack() as esB:
                sbB, psB = mk(esB)
                X2s = [sbB(f"X2{i}", [128, 16, 1024], BF16) for i in range(2)]
                W1 = [sbB(f"W1_{i}", [128, 16, 256], BF16) for i in range(2)]
                W2 = [sbB(f"W2_{i}", [128, 2, 64], BF16) for i in range(2)]
                posc = [sbB(f"posc{i}", [128, 16], BF16) for i in range(2)]
                cb = [sbB(f"cb{i}", [128, 2], F32) for i in range(2)]
                hbs = [sbB(f"hb{i}", [128, 512], F32) for i in range(2)]
                t1s = [sbB(f"t1{i}", [128, 512], F32) for i in range(2)]
                sgs = [sbB(f"sg{i}", [128, 512], F32) for i in range(2)]
                G = [sbB(f"G{i}", [128, 512], BF16) for i in range(2)]
                p_h = [psB(f"p_h{i}") for i in range(2)]
                p_c = psB("p_c", [128, 2])
                p_o = psB("p_o")
                p_vo = psB("p_vo", [128, 4, 64])
                for i in range(2):
                    sc.dma("pool", W1[i][:], w1c[i].ap().rearrange("(jp p) n -> p jp n", p=128), w=[("W1", i)])
                    sc.dma("pool", W2[i][:], w2c[i].ap().rearrange("(hc p) d -> p hc d", p=128), w=[("W2", i)])
                    sc.dma("pool", posc[i][:], posc_in[i].ap(), w=[("posc", i)])
                sc.op("pool", "memset", G[0][:], 0.0, w=[("G", 0)])
                sc.op("pool", "memset", G[1][:], 0.0, w=[("G", 1)])
                for i in range(2):
                    sc.op("pool", "memset", X2s[i][:], 0.0, w=[("X2", i)])
                def loadX2(kv_, g_):
                    xi_ = (kv_ * 2 + g_) % 2
                    src_ = kcnT if kv_ == 0 else vcnT
                    key_ = "kcnT" if kv_ == 0 else "vcnT"
                    srcv = src_.ap().rearrange("p (s n) -> p s n", s=16)[g_ * 64:(g_ + 1) * 64]
                    sc.dma("sp", X2s[xi_][0:64, :, :], srcv, r=[key_], w=[("X2", xi_)])
                    sc.dma("act", X2s[xi_][64:128, 0:15, :], srcv[:, 1:16, :], r=[key_], w=[("X2", xi_)])
                    sc.dma("act", X2s[xi_][64:128, 15, 0:1023], srcv[:, 0, 1:1024], r=[key_], w=[("X2", xi_)])

                for kv in range(2):
                    for hc in range(2):
                        for jp in range(16):
                            sc.op("pe", "matmul", p_c[:, hc:hc + 1], W1[kv][:, jp, hc * 128:(hc + 1) * 128],
                                  posc[kv][:, jp:jp + 1], start=(jp == 0), stop=(jp == 15),
                                  r=[("W1", kv), ("posc", kv)], w=["p_c"])
                    sc.op("dve", "tensor_copy", cb[kv][:], p_c[:], r=["p_c"], w=[("cb", kv)])
                    src = kcnT if kv == 0 else vcnT
                    srckey = "kcnT" if kv == 0 else "vcnT"
                    for g in range(2):
                        xi = (kv * 2 + g) % 2
                        X2 = X2s[xi]
                        if kv == 0 and g == 0:
                            loadX2(0, 0)
                        if kv * 2 + g + 1 < 4:
                            loadX2((kv * 2 + g + 1) // 2, (kv * 2 + g + 1) % 2)
                        for nb in range(2):
                            n0 = nb * 512
                            ncols = 512 if nb == 0 else 511
                            for hc in range(2):
                                for jp in range(16):
                                    if jp < 8:
                                        rhs = X2[:, 2 * jp, n0:n0 + ncols]
                                    else:
                                        rhs = X2[:, 2 * jp - 16, n0 + 1:n0 + 1 + ncols]
                                    sc.op("pe", "matmul", p_h[hc][:, 0:ncols], W1[kv][:, jp, hc * 128:(hc + 1) * 128], rhs,
                                          start=(jp == 0), stop=(jp == 15), r=[("W1", kv), ("X2", xi)], w=[("p_h", hc)])
                                hb, t1, sg = hbs[hc], t1s[hc], sgs[hc]
                                hbk, t1k, sgk = ("hb", hc), ("t1", hc), ("sg", hc)
                                sc.op("act", "activation", hb[:, 0:ncols], p_h[hc][:, 0:ncols], AF.Identity,
                                      bias=cb[kv][:, hc:hc + 1], r=[("p_h", hc), ("cb", kv)], w=[hbk])
                                sc.op("dve", "tensor_tensor", t1[:, 0:ncols], hb[:, 0:ncols], hb[:, 0:ncols], ALU.mult,
                                      r=[hbk], w=[t1k])
                                sc.op("dve", "tensor_scalar", t1[:, 0:ncols], t1[:, 0:ncols], 0.044715, 1.0, ALU.mult, ALU.add,
                                      r=[t1k], w=[t1k])
                                sc.op("dve", "tensor_tensor", t1[:, 0:ncols], t1[:, 0:ncols], hb[:, 0:ncols], ALU.mult,
                                      r=[t1k, hbk], w=[t1k])
                                sc.op("act", "activation", sg[:, 0:ncols], t1[:, 0:ncols], AF.Sigmoid, scale=1.5957691216,
                                      r=[t1k], w=[sgk])
                                sc.op("dve", "tensor_tensor", G[hc][:, 0:ncols], hb[:, 0:ncols], sg[:, 0:ncols], ALU.mult,
                                      r=[hbk, sgk], w=[("G", hc)])
                            if kv == 0:
                                for hc in range(2):
                                    sc.op("pe", "matmul", p_o[0:64, 0:ncols], W2[0][:, hc, :], G[hc][:, 0:ncols],
                                          start=(hc == 0), stop=(hc == 1), r=[("W2", 0), ("G", hc)], w=["p_o"])
                                sc.op("act", "activation", KC[0:64, g, n0:n0 + ncols], p_o[0:64, 0:ncols], AF.Copy,
                                      r=["p_o"], w=["KC"])
                            else:
                                for t4 in range(4):
                                    for hc in range(2):
                                        sc.op("pe", "matmul", p_vo[:, t4, :], G[hc][:, t4 * 128:(t4 + 1) * 128],
                                              W2[1][:, hc, :], start=(hc == 0), stop=(hc == 1),
                                              r=[("W2", 1), ("G", hc)], w=["p_vo"])
                                sc.op("dve", "tensor_copy", VC[:, nb * 4:(nb + 1) * 4, g, 0:64], p_vo[:],
                                      r=["p_vo"], w=["VC"])
                if "B" in dbg:
                    d_kc = dbgout("d_kc", [128, 2, 1024], BF16)
                    d_vc = dbgout("d_vc", [128, 8, 2, 65], BF16)
                    sc.dma("sp", d_kc.ap(), KC[:], r=["KC"], w=["d_kc"])
                    sc.dma("sp", d_vc.ap(), VC[:], r=["VC"], w=["d_vc"])
                barrier()

            with contextlib.ExitStack() as esC:
                sbC, psC = mk(esC)
                Wbs = [sbC(f"Wb{i}", [128, 8, 1024], BF16) for i in range(2)]
                Wb = Wbs[0]
                Wgn = sbC("Wgn", [128, 8, 24], BF16)
                uTo = sbC("uTo", [128, 8, 2048], BF16)
                xos = [sbC(f"xo{i}", [128, 8, 256], F32) for i in range(2)]
                sqo = sbC("sqo", [128, 8, 256], BF16)
                rso = sbC("rso", [128, 256], F32)
                qst1 = sbC("qst", [64, 4, 8, 128], BF16)
                qst = [qst1, qst1]
                gst = [sbC(f"gst{i}", [128, 512], BF16) for i in range(2)]
                p_st = psC("p_st")
                p_q = [psC(f"p_q{i}") for i in range(3)]
                p_gn = psC("p_gn", [128, 24])
                xosrc = xT_own.ap().rearrange("(kc p) t -> p kc t", p=128)
                sc.dma("pool", Wb[:], w_Q.ap().rearrange("(kc p) n -> p kc n", p=128), w=[("Wb", 0)])
                sc.dma("pool", Wgn[:], w_gn.ap().rearrange("(kc p) n -> p kc n", p=128), w=["Wgn"])
                fold_gain(Wb, ("Wb", 0), 0)
                fold_gain(Wgn, "Wgn", 0)
                sc.dma("pool", Wbs[1][:], w_ga.ap().rearrange("(kc p) n -> p kc n", p=128), w=[("Wb", 1)])
                fold_gain(Wbs[1], ("Wb", 1), 0)
                sc.dma("sp", xos[0][:], xosrc[:, :, 0:256], w=[("xo", 0)])
                for ob in range(8):
                    cs = slice(ob * 256, (ob + 1) * 256)
                    if ob + 1 < 8:
                        sc.dma("sp", xos[(ob + 1) % 2][:], xosrc[:, :, (ob + 1) * 256:(ob + 2) * 256], w=[("xo", (ob + 1) % 2)])
                    norm_cols(xos[ob % 2], ("xo", ob % 2), sqo, "sqo", p_st, "p_st", rso, "rso", uTo[:, :, cs],
                              ("uTo", ob // 2), 0, 256)
                cnt = 0
                for ob in range(4):
                    cs = slice(ob * 512, (ob + 1) * 512)
                    for h16 in range(16):
                        b = cnt % 3
                        cnt += 1
                        for kc in range(8):
                            sc.op("pe", "matmul", p_q[b][0:64, :], Wb[:, kc, h16 * 64:(h16 + 1) * 64], uTo[:, kc, cs],
                                  start=(kc == 0), stop=(kc == 7), r=[("Wb", 0), ("uTo", ob)], w=[("p_q", b)])
                        sc.op("dve", "tensor_scalar", qst[h16 // 8][:, :, h16 % 8, :],
                              p_q[b][0:64, :].rearrange("p (j q) -> p j q", j=4), SCALE, None, ALU.mult,
                              r=[("p_q", b)], w=["qst"])
                        if h16 == 7:
                            sc.dma("sp", qn_d.ap()[:, ob * 4:(ob + 1) * 4], qst[0][:], r=["qst"], w=["qn_d"])
                    sc.dma("sp", qs_d.ap()[:, ob * 4:(ob + 1) * 4], qst[1][:], r=["qst"], w=["qs_d"])
                for j in range(16):
                    for kc in range(8):
                        sc.op("pe", "matmul", p_gn[:], uTo[:, kc, j * 128:(j + 1) * 128], Wgn[:, kc, :],
                              start=(kc == 0), stop=(kc == 7), r=[("uTo", j // 4), "Wgn"], w=["p_gn"])
                    sc.op("act", "activation", GS[:, j, :], p_gn[:], AF.Sigmoid, r=["p_gn"], w=["GS"])
                cnt = 0
                for gi, wsrc in enumerate((w_ga, w_gb)):
                    Wb = Wbs[1 - gi]
                    wkey = ("Wb", 1 - gi)
                    if gi == 1:
                        sc.dma("pool", Wb[:], wsrc.ap().rearrange("(kc p) n -> p kc n", p=128), r=[], w=[wkey])
                        fold_gain(Wb, wkey, 0)
                    for ob in range(4):
                        cs = slice(ob * 512, (ob + 1) * 512)
                        for fc in range(8):
                            b = cnt % 3
                            s2 = cnt % 2
                            cnt += 1
                            for kc in range(8):
                                sc.op("pe", "matmul", p_q[b][:], Wb[:, kc, fc * 128:(fc + 1) * 128], uTo[:, kc, cs],
                                      start=(kc == 0), stop=(kc == 7), r=[wkey, ("uTo", ob)], w=[("p_q", b)])
                            sc.op("act", "activation", gst[s2][:], p_q[b][:], AF.Sigmoid, r=[("p_q", b)], w=[("gst", s2)])
                            sc.dma("act", gate_d[gi].ap()[:, fc, cs], gst[s2][:], r=[("gst", s2)], w=[("gate_d", gi)])
                if "C" in dbg:
                    d_gs = dbgout("d_gs", [128, 16, 24])
                    sc.dma("sp", d_gs.ap(), GS[:], r=["GS"], w=["d_gs"])
                    d_qn = dbgout("d_qn", [64, 16, 8, 128], BF16)
                    sc.dma("sp", d_qn.ap(), qn_d.ap(), r=["qn_d"], w=["d_qn"])
                    d_ga = dbgout("d_ga", [128, 8, 2048], BF16)
                    sc.dma("sp", d_ga.ap(), gate_d[0].ap(), r=[("gate_d", 0)], w=["d_ga"])
                barrier()

            with contextlib.ExitStack() as esC:
                sbC, psC = mk(esC)
                Wk = sbC("Wk", [128, 8, 512], BF16)
                hvs = sbC("hvs", [128, 16, 5], F32)
                xks = [sbC(f"xk{i}", [128, 8, 640], F32) for i in range(2)]
                sqk = sbC("sqk", [128, 8, 640], BF16)
                rsks = [sbC(f"rsk{i}", [128, 640], F32) for i in range(2)]
                uTk = [sbC(f"uTk{i}", [128, 8, 640], BF16) for i in range(2)]
                kst = [sbC(f"kst{i}", [64, 2, 640], BF16) for i in range(2)]
                vst = [sbC(f"vst{i}", [128, 5, 2, 65], BF16) for i in range(2)]
                k2st = [sbC(f"k2st{i}", [64, 2, 256], BF16) for i in range(2)]
                v2st = [sbC(f"v2st{i}", [128, 2, 2, 65], BF16) for i in range(2)]
                p_st = psC("p_st")
                p_kw = [psC(f"p_kw{i}") for i in range(2)]
                p_vw = [psC(f"p_vw{i}", [128, 4, 128]) for i in range(2)]
                p_k2 = psC("p_k2", [128, 256])
                p_v2 = psC("p_v2", [128, 2, 128])
                sc.dma("pool", Wk[:], w_kv2.ap().rearrange("(kc p) n -> p kc n", p=128), w=["Wk"])
                fold_gain(Wk, "Wk", 0)
                sc.dma("sp", hvs[:], hv_in.ap(), w=["hvs"])
                xksrc = xT_kv2.ap().rearrange("(kc p) j t -> p kc j t", p=128)
                def loadK(j):
                    sc.dma("sp", xks[j % 2][:], xksrc[:, :, j, :], w=[("xk", j % 2)])

                def normK(j):
                    s = j % 2
                    norm_cols(xks[s], ("xk", s), sqk, "sqk", p_st, "p_st", rsks[s], ("rsk", s), uTk[s], ("uTk", s), 0, 640)

                loadK(0)
                loadK(1)
                normK(0)
                for j in range(16):
                    s = j % 2
                    if j + 1 < 16:
                        normK(j + 1)
                    for (c0, c1, pi) in ((0, 512, 0), (512, 640, 1)):
                        for kc in range(8):
                            sc.op("pe", "matmul", p_kw[pi][:, 0:c1 - c0], Wk[:, kc, 0:128], uTk[s][:, kc, c0:c1],
                                  start=(kc == 0), stop=(kc == 7), r=["Wk", ("uTk", s)], w=[("p_kw", pi)])
                        sc.op("dve", "tensor_copy", kst[s][0:64, 0, c0:c1], p_kw[pi][0:64, 0:c1 - c0],
                              r=[("p_kw", pi)], w=[("kst", s)])
                        sc.op("dve", "tensor_copy", kst[s][0:64, 1, c0:c1], p_kw[pi][64:128, 0:c1 - c0],
                              r=[("p_kw", pi)], w=[("kst", s)])
                    for t5 in range(5):
                        pv = p_vw[0][:, t5, :] if t5 < 4 else p_vw[1][:, 0, :]
                        for kc in range(8):
                            sc.op("pe", "matmul", pv, uTk[s][:, kc, t5 * 128:(t5 + 1) * 128], Wk[:, kc, 128:256],
                                  start=(kc == 0), stop=(kc == 7), r=["Wk", ("uTk", s)], w=[("p_vw", 0 if t5 < 4 else 1)])
                    sc.op("dve", "tensor_copy", vst[s][:, 0:4, :, 0:64], p_vw[0][:].rearrange("p t (g d) -> p t g d", g=2),
                          r=[("p_vw", 0)], w=[("vst", s)])
                    sc.op("dve", "tensor_copy", vst[s][:, 4, :, 0:64], p_vw[1][:, 0, :].rearrange("p (g d) -> p g d", g=2),
                          r=[("p_vw", 1)], w=[("vst", s)])
                    sc.op("dve", "tensor_copy", vst[s][:, :, :, 64],
                          hvs[:, j, :].unsqueeze(2).broadcast_to([128, 5, 2]), r=["hvs"], w=[("vst", s)])
                    for kc in range(8):
                        sc.op("pe", "matmul", p_k2[:], Wk[:, kc, 256:384], uTk[s][:, kc, 384:640],
                              start=(kc == 0), stop=(kc == 7), r=["Wk", ("uTk", s)], w=["p_k2"])
                    sc.op("dve", "tensor_copy", k2st[s][0:64, 0, :], p_k2[0:64, :], r=["p_k2"], w=[("k2st", s)])
                    sc.op("dve", "tensor_copy", k2st[s][0:64, 1, :], p_k2[64:128, :], r=["p_k2"], w=[("k2st", s)])
                    for t2 in range(2):
                        for kc in range(8):
                            sc.op("pe", "matmul", p_v2[:, t2, :], uTk[s][:, kc, (3 + t2) * 128:(4 + t2) * 128],
                                  Wk[:, kc, 384:512], start=(kc == 0), stop=(kc == 7), r=["Wk", ("uTk", s)], w=["p_v2"])
                    sc.op("dve", "tensor_copy", v2st[s][:, :, :, 0:64], p_v2[:].rearrange("p t (g d) -> p t g d", g=2),
                          r=["p_v2"], w=[("v2st", s)])
                    sc.op("dve", "tensor_copy", v2st[s][:, :, :, 64],
                          hvs[:, j, 3:5].unsqueeze(2).broadcast_to([128, 2, 2]), r=["hvs"], w=[("v2st", s)])
                    sc.dma("pool", kw_d.ap()[:, j], kst[s][:], r=[("kst", s)], w=["kw_d"])
                    sc.dma("pool", vw_d.ap()[:, j], vst[s][:], r=[("vst", s)], w=["vw_d"])
                    sc.dma("pool", k2_d.ap()[:, j], k2st[s][:], r=[("k2st", s)], w=["k2_d"])
                    sc.dma("pool", v2_d.ap()[:, j], v2st[s][:], r=[("v2st", s)], w=["v2_d"])
                    if j + 2 < 16:
                        loadK(j + 2)
                if "C" in dbg:
                    d_kw = dbgout("d_kw", [64, 16, 2, 640], BF16)
                    sc.dma("sp", d_kw.ap(), kw_d.ap(), r=["kw_d"], w=["d_kw"])
                    d_vw = dbgout("d_vw", [128, 16, 5, 2, 65], BF16)
                    sc.dma("sp", d_vw.ap(), vw_d.ap(), r=["vw_d"], w=["d_vw"])
                barrier()

            with contextlib.ExitStack() as esD:
                sbD, psD = mk(esD)
                QA = [[sbD(f"QA{p}{g}", [128, 7, 512], BF16) for g in range(2)] for p in range(2)]
                QS = [sbD(f"QS{p}", [64, 2, 512], BF16) for p in range(2)]
                KWj = [sbD(f"KWj{p}", [64, 2, 640], BF16) for p in range(2)]
                VWj = [sbD(f"VWj{p}", [128, 5 * 130 + 64], BF16) for p in range(2)]
                K2j = [sbD(f"K2j{p}", [64, 2, 256], BF16) for p in range(2)]
                V2j = [sbD(f"V2j{p}", [128, 2 * 130 + 64], BF16) for p in range(2)]
                atj = [sbD(f"atj{p}", [128, 310], BF16) for p in range(2)]
                trj = [sbD(f"trj{p}", [128, 128], F32) for p in range(2)]
                cend = sbD("cend", [128, 8], F32)
                band = sbD("band", [128, 8, 256], BF16)
                ident = sbD("ident", [128, 128], BF16)
                identf = sbD("identf", [128, 128], F32)
                esink = sbD("esink", [128, 8], F32)
                b31s = sbD("b31s", [1, 2, 512], F32)
                b31b = sbD("b31b", [1, 2, 512], BF16)
                cm = [sbD(f"cm{p}", [128, 8, 128], BF16) for p in range(2)]
                PT = [sbD(f"PT{i}", [128, 1024], BF16) for i in range(3)]
                NT = [sbD(f"NT{i}", [128, 512], BF16) for i in range(6)]
                score = sbD("score", [128, 310], F32)
                work = sbD("work", [128, 310], F32)
                work2 = sbD("work2", [128, 310], F32)
                top = sbD("top", [128, 16], F32)
                thr = sbD("thr", [128, 1], F32)
                rs4 = sbD("rs4", [128, 4], F32)
                fac = sbD("fac", [128, 4], F32)
                Mtok = [[sbD(f"Mtok{p}{g}", [128, 5, 128], BF16) for g in range(2)] for p in range(2)]
                rs4s = sbD("rs4s", [128, 4], F32)
                OT = [sbD(f"OT{i}", [128, 512], BF16) for i in range(10)]
                oacc = [[sbD(f"oa{p}", [128, 2, 4, 64], F32), sbD(f"ob{p}", [128, 2, 4, 64], F32)] for p in range(2)]
                oab = [sbD(f"oab{i}", [128, 512], BF16) for i in range(2)]
                oTs = [sbD(f"oTs{i}", [128, 4, 128], BF16) for i in range(2)]
                psS = [psD(f"psS{i}", [128, 2, 512]) for i in range(2)]
                psO = [psD("psO0")]
                psN = psD("psN")
                psM = [psD(f"psM{i}") for i in range(2)]

                sc.dma("sp", cend[:], cend_in.ap(), w=["cend"])
                sc.dma("sp", band[:], band_in.ap(), w=["band"])
                sc.dma("sp", ident[:], ident_in.ap(), w=["ident"])
                sc.dma("sp", identf[:], identf_in.ap(), w=["identf"])
                sc.dma("sp", esink[:], sinks_in.ap(), w=["esink"])
                sc.op("act", "activation", esink[:], esink[:], AF.Exp, r=["esink"], w=["esink"])
                sc.dma("sp", b31s[:], b31_in.ap(), w=["b31s"])
                sc.op("dve", "tensor_copy", b31b[:], b31s[:], r=["b31s"], w=["b31b"])
                sc.op("pool", "memset", score[:], 0.0, w=["score"])
                for p in range(2):
                    for g in range(2):
                        sc.op("pool", "memset", QA[p][g][:], 0.0, w=[("QA", p, g)])
                        sc.op("pool", "memset", Mtok[p][g][:], 0.0, w=[("Mtok", p, g)])
                        for slot in range(5):
                            sc.dma("sp", QA[p][g][126:127, slot, :], b31b[0:1, g, :], r=["b31b"], w=[("QA", p, g)])

                for p in range(2):
                    sc.op("pool", "memset", VWj[p][:], 0.0, w=[("VWj", p)])
                    sc.op("pool", "memset", V2j[p][:], 0.0, w=[("V2j", p)])
                rot = {"S": 0, "O": 0, "PT": 0, "tmp": 0, "NT": 0, "OT": 0, "M": 0}

                def nxt(name, n):
                    v = rot[name]
                    rot[name] = (v + 1) % n
                    return v

                def chunk_info(j):
                    k, typ = j // 2, j % 2
                    Bsel = 16 * k + (8 if typ == 0 else 16)
                    base = 16 * k - 7 if typ == 0 else 16 * k + 1
                    near = {kt: kt - base for kt in range(max(base, 0), min(base + 15, Bsel))}
                    nseg = (2 * Bsel + 61) // 62
                    nsegs = sorted(set((2 * kt) // 62 for kt in near))
                    far_segs = sorted(set((2 * kt) // 62 for kt in range(Bsel) if kt not in near))
                    return k, typ, Bsel, near, nseg, nsegs, far_segs

                def load_misc(j):
                    p = j % 2
                    sc.dma("sp", atj[p][:], atab_in.ap()[:, j, :], w=[("atj", p)])
                    sc.dma("sp", trj[p][:], trow_in.ap()[:, j, :], w=[("trj", p)])
                    sc.dma("sp", KWj[p][:], kw_d.ap()[:, j], r=["kw_d"], w=[("KWj", p)])
                    sc.dma("sp", VWj[p][:, 0:650], vw_d.ap()[:, j].rearrange("p t g e -> p (t g e)"), r=["vw_d"], w=[("VWj", p)])
                    sc.dma("sp", K2j[p][:], k2_d.ap()[:, j], r=["k2_d"], w=[("K2j", p)])
                    sc.dma("sp", V2j[p][:, 0:260], v2_d.ap()[:, j].rearrange("p t g e -> p (t g e)"), r=["v2_d"], w=[("V2j", p)])
                    for g in range(2):
                        sc.dma("sp", QS[p][:, g, :].rearrange("p (r q) -> p r q", r=4), qs_d.ap()[:, j, 4 * g:4 * g + 4, :],
                               r=["qs_d"], w=[("QS", p)])

                def load_QA(j, g):
                    p = j % 2
                    k, typ, Bsel, near, nseg, nsegs, far_segs = chunk_info(j)
                    slots = list(far_segs) + [5 + (sg_ - nsegs[0]) for sg_ in nsegs]
                    for slot in slots:
                        sc.dma("sp", QA[p][g][0:64, slot, :].rearrange("p (r q) -> p r q", r=4),
                               qn_d.ap()[:, j, 4 * g:4 * g + 4, :], r=["qn_d"], w=[("QA", p, g)])

                def load_chunk(j):
                    load_misc(j)
                    load_QA(j, 0)
                    load_QA(j, 1)

                def evac(bO):
                    o = nxt("OT", 10)
                    sc.op("act", "activation", OT[o][0:65, :], psO[bO][0:65, :], AF.Copy, r=[("psO", bO)], w=[("OT", o)])
                    return o

                def normalize(j, g, o, kind):
                    trv = psN[:].bitcast(BF16)[:, 0:264].rearrange("p (h e) -> p h e", h=4)
                    for h in range(4):
                        sc.op("pe", "transpose", trv[:, h, 0:65], OT[o][0:65, h * 128:(h + 1) * 128], ident[0:65, 0:65],
                              r=[("OT", o), "ident"], w=["psN"])
                    if kind == 3:
                        sc.op("dve", "tensor_tensor", rs4[:], trv[:, :, 64], esink[:, 4 * g:4 * g + 4], ALU.add,
                              r=["psN", "esink"], w=["rs4"])
                    else:
                        sc.op("dve", "tensor_scalar", rs4[:], trv[:, :, 64], 1e-30, None, ALU.max, r=["psN"], w=["rs4"])
                    sc.op("dve", "reciprocal", fac[:], rs4[:], r=["rs4"], w=["fac"])
                    if kind != 3:
                        gv = GS[:, j, :].rearrange("p (g r b) -> p g r b", g=2, r=4)[:, g, :, kind]
                        sc.op("dve", "tensor_tensor", fac[:], fac[:], gv, ALU.mult, r=["fac", "GS"], w=["fac"])
                    ab = 1 if kind == 3 else 0
                    acc = oacc[j % 2][ab]
                    akey = ("oacc", j % 2, ab)
                    for h in range(4):
                        if kind in (0, 3):
                            sc.op("dve", "tensor_scalar", acc[:, g, h, :], trv[:, h, 0:64], fac[:, h:h + 1], None, ALU.mult,
                                  r=["psN", "fac"], w=[akey])
                        else:
                            sc.op("dve", "scalar_tensor_tensor", acc[:, g, h, :], trv[:, h, 0:64], fac[:, h:h + 1],
                                  acc[:, g, h, :], ALU.mult, ALU.add, r=["psN", "fac", akey], w=[akey])

                def attn_tiles(p, g, tiles, qrhs, qkeys):
                    bO = 0
                    n = len(tiles)
                    npair = (n + 1) // 2
                    state = {}
                    ntbuf = {}
                    LOOK = 4

                    def issue_nt(i):
                        t = tiles[i]
                        if t["nt"] is not None:
                            nti = nxt("NT", 6)
                            sc.dma("pool", NT[nti][:], t["nt"], w=[("NT", nti)])
                            ntbuf[i] = nti

                    def issue_S(pi):
                        b = nxt("S", 2)
                        for h2 in range(2):
                            i = 2 * pi + h2
                            if i >= n:
                                break
                            t = tiles[i]
                            nti = ntbuf.get(i)
                            sc.op("pe", "matmul", psS[b][:, h2, :], t["lhsT"], qrhs(t), start=True, stop=(nti is None),
                                  r=t["lkeys"] + qkeys, w=[("psS", b, h2)])
                            if nti is not None:
                                sc.op("pe", "matmul", psS[b][:, h2, :], ident[:], NT[nti][:], start=False, stop=True,
                                      r=["ident", ("NT", nti)], w=[("psS", b, h2)])
                        state[pi] = b

                    def issue_rest(pi):
                        b = state[pi]
                        w2 = min(2, n - 2 * pi)
                        pt = nxt("PT", 3)
                        sc.op("act", "activation", PT[pt][:, 0:w2 * 512], psS[b][:, 0:w2, :].rearrange("p a c -> p (a c)"),
                              AF.Exp, r=[("psS", b, h2) for h2 in range(w2)], w=[("PT", pt)])
                        for h2 in range(w2):
                            t = tiles[2 * pi + h2]
                            if t.get("post") is not None:
                                t["post"](PT[pt][:, h2 * 512:(h2 + 1) * 512], ("PT", pt))
                        for h2 in range(w2):
                            i = 2 * pi + h2
                            t = tiles[i]
                            sc.op("pe", "matmul", psO[bO][:], t["v"], PT[pt][:, h2 * 512:(h2 + 1) * 512],
                                  start=(i == 0), stop=(i == n - 1), r=t["vkeys"] + [("PT", pt)], w=[("psO", bO)])
                        for h2 in range(w2):
                            i = 2 * pi + h2
                            t = tiles[i]
                            if t.get("extra") is not None:
                                t["extra"](PT[pt][:, h2 * 512:(h2 + 1) * 512], ("PT", pt), i, n)

                    for i in range(min(LOOK, n)):
                        issue_nt(i)
                    issue_S(0)
                    for pi in range(npair):
                        for i in range(2 * pi + LOOK, min(2 * pi + LOOK + 2, n)):
                            issue_nt(i)
                        if pi + 1 < npair:
                            issue_S(pi + 1)
                        issue_rest(pi)
                        between()
                    return bO

                pend = []
                tick = [0]

                drain_on = [False]

                def between():
                    tick[0] += 1
                    if drain_on[0] and tick[0] % 3 == 0 and pend:
                        normalize(*pend.pop(0))

                def drain():
                    while pend:
                        normalize(*pend.pop(0))

                def cmp_sel(j, g):
                    p = j % 2
                    k, typ, Bsel, near, nseg, nsegs, far_segs = chunk_info(j)
                    NCT = k + 1
                    slotq = far_segs[0] if far_segs else 5
                    tiles = []
                    for ct in range(NCT):
                        def post(pta, ptk, ct=ct):
                            sc.op("dve", "tensor_tensor", pta.rearrange("p (h q) -> p h q", h=4),
                                  pta.rearrange("p (h q) -> p h q", h=4),
                                  cm[p][:, ct, :].unsqueeze(1).broadcast_to([128, 4, 128]), ALU.mult,
                                  r=[ptk, ("cm", p)], w=[ptk])

                        def extra(pta, ptk, i, n, ct=ct):
                            for h in range(4):
                                dst = psM[h // 2][:].rearrange("p (a c) -> p a c", a=2)[:, h % 2, :]
                                sc.op("pe", "matmul", dst, pta[:, h * 128:(h + 1) * 128], band[:, ct, :],
                                      start=(i == 0 and h % 2 == 0), stop=(i == n - 1 and h % 2 == 1), r=[ptk, "band"],
                                      w=[("psM", h // 2)])
                        tiles.append(dict(lhsT=KC[0:64, g, ct * 128:(ct + 1) * 128], lkeys=["KC"], nt=None,
                                          v=VCf[:, (ct * 2 + g) * 65:(ct * 2 + g) * 65 + 128], vkeys=["VC"],
                                          post=post, extra=extra))
                    bO = attn_tiles(p, g, tiles, lambda t: QA[p][g][0:64, slotq, :], [("QA", p, g)])
                    pend.append((j, g, evac(bO), 0))
                    for i2 in range(2):
                        sc.op("dve", "tensor_scalar", rs4s[:, 2 * i2:2 * i2 + 2],
                              psM[i2][:].rearrange("p (a c) -> p a c", a=2)[:, :, 0], 1e-30, None, ALU.max,
                              r=[("psM", i2)], w=["rs4s"])
                    sc.op("dve", "reciprocal", rs4s[:], rs4s[:], r=["rs4s"], w=["rs4s"])
                    for h in range(4):
                        src = psM[h // 2][:, (h % 2) * 256:(h % 2) * 256 + 256]
                        if h == 0:
                            sc.op("dve", "tensor_scalar", score[:, 0:256], src, rs4s[:, 0:1], None, ALU.mult,
                                  r=[("psM", 0), "rs4s"], w=["score"])
                        else:
                            sc.op("dve", "scalar_tensor_tensor", score[:, 0:256], src, rs4s[:, h:h + 1], score[:, 0:256],
                                  ALU.mult, ALU.add, r=[("psM", h // 2), "rs4s", "score"], w=["score"])
                    sc.op("dve", "tensor_tensor", work[:], score[:], atj[p][:], ALU.add, r=["score", ("atj", p)], w=["work"])
                    sc.op("dve", "max", top[:, 0:8], work[:], r=["work"], w=["top"])
                    sc.op("dve", "match_replace", work2[:], top[:, 0:8], work[:], -1e9, r=["top", "work"], w=["work2"])
                    sc.op("dve", "max", top[:, 8:16], work2[:], r=["work2"], w=["top"])
                    sc.op("dve", "tensor_scalar", thr[:], top[:, 15:16], -4096.0, None, ALU.max, r=["top"], w=["thr"])
                    sc.op("dve", "tensor_scalar", Mtok[p][g][:, :, 64:126], work[:].rearrange("p (m c) -> p m c", m=5),
                          thr[:, 0:1], NEG, ALU.is_lt, ALU.mult, r=["work", "thr"], w=[("Mtok", p, g)])

                def make_cm(j):
                    p = j % 2
                    for ct in range(j // 2 + 1):
                        sc.op("dve", "tensor_scalar", cm[p][:, ct, :], trj[p][:], cend[:, ct:ct + 1], None, ALU.is_ge,
                              r=[("trj", p), "cend"], w=[("cm", p)])

                def drain_upto(jmax):
                    keep = []
                    while pend:
                        e = pend.pop(0)
                        if e[0] <= jmax:
                            normalize(*e)
                        else:
                            keep.append(e)
                    pend.extend(keep)

                def trbqa(j):
                    p = j % 2
                    k, typ, Bsel, near, nseg, nsegs, far_segs = chunk_info(j)
                    for g in range(2):
                        trb = psM[g][:].bitcast(BF16).rearrange("p (m c) -> p m c", m=8)
                        for m in range(nseg):
                            sc.op("pe", "transpose", trb[:, m, :], Mtok[p][g][:, m, :], ident[:],
                                  r=[("Mtok", p, g), "ident"], w=[("psM", g)])
                        if far_segs:
                            f0, f1 = far_segs[0], far_segs[-1] + 1
                            sc.op("dve", "tensor_copy",
                                  QA[p][g][64:126, f0:f1, :].rearrange("p s (h q) -> p s h q", h=4),
                                  trb[64:126, f0:f1, :].unsqueeze(2).broadcast_to([62, f1 - f0, 4, 128]),
                                  r=[("psM", g)], w=[("QA", p, g)])
                        if nsegs:
                            n0_, n1_ = nsegs[0], nsegs[-1] + 1
                            sc.op("dve", "tensor_copy",
                                  QA[p][g][64:126, 5:5 + n1_ - n0_, :].rearrange("p s (h q) -> p s h q", h=4),
                                  trb[64:126, n0_:n1_, :].unsqueeze(2).broadcast_to([62, n1_ - n0_, 4, 128]),
                                  r=[("psM", g)], w=[("QA", p, g)])

                def winswa(j, g):
                    p = j % 2
                    k, typ, Bsel, near, nseg, nsegs, far_segs = chunk_info(j)
                    slotq = far_segs[0] if far_segs else 5
                    tiles = []
                    for i5 in range(5):
                        tiles.append(dict(lhsT=KWj[p][0:64, g, i5 * 128:(i5 + 1) * 128], lkeys=[("KWj", p)],
                                          nt=nt_win.ap()[i5, g], v=VWj[p][:, (i5 * 2 + g) * 65:(i5 * 2 + g) * 65 + 128],
                                          vkeys=[("VWj", p)]))
                    bO = attn_tiles(p, g, tiles, lambda t: QA[p][g][0:64, slotq, :], [("QA", p, g)])
                    pend.append((j, g, evac(bO), 2))
                    tiles = []
                    for i2 in range(2):
                        tiles.append(dict(lhsT=K2j[p][0:64, g, i2 * 128:(i2 + 1) * 128], lkeys=[("K2j", p)],
                                          nt=nt_swa.ap()[i2, g], v=V2j[p][:, (i2 * 2 + g) * 65:(i2 * 2 + g) * 65 + 128],
                                          vkeys=[("V2j", p)]))
                    bO = attn_tiles(p, g, tiles, lambda t: QS[p][0:64, g, :], [("QS", p)])
                    pend.append((j, g, evac(bO), 3))

                def selloop(j, g):
                    p = j % 2
                    k, typ, Bsel, near, nseg, nsegs, far_segs = chunk_info(j)
                    tiles = []
                    for kt in range(Bsel):
                        sg_ = (2 * kt) // 62
                        if kt in near:
                            slot = 5 + (sg_ - nsegs[0])
                            nt = nt_sel.ap()[typ, near[kt], g]
                        else:
                            slot = sg_
                            nt = None
                        tiles.append(dict(lhsT=KT[0:127, g, kt * 128:(kt + 1) * 128], lkeys=[("KT", g), ("KTc", g)],
                                          nt=nt, v=VAf[:, (kt * 2 + g) * 65:(kt * 2 + g) * 65 + 128], vkeys=["VA"], slot=slot))
                    bO = attn_tiles(p, g, tiles, lambda t: QA[p][g][0:127, t["slot"], :], [("QA", p, g)])
                    pend.append((j, g, evac(bO), 1))

                def final_copy(j):
                    for ab in range(2):
                        sc.op("dve", "tensor_copy", oab[ab][:], oacc[j % 2][ab][:].rearrange("p g h d -> p (g h d)"),
                              r=[("oacc", j % 2, ab)], w=[("oab", ab)])

                def final_T(j):
                    for ab in range(2):
                        trb = psN[:].bitcast(BF16).rearrange("p (m c) -> p m c", m=8)
                        for c4 in range(4):
                            sc.op("pe", "transpose", trb[:, c4, :], oab[ab][:, c4 * 128:(c4 + 1) * 128], ident[:],
                                  r=[("oab", ab), "ident"], w=["psN"])
                        sc.op("dve", "tensor_copy", oTs[ab][:], trb[:, 0:4, :], r=["psN"], w=[("oTs", ab)])
                        dst = (oat_d if ab == 0 else obt_d).ap()[:, :, j * 128:(j + 1) * 128]
                        sc.dma("sp", dst, oTs[ab][:], r=[("oTs", ab)], w=[("o_d", ab)])

                jlist = list(range(16)) if "Dshort" not in dbg else [0, 1]
                NJ = len(jlist)
                load_chunk(0)
                make_cm(0)
                cmp_sel(0, 0)
                cmp_sel(0, 1)
                trbqa(0)
                if NJ > 1:
                    load_chunk(1)
                    make_cm(1)
                for j in jlist:
                    if j + 1 < NJ:
                        cmp_sel(j + 1, 0)
                    winswa(j, 0)
                    if j + 1 < NJ:
                        cmp_sel(j + 1, 1)
                    winswa(j, 1)
                    if j + 2 < NJ:
                        load_misc(j + 2)
                    drain_on[0] = True
                    selloop(j, 0)
                    if j + 2 < NJ:
                        load_QA(j + 2, 0)
                        make_cm(j + 2)
                    selloop(j, 1)
                    drain_on[0] = False
                    if j + 2 < NJ:
                        load_QA(j + 2, 1)
                    drain()
                    final_copy(j)
                    final_T(j)
                    if j + 1 < NJ:
                        trbqa(j + 1)
                if "D" in dbg:
                    d_oa = dbgout("d_oa", [128, 4, 2048], BF16)
                    sc.dma("sp", d_oa.ap(), oat_d.ap(), r=[("o_d", 0)], w=["d_oa"])
                    d_ob = dbgout("d_ob", [128, 4, 2048], BF16)
                    sc.dma("sp", d_ob.ap(), obt_d.ap(), r=[("o_d", 1)], w=["d_ob"])
                barrier()

        with contextlib.ExitStack() as esE:
            sbE, psE = mk(esE)
            W1m = sbE("W1m", [128, 8, 4096], BF16)
            with contextlib.ExitStack() as esE1:
                sbE1, psE1 = mk(esE1)
                WuA = sbE1("WuA", [128, 4, 1024], BF16)
                WuB = sbE1("WuB", [128, 4, 1024], BF16)
                Wo = sbE1("Wo", [128, 8, 1024], BF16)
                oaT = [sbE1(f"oaT{i}", [128, 4, EB], BF16) for i in range(2)]
                obT = [sbE1(f"obT{i}", [128, 4, EB], BF16) for i in range(2)]
                GA = [sbE1(f"GA{i}", [128, 8, EB], BF16) for i in range(2)]
                GB = [sbE1(f"GB{i}", [128, 8, EB], BF16) for i in range(2)]
                xo = [sbE1(f"xoE{i}", [128, 8, EB], F32) for i in range(3)]
                t1 = [sbE1(f"t1E{i}", [128, EB], F32) for i in range(2)]
                t2 = [sbE1(f"t2E{i}", [128, EB], F32) for i in range(2)]
                t3 = [sbE1(f"t3E{i}", [128, EB], F32) for i in range(2)]
                yTs = [sbE1(f"yT{i}", [128, 8, EB], BF16) for i in range(2)]
                hst = sbE1("hst", [128, 8, EB], F32)
                zTs = [sbE1(f"zT{i}", [128, 8, EB], F32) for i in range(2)]
                zsq = sbE1("zsq", [128, 8, EB], BF16)
                rsE = sbE1("rsE", [128, EB], F32)
                pA = [psE1(f"pA{i}") for i in range(2)]
                pB = [psE1(f"pB{i}") for i in range(2)]
                pZ = [psE1(f"pZ{i}") for i in range(2)]
                pSt = psE1("pSt")
                sc.dma("pool", WuA[:], w_upA.ap().rearrange("(c p) n -> p c n", p=128), w=["WuA"])
                sc.dma("pool", WuB[:], w_upB.ap().rearrange("(c p) n -> p c n", p=128), w=["WuB"])
                sc.dma("pool", Wo[:], w_out.ap().rearrange("(c p) n -> p c n", p=128), w=["Wo"])
                for q4 in range(4):
                    sc.dma("pool", W1m[:, :, q4 * 1024:(q4 + 1) * 1024],
                           w_m1.ap()[:, q4 * 1024:(q4 + 1) * 1024].rearrange("(c p) n -> p c n", p=128), w=["W1m"])
                xosrc = xT_own.ap().rearrange("(kc p) t -> p kc t", p=128)
                def loadE(eb):
                    s = eb % 2
                    cs = slice(eb * EB, (eb + 1) * EB)
                    sc.dma("sp", oaT[s][:], oat_d.ap()[:, :, cs], r=[("o_d", 0)], w=[("oaT", s)])
                    sc.dma("sp", obT[s][:], obt_d.ap()[:, :, cs], r=[("o_d", 1)], w=[("obT", s)])
                    sc.dma("sp", GA[s][:], gate_d[0].ap()[:, :, cs], r=[("gate_d", 0)], w=[("GA", s)])
                    sc.dma("sp", GB[s][:], gate_d[1].ap()[:, :, cs], r=[("gate_d", 1)], w=[("GB", s)])
                    sc.dma("sp", xo[eb % 3][:], xosrc[:, :, cs], w=[("xoE", eb % 3)])

                def upE(eb):
                    s = eb % 2
                    yT = yTs[s]
                    for fc in range(8):
                        b = fc % 2
                        for c4 in range(4):
                            sc.op("pe", "matmul", pA[b][:, 0:EB], WuA[:, c4, fc * 128:(fc + 1) * 128], oaT[s][:, c4, :],
                                  start=(c4 == 0), stop=(c4 == 3), r=["WuA", ("oaT", s)], w=[("pA", b)])
                        for c4 in range(4):
                            sc.op("pe", "matmul", pB[b][:, 0:EB], WuB[:, c4, fc * 128:(fc + 1) * 128], obT[s][:, c4, :],
                                  start=(c4 == 0), stop=(c4 == 3), r=["WuB", ("obT", s)], w=[("pB", b)])
                        sc.op("dve", "tensor_tensor", t1[b][:], pA[b][:, 0:EB], GA[s][:, fc, :], ALU.mult,
                              r=[("pA", b), ("GA", s)], w=[("t1E", b)])
                        sc.op("dve", "tensor_tensor", t2[b][:], pB[b][:, 0:EB], GB[s][:, fc, :], ALU.mult,
                              r=[("pB", b), ("GB", s)], w=[("t2E", b)])
                        sc.op("dve", "tensor_tensor", yT[:, fc, :], t1[b][:], t2[b][:], ALU.add,
                              r=[("t1E", b), ("t2E", b)], w=[("yT", s)])

                def outmmE(eb):
                    s = eb % 2
                    yT = yTs[s]
                    zT = zTs[s]
                    for fo in range(8):
                        b = fo % 2
                        for fc in range(8):
                            sc.op("pe", "matmul", pZ[b][:, 0:EB], Wo[:, fc, fo * 128:(fo + 1) * 128], yT[:, fc, :],
                                  start=(fc == 0), stop=(fc == 7), r=["Wo", ("yT", s)], w=[("pZ", b)])
                        sc.op("act", "activation", zT[:, fo, :], pZ[b][:, 0:EB], AF.Copy, r=[("pZ", b)], w=[("zT", s)])

                def finE(eb):
                    s = eb % 2
                    zT = zTs[s]
                    cs = slice(eb * EB, (eb + 1) * EB)
                    norm_cols(zT, ("zT", s), zsq, "zsq", pSt, "pSt", rsE, "rsE", None, None, 1, EB)
                    for fo in range(8):
                        b = fo % 2
                        sc.op("dve", "scalar_tensor_tensor", t3[b][:], zT[:, fo, :], gn[:, 1, fo:fo + 1], rsE[:],
                              ALU.mult, ALU.mult, r=[("zT", s), "rsE", "gn"], w=[("t3E", b)])
                        sc.op("pool", "tensor_tensor", hst[:, fo, :], t3[b][:], xo[eb % 3][:, fo, :], ALU.add,
                              r=[("t3E", b), ("xoE", eb % 3)], w=["hst"])
                    sc.dma("act", ht_d.ap()[:, :, cs], hst[:], r=["hst"], w=["ht_d"])

                NEB = 2048 // EB
                loadE(0)
                loadE(1)
                upE(0)
                for eb in range(NEB):
                    if eb + 1 < NEB:
                        upE(eb + 1)
                    outmmE(eb)
                    if eb >= 1:
                        finE(eb - 1)
                    if eb + 2 < NEB:
                        loadE(eb + 2)
                finE(NEB - 1)
                fold_gain(W1m, "W1m", 2)
                if "E" in dbg:
                    d_h = dbgout("d_h", [128, 8, 2048])
                    sc.dma("sp", d_h.ap(), ht_d.ap(), r=["ht_d"], w=["d_h"])
                barrier()

            with contextlib.ExitStack() as esF:
                sbF, psF = mk(esF)
                W2m = sbF("W2m", [128, 32, 1024], BF16)
                hT = [sbF(f"hT{i}", [128, 8, EB], F32) for i in range(3)]
                hsq = sbF("hsq", [128, 8, EB], BF16)
                rsF = sbF("rsF", [128, EB], F32)
                rsO = sbF("rsO", [128, EB], F32)
                u2 = [sbF(f"u2{i}", [128, 8, EB], BF16) for i in range(2)]
                rl = [sbF(f"rl{i}", [128, EB], BF16) for i in range(2)]
                hid = sbF("hid", [128, 32, EB], BF16)
                fT = sbF("fT", [128, 8, EB], F32)
                fsq = sbF("fsq", [128, 8, EB], BF16)
                t1 = [sbF(f"t1F{i}", [128, EB], F32) for i in range(2)]
                pH = [psF(f"pH{i}") for i in range(3)]
                pF = [psF(f"pF{i}") for i in range(2)]
                pSt = psF("pStF")
                pSo = psF("pStO")
                for q4 in range(4):
                    sc.dma("pool", W2m[:, q4 * 8:(q4 + 1) * 8, :],
                           w_m2.ap()[q4 * 1024:(q4 + 1) * 1024, :].rearrange("(c p) n -> p c n", p=128), w=["W2m"])
                outv = out.ap().rearrange("(c p) t -> p c t", p=128)
                NFB = 2048 // EB

                def loadF(fb):
                    sc.dma("sp", hT[fb % 3][:], ht_d.ap()[:, :, fb * EB:(fb + 1) * EB], r=["ht_d"], w=[("hT", fb % 3)])

                def normF(fb):
                    s = fb % 2
                    norm_cols(hT[fb % 3], ("hT", fb % 3), hsq, "hsq", pSt, "pStF", rsF, "rsF", u2[s], ("u2", s), 2, EB,
                              split=(fb > 0))

                def hiddenF(fb):
                    s = fb % 2
                    for hc in range(32):
                        b = hc % 3
                        for kc in range(8):
                            sc.op("pe", "matmul", pH[b][:, 0:EB], W1m[:, kc, hc * 128:(hc + 1) * 128], u2[s][:, kc, :],
                                  start=(kc == 0), stop=(kc == 7), r=["W1m", ("u2", s)], w=[("pH", b)])
                        sc.op("act", "activation", rl[hc % 2][:], pH[b][:, 0:EB], AF.Relu, r=[("pH", b)], w=[("rl", hc % 2)])
                        sc.op("pool" if (fb > 0 and hc % 2 == 1) else "dve", "tensor_tensor", hid[:, hc, :], rl[hc % 2][:],
                              rl[hc % 2][:], ALU.mult, r=[("rl", hc % 2)], w=["hid"])

                def secondF(fb):
                    for fo in range(8):
                        b = fo % 2
                        for hc in range(32):
                            sc.op("pe", "matmul", pF[b][:, 0:EB], W2m[:, hc, fo * 128:(fo + 1) * 128], hid[:, hc, :],
                                  start=(hc == 0), stop=(hc == 31), r=["W2m", "hid"], w=[("pF", b)])
                        sc.op("act", "activation", fT[:, fo, :], pF[b][:, 0:EB], AF.Copy, r=[("pF", b)], w=["fT"])

                def outF(fb):
                    s = fb % 2
                    cs = slice(fb * EB, (fb + 1) * EB)
                    norm_cols(fT, "fT", fsq, "fsq", pSo, "pStO", rsO, "rsO", None, None, 3, EB)
                    for fo in range(8):
                        b = fo % 2
                        sc.op("dve", "scalar_tensor_tensor", t1[b][:], fT[:, fo, :], gn[:, 3, fo:fo + 1], rsO[:],
                              ALU.mult, ALU.mult, r=["fT", "rsO", "gn"], w=[("t1F", b)])
                        sc.op("dve" if fo % 2 == 0 else "pool", "tensor_tensor", hT[fb % 3][:, fo, :], t1[b][:],
                              hT[fb % 3][:, fo, :], ALU.add, r=[("t1F", b), ("hT", fb % 3)], w=[("hT", fb % 3)])
                    sc.dma("act", outv[:, :, cs], hT[fb % 3][:], r=[("hT", fb % 3)], w=["out"])

                loadF(0)
                loadF(1)
                loadF(2)
                normF(0)
                hiddenF(0)
                for fb in range(NFB):
                    if fb + 1 < NFB:
                        normF(fb + 1)
                    secondF(fb)
                    if fb + 1 < NFB:
                        hiddenF(fb + 1)
                    outF(fb)
                    if fb + 3 < NFB:
                        loadF(fb + 3)

        sc.wait_all("sp", ["out"] + list(dbo.keys()))
    print("instructions:", sc.ninst, "sems:", sc.nsem)
    return nc


IN_SIZES = (512,) + (128,) * 6 + (24, 512, 128, 128, 1024, 1024)
OFF = np.concatenate([[0], np.cumsum(IN_SIZES)]).astype(int)


def _bias_table(relx, delta, heads, lo, hi):
    p = np.arange(128)[:, None]
    q = np.arange(128)[None, :]
    dist = 128 * delta + q - p
    valid = (dist >= lo) & (dist < hi)
    b = np.where(valid, t5_bucket_np(dist), 32)
    out = np.stack([relx[b, h] for h in heads], axis=1)
    return np.ascontiguousarray(out.reshape(128, 512))


def prep_inputs(inputs):
    f = lambda k: np.asarray(inputs[k], np.float32)
    x = f("x")[0]
    w_in = f("w_in")[0]
    xT = np.ascontiguousarray(x.T)
    seg = lambda i: w_in[:, OFF[i]:OFF[i + 1]]
    cat = lambda l: np.ascontiguousarray(np.concatenate(l, axis=1))
    gains = np.zeros((128, 4, 8), np.float32)
    for gi, nm in enumerate(["norm_mix_pre", "norm_mix_post", "norm_mlp_pre", "norm_mlp_post"]):
        gains[:, gi, :] = f(nm)[0].reshape(8, 128).T
    indic = np.zeros((63, S), np.float32)
    blk = np.arange(S) // 64
    indic[blk % 62, np.arange(S)] = 1.0
    indic[62, :] = 1.0
    rel = f("rel_bias")
    relx = np.concatenate([rel, np.full((1, 16), NEG, np.float32)], axis=0)
    b31row = np.zeros((1, 2, 512), np.float32)
    for g in range(2):
        for h in range(4):
            b31row[0, g, h * 128:(h + 1) * 128] = rel[31, 4 * g + h]
    pcol = np.arange(128)[:, None]
    cend = (16 * (np.arange(8)[None, :] * 128 + pcol) + 31).astype(np.float32)
    n = (np.arange(8)[None, :, None] * 128 + pcol[:, :, None])
    jb = np.arange(256)[None, None, :]
    band = ((n >= 4 * jb - 1) & (n <= 4 * jb + 3) & (n <= 1022)).astype(np.float32)
    band[:, :, 0] = 1.0
    nt_win = np.stack([np.stack([_bias_table(relx, 4 - i, [4 * g + h for h in range(4)], 0, 512) for g in range(2)])
                       for i in range(5)])
    nt_swa = np.stack([np.stack([_bias_table(relx, 1 - i, [8 + 4 * g + h for h in range(4)], 0, 128) for g in range(2)])
                       for i in range(2)])
    common = dict(
        xT_all=xT,
        w_A=cat([seg(1), seg(2), seg(3), seg(4)]), w_Q=cat([seg(0), seg(8)]),
        w_ga=np.ascontiguousarray(seg(11)), w_gb=np.ascontiguousarray(seg(12)),
        w_kv2=cat([seg(5), seg(6), seg(9), seg(10)]), w_gn=np.ascontiguousarray(seg(7)),
        gains=gains, indic=indic.astype(ml_dtypes.bfloat16),
        w1k=f("cmp_w1_k")[0], w1v=f("cmp_w1_v")[0], w2k=f("cmp_w2_k")[0], w2v=f("cmp_w2_v")[0],
        posk=np.ascontiguousarray(f("cmp_pos_k")[0].reshape(16, 128).T),
        posv=np.ascontiguousarray(f("cmp_pos_v")[0].reshape(16, 128).T),
        nt_win=nt_win, nt_swa=nt_swa, b31row=b31row,
        sinks=np.ascontiguousarray(np.tile(f("attn_sinks")[0][None, :], (128, 1))),
        cend=cend, band=band.astype(ml_dtypes.bfloat16),
        ident=np.eye(128, dtype=np.float32).astype(ml_dtypes.bfloat16), identf=np.eye(128, dtype=np.float32),
        w_upA=f("w_up_nsa")[0], w_upB=f("w_up_swa")[0], w_out=f("w_out")[0],
        w_m1=f("w_mlp_in")[0], w_m2=f("w_mlp_out")[0],
    )
    in_maps = []
    for c in range(NCORES):
        own = own_chunks(c)
        m = dict(common)
        m["xT_own"] = np.ascontiguousarray(np.concatenate([xT[:, ci * 128:(ci + 1) * 128] for ci in own], axis=1))
        kv2 = np.zeros((D, 16, 640), np.float32)
        hv = np.zeros((128, 16, 5), np.float32)
        atab = np.zeros((128, 16, 310), np.float32)
        trow = np.zeros((128, 16, 128), np.float32)
        bl = np.arange(310)[None, :]
        for j, ci in enumerate(own):
            for i in range(5):
                cc = ci - 4 + i
                if cc >= 0:
                    kv2[:, j, i * 128:(i + 1) * 128] = xT[:, cc * 128:(cc + 1) * 128]
                    hv[:, j, i] = 1.0
            t = 128 * ci + np.arange(128)[:, None]
            cur = t // 64
            valid = (bl <= cur) & (bl < 256)
            forced = (bl == 0) | (bl == cur) | (bl == cur - 1)
            atab[:, j, :] = np.where(valid, np.where(forced, 8192.0, 0.0), -8192.0)
            trow[:, j, :] = 128 * ci + np.arange(128)[None, :]
        m["xT_kv2"] = kv2
        m["hv"] = hv
        m["atab"] = atab.astype(ml_dtypes.bfloat16)
        m["trow"] = trow
        nts = np.zeros((2, 15, 2, 128, 512), np.float32)
        for s in range(15):
            for g in range(2):
                heads = [4 * g + h for h in range(4)]
                nts[0, s, g] = _bias_table(relx, c + 7 - s, heads, 0, 1 << 30)
                nts[1, s, g] = _bias_table(relx, 14 - c - s, heads, 0, 1 << 30)
        m["nt_sel"] = nts
        in_maps.append(m)
    return in_maps


def kernel(**inputs):
    dbg = set(os.environ.get("KDBG", "").split(",")) - {""}
    in_maps = prep_inputs(inputs)
    nc = build(dbg)
    res = run_bass_kernel_spmd(nc, in_maps, core_ids=list(range(NCORES)))
    if dbg:
        kernel.last = res
        kernel.in_maps = in_maps
    outT = np.zeros((D, S), np.float32)
    for c in range(NCORES):
        o = np.asarray(res.results[c]["out"])
        for j, ci in enumerate(own_chunks(c)):
            outT[:, ci * 128:(ci + 1) * 128] = o[:, j * 128:(j + 1) * 128]
    return np.ascontiguousarray(outT.T)[None].astype(np.float32)
```

```python
import contextlib
import os
import numpy as np
import ml_dtypes
import concourse.bass as bass
import concourse.mybir as mybir
from concourse.bass_utils import run_bass_kernel_spmd

F32 = mybir.dt.float32
BF16 = mybir.dt.bfloat16
ALU = mybir.AluOpType
AF = mybir.ActivationFunctionType
AX = mybir.AxisListType

NCORES = 8
S = 16384
D = 1024
NCH = S // 128
NOWN = 16
EPS = 1e-6
NEG = -30000.0
SCALE = 0.125


class Sch:
    def __init__(self, nc, es):
        self.nc, self.es = nc, es
        self.eng = {"pe": nc.tensor, "dve": nc.vector, "act": nc.scalar, "pool": nc.gpsimd, "sp": nc.sync}
        self.nsem = 0
        self.psem, self.pcnt = {}, {}
        for e in self.eng:
            self._new_psem(e)
        self.waited = {}
        self.lastw = {}
        self.readers = {}
        self.dsem = {}
        self.ninst = 0

    def _sem(self, name):
        self.nsem += 1
        return self.es.enter_context(self.nc.semaphore(f"{name}_{self.nsem}"))

    def _new_psem(self, e):
        self.psem[e] = self._sem("p_" + e)
        self.pcnt[e] = 0

    def _deps(self, r, w):
        toks = []
        for k in r:
            if k in self.lastw:
                toks.append(self.lastw[k])
        for k in w:
            if k in self.lastw:
                toks.append(self.lastw[k])
            toks.extend(self.readers.get(k, {}).values())
        return toks

    def _wait(self, e, toks):
        for (sem, val, src) in toks:
            if src == "pe" and e == "pe":
                continue
            k = (e, id(sem))
            if self.waited.get(k, 0) >= val:
                continue
            self.eng[e].wait_ge(sem, val)
            self.waited[k] = val

    def _record(self, tok, r, w):
        for k in r:
            d = self.readers.setdefault(k, {})
            d[id(tok[0])] = tok
        for k in w:
            self.lastw[k] = tok
            self.readers[k] = {}

    def op(self, e, meth, *args, r=(), w=(), **kw):
        self._wait(e, self._deps(r, w))
        ins = getattr(self.eng[e], meth)(*args, **kw)
        if self.pcnt[e] >= 8000:
            self._new_psem(e)
        self.pcnt[e] += 1
        ins.then_inc(self.psem[e], 1)
        tok = (self.psem[e], self.pcnt[e], e)
        self._record(tok, r, w)
        self.ninst += 1
        return tok

    def dma(self, q, out, in_, r=(), w=(), key=None):
        self._wait(q, self._deps(r, w))
        key = key if key is not None else (w[0] if w else ("rd", r[0]))
        ent = self.dsem.get(key)
        if ent is None or ent[1] >= 30000:
            ent = [self._sem("d"), 0]
            self.dsem[key] = ent
        ins = self.eng[q].dma_start(out=out, in_=in_)
        ent[1] += 16
        ins.then_inc(ent[0], 16)
        tok = (ent[0], ent[1], "dma")
        self._record(tok, r, w)
        self.ninst += 1
        return tok

    def wait_all(self, e, keys):
        toks = []
        for k in keys:
            if k in self.lastw:
                toks.append(self.lastw[k])
            toks.extend(self.readers.get(k, {}).values())
        self._wait(e, toks)


def own_chunks(c):
    out = []
    for k in range(8):
        out.append(16 * k + c)
        out.append(16 * k + 15 - c)
    return out


def t5_bucket_np(dist):
    d = np.maximum(dist, 0)
    df = np.maximum(d, 1).astype(np.float32)
    large = 16 + (np.log(df / 16) / np.float32(np.log(1024 / 16)) * 16).astype(np.int32)
    large = np.minimum(large, 31)
    return np.where(d < 16, d, large)


def build(dbg=None):
    dbg = dbg or set()
    nc = bass.Bass("TRN2", target_bir_lowering=False)
    I, O = {}, {}

    def inp(name, shape, dt=F32):
        I[name] = nc.dram_tensor(name, list(shape), dt, kind="ExternalInput")
        return I[name]

    def outp(name, shape, dt=F32):
        O[name] = nc.dram_tensor(name, list(shape), dt, kind="ExternalOutput")
        return O[name]

    def scratch(name, shape, dt=BF16):
        return nc.dram_tensor(name, list(shape), dt)

    xT_all = inp("xT_all", [D, S])
    xT_own = inp("xT_own", [D, 2048])
    xT_kv2 = inp("xT_kv2", [D, 16, 640])
    hv_in = inp("hv", [128, 16, 5])
    w_A = inp("w_A", [D, 512])
    w_Q = inp("w_Q", [D, 1024])
    w_ga = inp("w_ga", [D, 1024])
    w_gb = inp("w_gb", [D, 1024])
    w_kv2 = inp("w_kv2", [D, 512])
    w_gn = inp("w_gn", [D, 24])
    gains = inp("gains", [128, 4, 8])
    indic = inp("indic", [63, S], BF16)
    w1c = [inp("w1k", [2048, 256]), inp("w1v", [2048, 256])]
    w2c = [inp("w2k", [256, 64]), inp("w2v", [256, 64])]
    posc_in = [inp("posk", [128, 16]), inp("posv", [128, 16])]
    nt_sel = inp("nt_sel", [2, 15, 2, 128, 512])
    nt_win = inp("nt_win", [5, 2, 128, 512])
    nt_swa = inp("nt_swa", [2, 2, 128, 512])
    b31_in = inp("b31row", [1, 2, 512])
    sinks_in = inp("sinks", [128, 8])
    atab_in = inp("atab", [128, 16, 310], BF16)
    trow_in = inp("trow", [128, 16, 128])
    cend_in = inp("cend", [128, 8])
    band_in = inp("band", [128, 8, 256], BF16)
    ident_in = inp("ident", [128, 128], BF16)
    identf_in = inp("identf", [128, 128])
    w_upA = inp("w_upA", [512, 1024])
    w_upB = inp("w_upB", [512, 1024])
    w_out = inp("w_out", [D, D])
    w_m1 = inp("w_m1", [D, 4096])
    w_m2 = inp("w_m2", [4096, D])

    kcnT = scratch("kcnT", [128, S])
    vcnT = scratch("vcnT", [128, S])
    qn_d = scratch("qn_d", [64, 16, 8, 128])
    qs_d = scratch("qs_d", [64, 16, 8, 128])
    gate_d = [scratch("ga_d", [128, 8, 2048]), scratch("gb_d", [128, 8, 2048])]
    kw_d = scratch("kw_d", [64, 16, 2, 640])
    vw_d = scratch("vw_d", [128, 16, 5, 2, 65])
    k2_d = scratch("k2_d", [64, 16, 2, 256])
    v2_d = scratch("v2_d", [128, 16, 2, 2, 65])
    oat_d = scratch("oat_d", [128, 4, 2048])
    obt_d = scratch("obt_d", [128, 4, 2048])
    ht_d = scratch("ht_d", [128, 8, 2048], F32)
    EB = 256

    out = outp("out", [D, NOWN * 128])
    dbo = {}

    def dbgout(name, shape, dt=F32):
        dbo[name] = outp(name, shape, dt)
        return dbo[name]

    es = contextlib.ExitStack()
    with es:
        sc = Sch(nc, es)

        uid = [0]

        def mk(stack):
            def sbx(name, shape, dt):
                uid[0] += 1
                return stack.enter_context(nc.sbuf_tensor(f"{name}_{uid[0]}", list(shape), dt))

            def psx(name, shape=(128, 512), dt=F32):
                uid[0] += 1
                return stack.enter_context(nc.psum_tensor(f"{name}_{uid[0]}", list(shape), dt))
            return sbx, psx

        sb, ps = mk(es)

        def barrier():
            toks = [(sc.psem[e], sc.pcnt[e], e) for e in sc.eng if sc.pcnt[e] > 0]
            toks += [(ent[0], ent[1], "dma") for ent in sc.dsem.values() if ent[1] > 0]
            for e in sc.eng:
                for (sem, val, src) in toks:
                    if src == e:
                        continue
                    k = (e, id(sem))
                    if sc.waited.get(k, 0) >= val:
                        continue
                    sc.eng[e].wait_ge(sem, val)
                    sc.waited[k] = val

        ones = sb("ones", [128, 128], BF16)
        gn = sb("gn", [128, 4, 8], F32)
        GS = sb("GS", [128, 16, 24], F32)
        sc.op("pool", "memset", ones[:], 1.0, w=["ones"])
        sc.dma("sp", gn[:], gains.ap(), w=["gn"])

        def norm_cols(xt_ap, xkey, sq_ap, sqkey, pstat, pkey, rs_ap, rskey, u_ap, ukey, gidx, n, split=True,
                      do_sq=True, do_rest=True):
            if do_sq:
                sc.op("act", "activation", sq_ap[:], xt_ap[:], AF.Square, r=[xkey], w=[sqkey])
            if not do_rest:
                return
            for c0 in range(0, n, 512):
                c1 = min(n, c0 + 512)
                for kc in range(8):
                    sc.op("pe", "matmul", pstat[:, 0:c1 - c0], ones[:], sq_ap[:, kc, c0:c1], start=(kc == 0), stop=(kc == 7),
                          r=[sqkey, "ones"], w=[pkey])
                sc.op("dve", "tensor_scalar", rs_ap[:, c0:c1], pstat[:, 0:c1 - c0], 1.0 / D, EPS, ALU.mult, ALU.add,
                      r=[pkey], w=[rskey])
            sc.op("act", "activation", rs_ap[:, 0:n], rs_ap[:, 0:n], AF.Sqrt, r=[rskey], w=[rskey])
            sc.op("dve", "reciprocal", rs_ap[:, 0:n], rs_ap[:, 0:n], r=[rskey], w=[rskey])
            if u_ap is not None:
                for (e, k0, k1) in ((("dve", 0, 5), ("pool", 5, 8)) if split else (("dve", 0, 8),)):
                    sc.op(e, "tensor_tensor", u_ap[:, k0:k1, :], xt_ap[:, k0:k1, :],
                          rs_ap[:, 0:n].unsqueeze(1).broadcast_to([128, k1 - k0, n]), ALU.mult,
                          r=[xkey, rskey], w=[ukey])

        def fold_gain(W, wkey, gidx):
            for kc in range(8):
                sc.op("dve", "tensor_scalar", W[:, kc, :], W[:, kc, :], gn[:, gidx, kc:kc + 1], None, ALU.mult,
                      r=[wkey, "gn"], w=[wkey])

        with contextlib.ExitStack() as esP:
            sbP, psP = mk(esP)
            KT = sbP("KT", [128, 2, S], BF16)
            VAf = sbP("VAf", [128, NCH * 130 + 64], BF16)
            VA = VAf[:, 0:NCH * 130].rearrange("p (t g e) -> p t g e", g=2, e=65)
            KC = sbP("KC", [128, 2, 1024], BF16)
            VCf = sbP("VCf", [128, 8 * 130 + 64], BF16)
            VC = VCf[:, 0:8 * 130].rearrange("p (t g e) -> p t g e", g=2, e=65)
            sc.op("pool", "memset", VAf[:], 0.0, w=["VA"])
            sc.op("pool", "memset", VA[:, :, :, 64:65], 1.0, w=["VA"])
            sc.op("pool", "memset", KC[:], 0.0, w=["KC"])
            sc.op("pool", "memset", VCf[:], 0.0, w=["VC"])
            sc.op("pool", "memset", VC[:, :, :, 64:65], 1.0, w=["VC"])
            for g in range(2):
                sc.dma("pool", KT[64:127, g, :], indic.ap(), w=[("KTc", g)])

            with contextlib.ExitStack() as esA:
                sbA, psA = mk(esA)
                WA = sbA("WA", [128, 8, 512], BF16)
                xt = [sbA(f"xt{i}", [128, 8, 512], F32) for i in range(3)]
                sq = [sbA(f"sq{i}", [128, 8, 512], BF16) for i in range(2)]
                uT = [sbA(f"uT{i}", [128, 8, 512], BF16) for i in range(2)]
                rs = [sbA(f"rs{i}", [128, 512], F32) for i in range(2)]
                stg = [sbA(f"stg{i}", [128, 2, 512], BF16) for i in range(2)]
                p_stat = [psA(f"p_stat{i}") for i in range(2)]
                p_kcn = psA("p_kcn")
                p_vcn = psA("p_vcn")
                p_ks = psA("p_ks")
                p_v = [psA(f"p_v{i}", [128, 4, 128]) for i in range(2)]

                sc.dma("pool", WA[:], w_A.ap().rearrange("(kc p) n -> p kc n", p=128), w=["WA"])
                fold_gain(WA, "WA", 0)
                xsrc = xT_all.ap().rearrange("(kc p) t -> p kc t", p=128)
                NB = 32 if "Ashort" not in dbg else 2
                kcn_v = kcnT.ap().rearrange("p (s n) -> p s n", s=16)
                vcn_v = vcnT.ap().rearrange("p (s n) -> p s n", s=16)

                def loadA(tb):
                    sc.dma("sp", xt[tb % 3][:], xsrc[:, :, tb * 512:(tb + 1) * 512], w=[("xt", tb % 3)])

                def sqA(tb):
                    s = tb % 2
                    norm_cols(xt[tb % 3], ("xt", tb % 3), sq[s], ("sq", s), p_stat[s], ("p_stat", s), rs[s], ("rs", s),
                              uT[s], ("uT", s), 0, 512, do_rest=False)

                def restA(tb):
                    s = tb % 2
                    norm_cols(xt[tb % 3], ("xt", tb % 3), sq[s], ("sq", s), p_stat[s], ("p_stat", s), rs[s], ("rs", s),
                              uT[s], ("uT", s), 0, 512, do_sq=False)

                def projA(tb):
                    s = tb % 2
                    cs = slice(tb * 512, (tb + 1) * 512)
                    for (pp, name, c0) in ((p_kcn, "p_kcn", 0), (p_vcn, "p_vcn", 128), (p_ks, "p_ks", 256)):
                        for kc in range(8):
                            sc.op("pe", "matmul", pp[:], WA[:, kc, c0:c0 + 128], uT[s][:, kc, :],
                                  start=(kc == 0), stop=(kc == 7), r=[("uT", s), "WA"], w=[name])
                    for t4 in range(4):
                        for kc in range(8):
                            sc.op("pe", "matmul", p_v[s][:, t4, :], uT[s][:, kc, t4 * 128:(t4 + 1) * 128],
                                  WA[:, kc, 384:512], start=(kc == 0), stop=(kc == 7),
                                  r=[("uT", s), "WA"], w=[("p_v", s)])
                    sc.op("act", "activation", stg[s][:, 0, :].rearrange("p (s n) -> p s n", s=16),
                          p_kcn[:].rearrange("p (n s) -> p s n", s=16), AF.Copy, r=["p_kcn"], w=[("stg", s)])
                    sc.op("act", "activation", stg[s][:, 1, :].rearrange("p (s n) -> p s n", s=16),
                          p_vcn[:].rearrange("p (n s) -> p s n", s=16), AF.Copy, r=["p_vcn"], w=[("stg", s)])
                    sc.dma("pool", kcn_v[:, :, tb * 32:(tb + 1) * 32], stg[s][:, 0, :].rearrange("p (s n) -> p s n", s=16),
                           r=[("stg", s)], w=["kcnT"])
                    sc.dma("pool", vcn_v[:, :, tb * 32:(tb + 1) * 32], stg[s][:, 1, :].rearrange("p (s n) -> p s n", s=16),
                           r=[("stg", s)], w=["vcnT"])
                    sc.op("dve", "tensor_copy", KT[0:64, 0, cs], p_ks[0:64, :], r=["p_ks"], w=[("KT", 0)])
                    sc.op("dve", "tensor_copy", KT[0:64, 1, cs], p_ks[64:128, :], r=["p_ks"], w=[("KT", 1)])
                    sc.op("dve", "tensor_copy", VA[:, tb * 4:(tb + 1) * 4, :, 0:64],
                          p_v[s][:].rearrange("p t (g d) -> p t g d", g=2), r=[("p_v", s)], w=["VA"])

                for tb in range(min(3, NB)):
                    loadA(tb)
                sqA(0)
                restA(0)
                if NB > 1:
                    sqA(1)
                for tb in range(NB):
                    if tb + 1 < NB:
                        restA(tb + 1)
                    if tb + 2 < NB:
                        sqA(tb + 2)
                    projA(tb)
                    if tb + 3 < NB:
                        loadA(tb + 3)
                barrier()

            with contextlib.ExitStack() as esB:
                sbB, psB = mk(esB)
                X2s = [sbB(f"X2{i}", [128, 16, 1024], BF16) for i in range(2)]
                W1 = [sbB(f"W1_{i}", [128, 16, 256], BF16) for i in range(2)]
                W2 = [sbB(f"W2_{i}", [128, 2, 64], BF16) for i in range(2)]
                posc = [sbB(f"posc{i}", [128, 16], BF16) for i in range(2)]
                cb = [sbB(f"cb{i}", [128, 2], F32) for i in range(2)]
                hbs = [sbB(f"hb{i}", [128, 512], F32) for i in range(2)]
                t1s = [sbB(f"t1{i}", [128, 512], F32) for i in range(2)]
                sgs = [sbB(f"sg{i}", [128, 512], F32) for i in range(2)]
                G = [sbB(f"G{i}", [128, 512], BF16) for i in range(2)]
                p_h = [psB(f"p_h{i}") for i in range(2)]
                p_c = psB("p_c", [128, 2])
                p_o = psB("p_o")
                p_vo = psB("p_vo", [128, 4, 64])
                for i in range(2):
                    sc.dma("pool", W1[i][:], w1c[i].ap().rearrange("(jp p) n -> p jp n", p=128), w=[("W1", i)])
                    sc.dma("pool", W2[i][:], w2c[i].ap().rearrange("(hc p) d -> p hc d", p=128), w=[("W2", i)])
                    sc.dma("pool", posc[i][:], posc_in[i].ap(), w=[("posc", i)])
                sc.op("pool", "memset", G[0][:], 0.0, w=[("G", 0)])
                sc.op("pool", "memset", G[1][:], 0.0, w=[("G", 1)])
                for i in range(2):
                    sc.op("pool", "memset", X2s[i][:], 0.0, w=[("X2", i)])
                def loadX2(kv_, g_):
                    xi_ = (kv_ * 2 + g_) % 2
                    src_ = kcnT if kv_ == 0 else vcnT
                    key_ = "kcnT" if kv_ == 0 else "vcnT"
                    srcv = src_.ap().rearrange("p (s n) -> p s n", s=16)[g_ * 64:(g_ + 1) * 64]
                    sc.dma("sp", X2s[xi_][0:64, :, :], srcv, r=[key_], w=[("X2", xi_)])
                    sc.dma("act", X2s[xi_][64:128, 0:15, :], srcv[:, 1:16, :], r=[key_], w=[("X2", xi_)])
                    sc.dma("act", X2s[xi_][64:128, 15, 0:1023], srcv[:, 0, 1:1024], r=[key_], w=[("X2", xi_)])

                for kv in range(2):
                    for hc in range(2):
                        for jp in range(16):
                            sc.op("pe", "matmul", p_c[:, hc:hc + 1], W1[kv][:, jp, hc * 128:(hc + 1) * 128],
                                  posc[kv][:, jp:jp + 1], start=(jp == 0), stop=(jp == 15),
                                  r=[("W1", kv), ("posc", kv)], w=["p_c"])
                    sc.op("dve", "tensor_copy", cb[kv][:], p_c[:], r=["p_c"], w=[("cb", kv)])
                    src = kcnT if kv == 0 else vcnT
                    srckey = "kcnT" if kv == 0 else "vcnT"
                    for g in range(2):
                        xi = (kv * 2 + g) % 2
                        X2 = X2s[xi]
                        if kv == 0 and g == 0:
                            loadX2(0, 0)
                        if kv * 2 + g + 1 < 4:
                            loadX2((kv * 2 + g + 1) // 2, (kv * 2 + g + 1) % 2)
                        for nb in range(2):
                            n0 = nb * 512
                            ncols = 512 if nb == 0 else 511
                            for hc in range(2):
                                for jp in range(16):
                                    if jp < 8:
                                        rhs = X2[:, 2 * jp, n0:n0 + ncols]
                                    else:
                                        rhs = X2[:, 2 * jp - 16, n0 + 1:n0 + 1 + ncols]
                                    sc.op("pe", "matmul", p_h[hc][:, 0:ncols], W1[kv][:, jp, hc * 128:(hc + 1) * 128], rhs,
                                          start=(jp == 0), stop=(jp == 15), r=[("W1", kv), ("X2", xi)], w=[("p_h", hc)])
                                hb, t1, sg = hbs[hc], t1s[hc], sgs[hc]
                                hbk, t1k, sgk = ("hb", hc), ("t1", hc), ("sg", hc)
                                sc.op("act", "activation", hb[:, 0:ncols], p_h[hc][:, 0:ncols], AF.Identity,
                                      bias=cb[kv][:, hc:hc + 1], r=[("p_h", hc), ("cb", kv)], w=[hbk])
                                sc.op("dve", "tensor_tensor", t1[:, 0:ncols], hb[:, 0:ncols], hb[:, 0:ncols], ALU.mult,
                                      r=[hbk], w=[t1k])
                                sc.op("dve", "tensor_scalar", t1[:, 0:ncols], t1[:, 0:ncols], 0.044715, 1.0, ALU.mult, ALU.add,
                                      r=[t1k], w=[t1k])
                                sc.op("dve", "tensor_tensor", t1[:, 0:ncols], t1[:, 0:ncols], hb[:, 0:ncols], ALU.mult,
                                      r=[t1k, hbk], w=[t1k])
                                sc.op("act", "activation", sg[:, 0:ncols], t1[:, 0:ncols], AF.Sigmoid, scale=1.5957691216,
                                      r=[t1k], w=[sgk])
                                sc.op("dve", "tensor_tensor", G[hc][:, 0:ncols], hb[:, 0:ncols], sg[:, 0:ncols], ALU.mult,
                                      r=[hbk, sgk], w=[("G", hc)])
                            if kv == 0:
                                for hc in range(2):
                                    sc.op("pe", "matmul", p_o[0:64, 0:ncols], W2[0][:, hc, :], G[hc][:, 0:ncols],
                                          start=(hc == 0), stop=(hc == 1), r=[("W2", 0), ("G", hc)], w=["p_o"])
                                sc.op("act", "activation", KC[0:64, g, n0:n0 + ncols], p_o[0:64, 0:ncols], AF.Copy,
                                      r=["p_o"], w=["KC"])
                            else:
                                for t4 in range(4):
                                    for hc in range(2):
                                        sc.op("pe", "matmul", p_vo[:, t4, :], G[hc][:, t4 * 128:(t4 + 1) * 128],
                                              W2[1][:, hc, :], start=(hc == 0), stop=(hc == 1),
                                              r=[("W2", 1), ("G", hc)], w=["p_vo"])
                                sc.op("dve", "tensor_copy", VC[:, nb * 4:(nb + 1) * 4, g, 0:64], p_vo[:],
                                      r=["p_vo"], w=["VC"])
                if "B" in dbg:
                    d_kc = dbgout("d_kc", [128, 2, 1024], BF16)
                    d_vc = dbgout("d_vc", [128, 8, 2, 65], BF16)
                    sc.dma("sp", d_kc.ap(), KC[:], r=["KC"], w=["d_kc"])
                    sc.dma("sp", d_vc.ap(), VC[:], r=["VC"], w=["d_vc"])
                barrier()

            with contextlib.ExitStack() as esC:
                sbC, psC = mk(esC)
                Wbs = [sbC(f"Wb{i}", [128, 8, 1024], BF16) for i in range(2)]
                Wb = Wbs[0]
                Wgn = sbC("Wgn", [128, 8, 24], BF16)
                uTo = sbC("uTo", [128, 8, 2048], BF16)
                xos = [sbC(f"xo{i}", [128, 8, 256], F32) for i in range(2)]
                sqo = sbC("sqo", [128, 8, 256], BF16)
                rso = sbC("rso", [128, 256], F32)
                qst1 = sbC("qst", [64, 4, 8, 128], BF16)
                qst = [qst1, qst1]
                gst = [sbC(f"gst{i}", [128, 512], BF16) for i in range(2)]
                p_st = psC("p_st")
                p_q = [psC(f"p_q{i}") for i in range(3)]
                p_gn = psC("p_gn", [128, 24])
                xosrc = xT_own.ap().rearrange("(kc p) t -> p kc t", p=128)
                sc.dma("pool", Wb[:], w_Q.ap().rearrange("(kc p) n -> p kc n", p=128), w=[("Wb", 0)])
                sc.dma("pool", Wgn[:], w_gn.ap().rearrange("(kc p) n -> p kc n", p=128), w=["Wgn"])
                fold_gain(Wb, ("Wb", 0), 0)
                fold_gain(Wgn, "Wgn", 0)
                sc.dma("pool", Wbs[1][:], w_ga.ap().rearrange("(kc p) n -> p kc n", p=128), w=[("Wb", 1)])
                fold_gain(Wbs[1], ("Wb", 1), 0)
                sc.dma("sp", xos[0][:], xosrc[:, :, 0:256], w=[("xo", 0)])
                for ob in range(8):
                    cs = slice(ob * 256, (ob + 1) * 256)
                    if ob + 1 < 8:
                        sc.dma("sp", xos[(ob + 1) % 2][:], xosrc[:, :, (ob + 1) * 256:(ob + 2) * 256], w=[("xo", (ob + 1) % 2)])
                    norm_cols(xos[ob % 2], ("xo", ob % 2), sqo, "sqo", p_st, "p_st", rso, "rso", uTo[:, :, cs],
                              ("uTo", ob // 2), 0, 256)
                cnt = 0
                for ob in range(4):
                    cs = slice(ob * 512, (ob + 1) * 512)
                    for h16 in range(16):
                        b = cnt % 3
                        cnt += 1
                        for kc in range(8):
                            sc.op("pe", "matmul", p_q[b][0:64, :], Wb[:, kc, h16 * 64:(h16 + 1) * 64], uTo[:, kc, cs],
                                  start=(kc == 0), stop=(kc == 7), r=[("Wb", 0), ("uTo", ob)], w=[("p_q", b)])
                        sc.op("dve", "tensor_scalar", qst[h16 // 8][:, :, h16 % 8, :],
                              p_q[b][0:64, :].rearrange("p (j q) -> p j q", j=4), SCALE, None, ALU.mult,
                              r=[("p_q", b)], w=["qst"])
                        if h16 == 7:
                            sc.dma("sp", qn_d.ap()[:, ob * 4:(ob + 1) * 4], qst[0][:], r=["qst"], w=["qn_d"])
                    sc.dma("sp", qs_d.ap()[:, ob * 4:(ob + 1) * 4], qst[1][:], r=["qst"], w=["qs_d"])
                for j in range(16):
                    for kc in range(8):
                        sc.op("pe", "matmul", p_gn[:], uTo[:, kc, j * 128:(j + 1) * 128], Wgn[:, kc, :],
                              start=(kc == 0), stop=(kc == 7), r=[("uTo", j // 4), "Wgn"], w=["p_gn"])
                    sc.op("act", "activation", GS[:, j, :], p_gn[:], AF.Sigmoid, r=["p_gn"], w=["GS"])
                cnt = 0
                for gi, wsrc in enumerate((w_ga, w_gb)):
                    Wb = Wbs[1 - gi]
                    wkey = ("Wb", 1 - gi)
                    if gi == 1:
                        sc.dma("pool", Wb[:], wsrc.ap().rearrange("(kc p) n -> p kc n", p=128), r=[], w=[wkey])
                        fold_gain(Wb, wkey, 0)
                    for ob in range(4):
                        cs = slice(ob * 512, (ob + 1) * 512)
                        for fc in range(8):
                            b = cnt % 3
                            s2 = cnt % 2
                            cnt += 1
                            for kc in range(8):
                                sc.op("pe", "matmul", p_q[b][:], Wb[:, kc, fc * 128:(fc + 1) * 128], uTo[:, kc, cs],
                                      start=(kc == 0), stop=(kc == 7), r=[wkey, ("uTo", ob)], w=[("p_q", b)])
                            sc.op("act", "activation", gst[s2][:], p_q[b][:], AF.Sigmoid, r=[("p_q", b)], w=[("gst", s2)])
                            sc.dma("act", gate_d[gi].ap()[:, fc, cs], gst[s2][:], r=[("gst", s2)], w=[("gate_d", gi)])
                if "C" in dbg:
                    d_gs = dbgout("d_gs", [128, 16, 24])
                    sc.dma("sp", d_gs.ap(), GS[:], r=["GS"], w=["d_gs"])
                    d_qn = dbgout("d_qn", [64, 16, 8, 128], BF16)
                    sc.dma("sp", d_qn.ap(), qn_d.ap(), r=["qn_d"], w=["d_qn"])
                    d_ga = dbgout("d_ga", [128, 8, 2048], BF16)
                    sc.dma("sp", d_ga.ap(), gate_d[0].ap(), r=[("gate_d", 0)], w=["d_ga"])
                barrier()

            with contextlib.ExitStack() as esC:
                sbC, psC = mk(esC)
                Wk = sbC("Wk", [128, 8, 512], BF16)
                hvs = sbC("hvs", [128, 16, 5], F32)
                xks = [sbC(f"xk{i}", [128, 8, 640], F32) for i in range(2)]
                sqk = sbC("sqk", [128, 8, 640], BF16)
                rsks = [sbC(f"rsk{i}", [128, 640], F32) for i in range(2)]
                uTk = [sbC(f"uTk{i}", [128, 8, 640], BF16) for i in range(2)]
                kst = [sbC(f"kst{i}", [64, 2, 640], BF16) for i in range(2)]
                vst = [sbC(f"vst{i}", [128, 5, 2, 65], BF16) for i in range(2)]
                k2st = [sbC(f"k2st{i}", [64, 2, 256], BF16) for i in range(2)]
                v2st = [sbC(f"v2st{i}", [128, 2, 2, 65], BF16) for i in range(2)]
                p_st = psC("p_st")
                p_kw = [psC(f"p_kw{i}") for i in range(2)]
                p_vw = [psC(f"p_vw{i}", [128, 4, 128]) for i in range(2)]
                p_k2 = psC("p_k2", [128, 256])
                p_v2 = psC("p_v2", [128, 2, 128])
                sc.dma("pool", Wk[:], w_kv2.ap().rearrange("(kc p) n -> p kc n", p=128), w=["Wk"])
                fold_gain(Wk, "Wk", 0)
                sc.dma("sp", hvs[:], hv_in.ap(), w=["hvs"])
                xksrc = xT_kv2.ap().rearrange("(kc p) j t -> p kc j t", p=128)
                def loadK(j):
                    sc.dma("sp", xks[j % 2][:], xksrc[:, :, j, :], w=[("xk", j % 2)])

                def normK(j):
                    s = j % 2
                    norm_cols(xks[s], ("xk", s), sqk, "sqk", p_st, "p_st", rsks[s], ("rsk", s), uTk[s], ("uTk", s), 0, 640)

                loadK(0)
                loadK(1)
                normK(0)
                for j in range(16):
                    s = j % 2
                    if j + 1 < 16:
                        normK(j + 1)
                    for (c0, c1, pi) in ((0, 512, 0), (512, 640, 1)):
                        for kc in range(8):
                            sc.op("pe", "matmul", p_kw[pi][:, 0:c1 - c0], Wk[:, kc, 0:128], uTk[s][:, kc, c0:c1],
                                  start=(kc == 0), stop=(kc == 7), r=["Wk", ("uTk", s)], w=[("p_kw", pi)])
                        sc.op("dve", "tensor_copy", kst[s][0:64, 0, c0:c1], p_kw[pi][0:64, 0:c1 - c0],
                              r=[("p_kw", pi)], w=[("kst", s)])
                        sc.op("dve", "tensor_copy", kst[s][0:64, 1, c0:c1], p_kw[pi][64:128, 0:c1 - c0],
                              r=[("p_kw", pi)], w=[("kst", s)])
                    for t5 in range(5):
                        pv = p_vw[0][:, t5, :] if t5 < 4 else p_vw[1][:, 0, :]
                        for kc in range(8):
                            sc.op("pe", "matmul", pv, uTk[s][:, kc, t5 * 128:(t5 + 1) * 128], Wk[:, kc, 128:256],
                                  start=(kc == 0), stop=(kc == 7), r=["Wk", ("uTk", s)], w=[("p_vw", 0 if t5 < 4 else 1)])
                    sc.op("dve", "tensor_copy", vst[s][:, 0:4, :, 0:64], p_vw[0][:].rearrange("p t (g d) -> p t g d", g=2),
                          r=[("p_vw", 0)], w=[("vst", s)])
                    sc.op("dve", "tensor_copy", vst[s][:, 4, :, 0:64], p_vw[1][:, 0, :].rearrange("p (g d) -> p g d", g=2),
                          r=[("p_vw", 1)], w=[("vst", s)])
                    sc.op("dve", "tensor_copy", vst[s][:, :, :, 64],
                          hvs[:, j, :].unsqueeze(2).broadcast_to([128, 5, 2]), r=["hvs"], w=[("vst", s)])
                    for kc in range(8):
                        sc.op("pe", "matmul", p_k2[:], Wk[:, kc, 256:384], uTk[s][:, kc, 384:640],
                              start=(kc == 0), stop=(kc == 7), r=["Wk", ("uTk", s)], w=["p_k2"])
                    sc.op("dve", "tensor_copy", k2st[s][0:64, 0, :], p_k2[0:64, :], r=["p_k2"], w=[("k2st", s)])
                    sc.op("dve", "tensor_copy", k2st[s][0:64, 1, :], p_k2[64:128, :], r=["p_k2"], w=[("k2st", s)])
                    for t2 in range(2):
                        for kc in range(8):
                            sc.op("pe", "matmul", p_v2[:, t2, :], uTk[s][:, kc, (3 + t2) * 128:(4 + t2) * 128],
                                  Wk[:, kc, 384:512], start=(kc == 0), stop=(kc == 7), r=["Wk", ("uTk", s)], w=["p_v2"])
                    sc.op("dve", "tensor_copy", v2st[s][:, :, :, 0:64], p_v2[:].rearrange("p t (g d) -> p t g d", g=2),
                          r=["p_v2"], w=[("v2st", s)])
                    sc.op("dve", "tensor_copy", v2st[s][:, :, :, 64],
                          hvs[:, j, 3:5].unsqueeze(2).broadcast_to([128, 2, 2]), r=["hvs"], w=[("v2st", s)])
                    sc.dma("pool", kw_d.ap()[:, j], kst[s][:], r=[("kst", s)], w=["kw_d"])
                    sc.dma("pool", vw_d.ap()[:, j], vst[s][:], r=[("vst", s)], w=["vw_d"])
                    sc.dma("pool", k2_d.ap()[:, j], k2st[s][:], r=[("k2st", s)], w=["k2_d"])
                    sc.dma("pool", v2_d.ap()[:, j], v2st[s][:], r=[("v2st", s)], w=["v2_d"])
                    if j + 2 < 16:
                        loadK(j + 2)
                if "C" in dbg:
                    d_kw = dbgout("d_kw", [64, 16, 2, 640], BF16)
                    sc.dma("sp", d_kw.ap(), kw_d.ap(), r=["kw_d"], w=["d_kw"])
                    d_vw = dbgout("d_vw", [128, 16, 5, 2, 65], BF16)
                    sc.dma("sp", d_vw.ap(), vw_d.ap(), r=["vw_d"], w=["d_vw"])
                barrier()

            with contextlib.ExitStack() as esD:
                sbD, psD = mk(esD)
                QA = [[sbD(f"QA{p}{g}", [128, 7, 512], BF16) for g in range(2)] for p in range(2)]
                QS = [sbD(f"QS{p}", [64, 2, 512], BF16) for p in range(2)]
                KWj = [sbD(f"KWj{p}", [64, 2, 640], BF16) for p in range(2)]
                VWj = [sbD(f"VWj{p}", [128, 5 * 130 + 64], BF16) for p in range(2)]
                K2j = [sbD(f"K2j{p}", [64, 2, 256], BF16) for p in range(2)]
                V2j = [sbD(f"V2j{p}", [128, 2 * 130 + 64], BF16) for p in range(2)]
                atj = [sbD(f"atj{p}", [128, 310], BF16) for p in range(2)]
                trj = [sbD(f"trj{p}", [128, 128], F32) for p in range(2)]
                cend = sbD("cend", [128, 8], F32)
                band = sbD("band", [128, 8, 256], BF16)
                ident = sbD("ident", [128, 128], BF16)
                identf = sbD("identf", [128, 128], F32)
                esink = sbD("esink", [128, 8], F32)
                b31s = sbD("b31s", [1, 2, 512], F32)
                b31b = sbD("b31b", [1, 2, 512], BF16)
                cm = [sbD(f"cm{p}", [128, 8, 128], BF16) for p in range(2)]
                PT = [sbD(f"PT{i}", [128, 1024], BF16) for i in range(3)]
                NT = [sbD(f"NT{i}", [128, 512], BF16) for i in range(6)]
                score = sbD("score", [128, 310], F32)
                work = sbD("work", [128, 310], F32)
                work2 = sbD("work2", [128, 310], F32)
                top = sbD("top", [128, 16], F32)
                thr = sbD("thr", [128, 1], F32)
                rs4 = sbD("rs4", [128, 4], F32)
                fac = sbD("fac", [128, 4], F32)
                Mtok = [[sbD(f"Mtok{p}{g}", [128, 5, 128], BF16) for g in range(2)] for p in range(2)]
                rs4s = sbD("rs4s", [128, 4], F32)
                OT = [sbD(f"OT{i}", [128, 512], BF16) for i in range(10)]
                oacc = [[sbD(f"oa{p}", [128, 2, 4, 64], F32), sbD(f"ob{p}", [128, 2, 4, 64], F32)] for p in range(2)]
                oab = [sbD(f"oab{i}", [128, 512], BF16) for i in range(2)]
                oTs = [sbD(f"oTs{i}", [128, 4, 128], BF16) for i in range(2)]
                psS = [psD(f"psS{i}", [128, 2, 512]) for i in range(2)]
                psO = [psD("psO0")]
                psN = psD("psN")
                psM = [psD(f"psM{i}") for i in range(2)]

                sc.dma("sp", cend[:], cend_in.ap(), w=["cend"])
                sc.dma("sp", band[:], band_in.ap(), w=["band"])
                sc.dma("sp", ident[:], ident_in.ap(), w=["ident"])
                sc.dma("sp", identf[:], identf_in.ap(), w=["identf"])
                sc.dma("sp", esink[:], sinks_in.ap(), w=["esink"])
                sc.op("act", "activation", esink[:], esink[:], AF.Exp, r=["esink"], w=["esink"])
                sc.dma("sp", b31s[:], b31_in.ap(), w=["b31s"])
                sc.op("dve", "tensor_copy", b31b[:], b31s[:], r=["b31s"], w=["b31b"])
                sc.op("pool", "memset", score[:], 0.0, w=["score"])
                for p in range(2):
                    for g in range(2):
                        sc.op("pool", "memset", QA[p][g][:], 0.0, w=[("QA", p, g)])
                        sc.op("pool", "memset", Mtok[p][g][:], 0.0, w=[("Mtok", p, g)])
                        for slot in range(5):
                            sc.dma("sp", QA[p][g][126:127, slot, :], b31b[0:1, g, :], r=["b31b"], w=[("QA", p, g)])

                for p in range(2):
                    sc.op("pool", "memset", VWj[p][:], 0.0, w=[("VWj", p)])
                    sc.op("pool", "memset", V2j[p][:], 0.0, w=[("V2j", p)])
                rot = {"S": 0, "O": 0, "PT": 0, "tmp": 0, "NT": 0, "OT": 0, "M": 0}

                def nxt(name, n):
                    v = rot[name]
                    rot[name] = (v + 1) % n
                    return v

                def chunk_info(j):
                    k, typ = j // 2, j % 2
                    Bsel = 16 * k + (8 if typ == 0 else 16)
                    base = 16 * k - 7 if typ == 0 else 16 * k + 1
                    near = {kt: kt - base for kt in range(max(base, 0), min(base + 15, Bsel))}
                    nseg = (2 * Bsel + 61) // 62
                    nsegs = sorted(set((2 * kt) // 62 for kt in near))
                    far_segs = sorted(set((2 * kt) // 62 for kt in range(Bsel) if kt not in near))
                    return k, typ, Bsel, near, nseg, nsegs, far_segs

                def load_misc(j):
                    p = j % 2
                    sc.dma("sp", atj[p][:], atab_in.ap()[:, j, :], w=[("atj", p)])
                    sc.dma("sp", trj[p][:], trow_in.ap()[:, j, :], w=[("trj", p)])
                    sc.dma("sp", KWj[p][:], kw_d.ap()[:, j], r=["kw_d"], w=[("KWj", p)])
                    sc.dma("sp", VWj[p][:, 0:650], vw_d.ap()[:, j].rearrange("p t g e -> p (t g e)"), r=["vw_d"], w=[("VWj", p)])
                    sc.dma("sp", K2j[p][:], k2_d.ap()[:, j], r=["k2_d"], w=[("K2j", p)])
                    sc.dma("sp", V2j[p][:, 0:260], v2_d.ap()[:, j].rearrange("p t g e -> p (t g e)"), r=["v2_d"], w=[("V2j", p)])
                    for g in range(2):
                        sc.dma("sp", QS[p][:, g, :].rearrange("p (r q) -> p r q", r=4), qs_d.ap()[:, j, 4 * g:4 * g + 4, :],
                               r=["qs_d"], w=[("QS", p)])

                def load_QA(j, g):
                    p = j % 2
                    k, typ, Bsel, near, nseg, nsegs, far_segs = chunk_info(j)
                    slots = list(far_segs) + [5 + (sg_ - nsegs[0]) for sg_ in nsegs]
                    for slot in slots:
                        sc.dma("sp", QA[p][g][0:64, slot, :].rearrange("p (r q) -> p r q", r=4),
                               qn_d.ap()[:, j, 4 * g:4 * g + 4, :], r=["qn_d"], w=[("QA", p, g)])

                def load_chunk(j):
                    load_misc(j)
                    load_QA(j, 0)
                    load_QA(j, 1)

                def obank(bO):
                    return (psO[0], ("psO", 0)) if bO == 0 else (psN, "psN")

                def evac(bO):
                    o = nxt("OT", 10)
                    sc.op("act", "activation", OT[o][0:65, :], obank(bO)[0][0:65, :], AF.Copy, r=[obank(bO)[1]], w=[("OT", o)])
                    return o

                def normalize(j, g, o, kind):
                    trv = psN[:].bitcast(BF16)[:, 0:264].rearrange("p (h e) -> p h e", h=4)
                    for h in range(4):
                        sc.op("pe", "transpose", trv[:, h, 0:65], OT[o][0:65, h * 128:(h + 1) * 128], ident[0:65, 0:65],
                              r=[("OT", o), "ident"], w=["psN"])
                    if kind == 3:
                        sc.op("dve", "tensor_tensor", rs4[:], trv[:, :, 64], esink[:, 4 * g:4 * g + 4], ALU.add,
                              r=["psN", "esink"], w=["rs4"])
                    else:
                        sc.op("dve", "tensor_scalar", rs4[:], trv[:, :, 64], 1e-30, None, ALU.max, r=["psN"], w=["rs4"])
                    sc.op("dve", "reciprocal", fac[:], rs4[:], r=["rs4"], w=["fac"])
                    if kind != 3:
                        gv = GS[:, j, :].rearrange("p (g r b) -> p g r b", g=2, r=4)[:, g, :, kind]
                        sc.op("dve", "tensor_tensor", fac[:], fac[:], gv, ALU.mult, r=["fac", "GS"], w=["fac"])
                    ab = 1 if kind == 3 else 0
                    acc = oacc[j % 2][ab]
                    akey = ("oacc", j % 2, ab)
                    for h in range(4):
                        if kind in (0, 3):
                            sc.op("dve", "tensor_scalar", acc[:, g, h, :], trv[:, h, 0:64], fac[:, h:h + 1], None, ALU.mult,
                                  r=["psN", "fac"], w=[akey])
                        else:
                            sc.op("dve", "scalar_tensor_tensor", acc[:, g, h, :], trv[:, h, 0:64], fac[:, h:h + 1],
                                  acc[:, g, h, :], ALU.mult, ALU.add, r=["psN", "fac", akey], w=[akey])

                def attn_tiles(p, g, tiles, qrhs, qkeys, bO=0):
                    n = len(tiles)
                    npair = (n + 1) // 2
                    state = {}
                    ntbuf = {}
                    LOOK = 4

                    def issue_nt(i):
                        t = tiles[i]
                        if t["nt"] is not None:
                            nti = nxt("NT", 6)
                            sc.dma("pool", NT[nti][:], t["nt"], w=[("NT", nti)])
                            ntbuf[i] = nti

                    def issue_S(pi):
                        b = nxt("S", 2)
                        for h2 in range(2):
                            i = 2 * pi + h2
                            if i >= n:
                                break
                            t = tiles[i]
                            nti = ntbuf.get(i)
                            sc.op("pe", "matmul", psS[b][:, h2, :], t["lhsT"], qrhs(t), start=True, stop=(nti is None),
                                  r=t["lkeys"] + qkeys, w=[("psS", b, h2)])
                            if nti is not None:
                                sc.op("pe", "matmul", psS[b][:, h2, :], ident[:], NT[nti][:], start=False, stop=True,
                                      r=["ident", ("NT", nti)], w=[("psS", b, h2)])
                        state[pi] = b

                    def issue_rest(pi):
                        b = state[pi]
                        w2 = min(2, n - 2 * pi)
                        pt = nxt("PT", 3)
                        sc.op("act", "activation", PT[pt][:, 0:w2 * 512], psS[b][:, 0:w2, :].rearrange("p a c -> p (a c)"),
                              AF.Exp, r=[("psS", b, h2) for h2 in range(w2)], w=[("PT", pt)])
                        for h2 in range(w2):
                            t = tiles[2 * pi + h2]
                            if t.get("post") is not None:
                                t["post"](PT[pt][:, h2 * 512:(h2 + 1) * 512], ("PT", pt))
                        for h2 in range(w2):
                            i = 2 * pi + h2
                            t = tiles[i]
                            sc.op("pe", "matmul", obank(bO)[0][:], t["v"], PT[pt][:, h2 * 512:(h2 + 1) * 512],
                                  start=(i == 0), stop=(i == n - 1), r=t["vkeys"] + [("PT", pt)], w=[obank(bO)[1]])
                        for h2 in range(w2):
                            i = 2 * pi + h2
                            t = tiles[i]
                            if t.get("extra") is not None:
                                t["extra"](PT[pt][:, h2 * 512:(h2 + 1) * 512], ("PT", pt), i, n)

                    for i in range(min(LOOK, n)):
                        issue_nt(i)
                    issue_S(0)
                    for pi in range(npair):
                        for i in range(2 * pi + LOOK, min(2 * pi + LOOK + 2, n)):
                            issue_nt(i)
                        if pi + 1 < npair:
                            issue_S(pi + 1)
                        issue_rest(pi)
                        between()
                    return bO

                pend = []
                tick = [0]

                drain_on = [False]

                def between():
                    tick[0] += 1
                    if drain_on[0] and tick[0] % 3 == 0 and pend:
                        normalize(*pend.pop(0))

                def drain():
                    while pend:
                        normalize(*pend.pop(0))

                def cmp_sel(j, g):
                    p = j % 2
                    k, typ, Bsel, near, nseg, nsegs, far_segs = chunk_info(j)
                    NCT = k + 1
                    slotq = far_segs[0] if far_segs else 5
                    tiles = []
                    for ct in range(NCT):
                        def post(pta, ptk, ct=ct):
                            sc.op("dve", "tensor_tensor", pta.rearrange("p (h q) -> p h q", h=4),
                                  pta.rearrange("p (h q) -> p h q", h=4),
                                  cm[p][:, ct, :].unsqueeze(1).broadcast_to([128, 4, 128]), ALU.mult,
                                  r=[ptk, ("cm", p)], w=[ptk])

                        def extra(pta, ptk, i, n, ct=ct):
                            for h in range(4):
                                dst = psM[h // 2][:].rearrange("p (a c) -> p a c", a=2)[:, h % 2, :]
                                sc.op("pe", "matmul", dst, pta[:, h * 128:(h + 1) * 128], band[:, ct, :],
                                      start=(i == 0 and h % 2 == 0), stop=(i == n - 1 and h % 2 == 1), r=[ptk, "band"],
                                      w=[("psM", h // 2)])
                        tiles.append(dict(lhsT=KC[0:64, g, ct * 128:(ct + 1) * 128], lkeys=["KC"], nt=None,
                                          v=VCf[:, (ct * 2 + g) * 65:(ct * 2 + g) * 65 + 128], vkeys=["VC"],
                                          post=post, extra=extra))
                    bO = attn_tiles(p, g, tiles, lambda t: QA[p][g][0:64, slotq, :], [("QA", p, g)], bO=nxt("O", 2))
                    pend.append((j, g, evac(bO), 0))
                    for i2 in range(2):
                        sc.op("dve", "tensor_scalar", rs4s[:, 2 * i2:2 * i2 + 2],
                              psM[i2][:].rearrange("p (a c) -> p a c", a=2)[:, :, 0], 1e-30, None, ALU.max,
                              r=[("psM", i2)], w=["rs4s"])
                    sc.op("dve", "reciprocal", rs4s[:], rs4s[:], r=["rs4s"], w=["rs4s"])
                    for h in range(4):
                        src = psM[h // 2][:, (h % 2) * 256:(h % 2) * 256 + 256]
                        if h == 0:
                            sc.op("dve", "tensor_scalar", score[:, 0:256], src, rs4s[:, 0:1], None, ALU.mult,
                                  r=[("psM", 0), "rs4s"], w=["score"])
                        else:
                            sc.op("dve", "scalar_tensor_tensor", score[:, 0:256], src, rs4s[:, h:h + 1], score[:, 0:256],
                                  ALU.mult, ALU.add, r=[("psM", h // 2), "rs4s", "score"], w=["score"])
                    sc.op("dve", "tensor_tensor", work[:], score[:], atj[p][:], ALU.add, r=["score", ("atj", p)], w=["work"])
                    sc.op("dve", "max", top[:, 0:8], work[:], r=["work"], w=["top"])
                    sc.op("dve", "match_replace", work2[:], top[:, 0:8], work[:], -1e9, r=["top", "work"], w=["work2"])
                    sc.op("dve", "max", top[:, 8:16], work2[:], r=["work2"], w=["top"])
                    sc.op("dve", "tensor_scalar", thr[:], top[:, 15:16], -4096.0, None, ALU.max, r=["top"], w=["thr"])
                    sc.op("dve", "tensor_scalar", Mtok[p][g][:, :, 64:126], work[:].rearrange("p (m c) -> p m c", m=5),
                          thr[:, 0:1], NEG, ALU.is_lt, ALU.mult, r=["work", "thr"], w=[("Mtok", p, g)])

                def make_cm(j):
                    p = j % 2
                    for ct in range(j // 2 + 1):
                        sc.op("dve", "tensor_scalar", cm[p][:, ct, :], trj[p][:], cend[:, ct:ct + 1], None, ALU.is_ge,
                              r=[("trj", p), "cend"], w=[("cm", p)])

                def drain_upto(jmax):
                    keep = []
                    while pend:
                        e = pend.pop(0)
                        if e[0] <= jmax:
                            normalize(*e)
                        else:
                            keep.append(e)
                    pend.extend(keep)

                def trbqa(j):
                    p = j % 2
                    k, typ, Bsel, near, nseg, nsegs, far_segs = chunk_info(j)
                    for g in range(2):
                        trb = psM[g][:].bitcast(BF16).rearrange("p (m c) -> p m c", m=8)
                        for m in range(nseg):
                            sc.op("pe", "transpose", trb[:, m, :], Mtok[p][g][:, m, :], ident[:],
                                  r=[("Mtok", p, g), "ident"], w=[("psM", g)])
                        if far_segs:
                            f0, f1 = far_segs[0], far_segs[-1] + 1
                            sc.op("dve", "tensor_copy",
                                  QA[p][g][64:126, f0:f1, :].rearrange("p s (h q) -> p s h q", h=4),
                                  trb[64:126, f0:f1, :].unsqueeze(2).broadcast_to([62, f1 - f0, 4, 128]),
                                  r=[("psM", g)], w=[("QA", p, g)])
                        if nsegs:
                            n0_, n1_ = nsegs[0], nsegs[-1] + 1
                            sc.op("dve", "tensor_copy",
                                  QA[p][g][64:126, 5:5 + n1_ - n0_, :].rearrange("p s (h q) -> p s h q", h=4),
                                  trb[64:126, n0_:n1_, :].unsqueeze(2).broadcast_to([62, n1_ - n0_, 4, 128]),
                                  r=[("psM", g)], w=[("QA", p, g)])

                def winswa(j, g):
                    p = j % 2
                    k, typ, Bsel, near, nseg, nsegs, far_segs = chunk_info(j)
                    slotq = far_segs[0] if far_segs else 5
                    tiles = []
                    for i5 in range(5):
                        tiles.append(dict(lhsT=KWj[p][0:64, g, i5 * 128:(i5 + 1) * 128], lkeys=[("KWj", p)],
                                          nt=nt_win.ap()[i5, g], v=VWj[p][:, (i5 * 2 + g) * 65:(i5 * 2 + g) * 65 + 128],
                                          vkeys=[("VWj", p)]))
                    bO = attn_tiles(p, g, tiles, lambda t: QA[p][g][0:64, slotq, :], [("QA", p, g)], bO=nxt("O", 2))
                    pend.append((j, g, evac(bO), 2))
                    tiles = []
                    for i2 in range(2):
                        tiles.append(dict(lhsT=K2j[p][0:64, g, i2 * 128:(i2 + 1) * 128], lkeys=[("K2j", p)],
                                          nt=nt_swa.ap()[i2, g], v=V2j[p][:, (i2 * 2 + g) * 65:(i2 * 2 + g) * 65 + 128],
                                          vkeys=[("V2j", p)]))
                    bO = attn_tiles(p, g, tiles, lambda t: QS[p][0:64, g, :], [("QS", p)], bO=nxt("O", 2))
                    pend.append((j, g, evac(bO), 3))

                def selloop(j, g):
                    p = j % 2
                    k, typ, Bsel, near, nseg, nsegs, far_segs = chunk_info(j)
                    tiles = []
                    for kt in range(Bsel):
                        sg_ = (2 * kt) // 62
                        if kt in near:
                            slot = 5 + (sg_ - nsegs[0])
                            nt = nt_sel.ap()[typ, near[kt], g]
                        else:
                            slot = sg_
                            nt = None
                        tiles.append(dict(lhsT=KT[0:127, g, kt * 128:(kt + 1) * 128], lkeys=[("KT", g), ("KTc", g)],
                                          nt=nt, v=VAf[:, (kt * 2 + g) * 65:(kt * 2 + g) * 65 + 128], vkeys=["VA"], slot=slot))
                    bO = attn_tiles(p, g, tiles, lambda t: QA[p][g][0:127, t["slot"], :], [("QA", p, g)])
                    pend.append((j, g, evac(bO), 1))

                def final_copy(j):
                    for ab in range(2):
                        sc.op("dve", "tensor_copy", oab[ab][:], oacc[j % 2][ab][:].rearrange("p g h d -> p (g h d)"),
                              r=[("oacc", j % 2, ab)], w=[("oab", ab)])

                def final_T(j):
                    for ab in range(2):
                        trb = psN[:].bitcast(BF16).rearrange("p (m c) -> p m c", m=8)
                        for c4 in range(4):
                            sc.op("pe", "transpose", trb[:, c4, :], oab[ab][:, c4 * 128:(c4 + 1) * 128], ident[:],
                                  r=[("oab", ab), "ident"], w=["psN"])
                        sc.op("dve", "tensor_copy", oTs[ab][:], trb[:, 0:4, :], r=["psN"], w=[("oTs", ab)])
                        dst = (oat_d if ab == 0 else obt_d).ap()[:, :, j * 128:(j + 1) * 128]
                        sc.dma("sp", dst, oTs[ab][:], r=[("oTs", ab)], w=[("o_d", ab)])

                jlist = list(range(16)) if "Dshort" not in dbg else [0, 1]
                NJ = len(jlist)
                load_chunk(0)
                make_cm(0)
                cmp_sel(0, 0)
                cmp_sel(0, 1)
                trbqa(0)
                if NJ > 1:
                    load_chunk(1)
                    make_cm(1)
                for j in jlist:
                    if j + 1 < NJ:
                        cmp_sel(j + 1, 0)
                    winswa(j, 0)
                    if j + 1 < NJ:
                        cmp_sel(j + 1, 1)
                    winswa(j, 1)
                    if j + 2 < NJ:
                        load_misc(j + 2)
                    drain_on[0] = True
                    selloop(j, 0)
                    if j + 2 < NJ:
                        load_QA(j + 2, 0)
                        make_cm(j + 2)
                    selloop(j, 1)
                    drain_on[0] = False
                    if j + 2 < NJ:
                        load_QA(j + 2, 1)
                    drain()
                    final_copy(j)
                    final_T(j)
                    if j + 1 < NJ:
                        trbqa(j + 1)
                if "D" in dbg:
                    d_oa = dbgout("d_oa", [128, 4, 2048], BF16)
                    sc.dma("sp", d_oa.ap(), oat_d.ap(), r=[("o_d", 0)], w=["d_oa"])
                    d_ob = dbgout("d_ob", [128, 4, 2048], BF16)
                    sc.dma("sp", d_ob.ap(), obt_d.ap(), r=[("o_d", 1)], w=["d_ob"])
                barrier()

        with contextlib.ExitStack() as esE:
            sbE, psE = mk(esE)
            W1m = sbE("W1m", [128, 8, 4096], BF16)
            with contextlib.ExitStack() as esE1:
                sbE1, psE1 = mk(esE1)
                WuA = sbE1("WuA", [128, 4, 1024], BF16)
                WuB = sbE1("WuB", [128, 4, 1024], BF16)
                Wo = sbE1("Wo", [128, 8, 1024], BF16)
                oaT = [sbE1(f"oaT{i}", [128, 4, EB], BF16) for i in range(2)]
                obT = [sbE1(f"obT{i}", [128, 4, EB], BF16) for i in range(2)]
                GA = [sbE1(f"GA{i}", [128, 8, EB], BF16) for i in range(2)]
                GB = [sbE1(f"GB{i}", [128, 8, EB], BF16) for i in range(2)]
                xo = [sbE1(f"xoE{i}", [128, 8, EB], F32) for i in range(3)]
                t1 = [sbE1(f"t1E{i}", [128, EB], F32) for i in range(2)]
                t2 = [sbE1(f"t2E{i}", [128, EB], F32) for i in range(2)]
                t3 = [sbE1(f"t3E{i}", [128, EB], F32) for i in range(2)]
                yTs = [sbE1(f"yT{i}", [128, 8, EB], BF16) for i in range(2)]
                hst = sbE1("hst", [128, 8, EB], F32)
                zTs = [sbE1(f"zT{i}", [128, 8, EB], F32) for i in range(2)]
                zsq = sbE1("zsq", [128, 8, EB], BF16)
                rsE = sbE1("rsE", [128, EB], F32)
                pA = [psE1(f"pA{i}") for i in range(2)]
                pB = [psE1(f"pB{i}") for i in range(2)]
                pZ = [psE1(f"pZ{i}") for i in range(2)]
                pSt = psE1("pSt")
                sc.dma("pool", WuA[:], w_upA.ap().rearrange("(c p) n -> p c n", p=128), w=["WuA"])
                sc.dma("pool", WuB[:], w_upB.ap().rearrange("(c p) n -> p c n", p=128), w=["WuB"])
                sc.dma("pool", Wo[:], w_out.ap().rearrange("(c p) n -> p c n", p=128), w=["Wo"])
                for q4 in range(4):
                    sc.dma("pool", W1m[:, :, q4 * 1024:(q4 + 1) * 1024],
                           w_m1.ap()[:, q4 * 1024:(q4 + 1) * 1024].rearrange("(c p) n -> p c n", p=128), w=["W1m"])
                xosrc = xT_own.ap().rearrange("(kc p) t -> p kc t", p=128)
                def loadE(eb):
                    s = eb % 2
                    cs = slice(eb * EB, (eb + 1) * EB)
                    sc.dma("sp", oaT[s][:], oat_d.ap()[:, :, cs], r=[("o_d", 0)], w=[("oaT", s)])
                    sc.dma("sp", obT[s][:], obt_d.ap()[:, :, cs], r=[("o_d", 1)], w=[("obT", s)])
                    sc.dma("sp", GA[s][:], gate_d[0].ap()[:, :, cs], r=[("gate_d", 0)], w=[("GA", s)])
                    sc.dma("sp", GB[s][:], gate_d[1].ap()[:, :, cs], r=[("gate_d", 1)], w=[("GB", s)])
                    sc.dma("sp", xo[eb % 3][:], xosrc[:, :, cs], w=[("xoE", eb % 3)])

                def upE(eb):
                    s = eb % 2
                    yT = yTs[s]
                    for fc in range(8):
                        b = fc % 2
                        for c4 in range(4):
                            sc.op("pe", "matmul", pA[b][:, 0:EB], WuA[:, c4, fc * 128:(fc + 1) * 128], oaT[s][:, c4, :],
                                  start=(c4 == 0), stop=(c4 == 3), r=["WuA", ("oaT", s)], w=[("pA", b)])
                        for c4 in range(4):
                            sc.op("pe", "matmul", pB[b][:, 0:EB], WuB[:, c4, fc * 128:(fc + 1) * 128], obT[s][:, c4, :],
                                  start=(c4 == 0), stop=(c4 == 3), r=["WuB", ("obT", s)], w=[("pB", b)])
                        sc.op("dve", "tensor_tensor", t1[b][:], pA[b][:, 0:EB], GA[s][:, fc, :], ALU.mult,
                              r=[("pA", b), ("GA", s)], w=[("t1E", b)])
                        sc.op("dve", "tensor_tensor", t2[b][:], pB[b][:, 0:EB], GB[s][:, fc, :], ALU.mult,
                              r=[("pB", b), ("GB", s)], w=[("t2E", b)])
                        sc.op("dve", "tensor_tensor", yT[:, fc, :], t1[b][:], t2[b][:], ALU.add,
                              r=[("t1E", b), ("t2E", b)], w=[("yT", s)])

                def outmmE(eb):
                    s = eb % 2
                    yT = yTs[s]
                    zT = zTs[s]
                    for fo in range(8):
                        b = fo % 2
                        for fc in range(8):
                            sc.op("pe", "matmul", pZ[b][:, 0:EB], Wo[:, fc, fo * 128:(fo + 1) * 128], yT[:, fc, :],
                                  start=(fc == 0), stop=(fc == 7), r=["Wo", ("yT", s)], w=[("pZ", b)])
                        sc.op("act", "activation", zT[:, fo, :], pZ[b][:, 0:EB], AF.Copy, r=[("pZ", b)], w=[("zT", s)])

                def finE(eb):
                    s = eb % 2
                    zT = zTs[s]
                    cs = slice(eb * EB, (eb + 1) * EB)
                    norm_cols(zT, ("zT", s), zsq, "zsq", pSt, "pSt", rsE, "rsE", None, None, 1, EB)
                    for fo in range(8):
                        b = fo % 2
                        sc.op("dve", "scalar_tensor_tensor", t3[b][:], zT[:, fo, :], gn[:, 1, fo:fo + 1], rsE[:],
                              ALU.mult, ALU.mult, r=[("zT", s), "rsE", "gn"], w=[("t3E", b)])
                        sc.op("pool", "tensor_tensor", hst[:, fo, :], t3[b][:], xo[eb % 3][:, fo, :], ALU.add,
                              r=[("t3E", b), ("xoE", eb % 3)], w=["hst"])
                    sc.dma("act", ht_d.ap()[:, :, cs], hst[:], r=["hst"], w=["ht_d"])

                NEB = 2048 // EB
                loadE(0)
                loadE(1)
                upE(0)
                for eb in range(NEB):
                    if eb + 1 < NEB:
                        upE(eb + 1)
                    outmmE(eb)
                    if eb >= 1:
                        finE(eb - 1)
                    if eb + 2 < NEB:
                        loadE(eb + 2)
                finE(NEB - 1)
                fold_gain(W1m, "W1m", 2)
                if "E" in dbg:
                    d_h = dbgout("d_h", [128, 8, 2048])
                    sc.dma("sp", d_h.ap(), ht_d.ap(), r=["ht_d"], w=["d_h"])
                barrier()

            with contextlib.ExitStack() as esF:
                sbF, psF = mk(esF)
                W2m = sbF("W2m", [128, 32, 1024], BF16)
                hT = [sbF(f"hT{i}", [128, 8, EB], F32) for i in range(3)]
                hsq = sbF("hsq", [128, 8, EB], BF16)
                rsF = sbF("rsF", [128, EB], F32)
                rsO = sbF("rsO", [128, EB], F32)
                u2 = [sbF(f"u2{i}", [128, 8, EB], BF16) for i in range(2)]
                rl = [sbF(f"rl{i}", [128, EB], BF16) for i in range(2)]
                hid = sbF("hid", [128, 32, EB], BF16)
                fT = sbF("fT", [128, 8, EB], F32)
                fsq = sbF("fsq", [128, 8, EB], BF16)
                t1 = [sbF(f"t1F{i}", [128, EB], F32) for i in range(2)]
                pH = [psF(f"pH{i}") for i in range(3)]
                pF = [psF(f"pF{i}") for i in range(2)]
                pSt = psF("pStF")
                pSo = psF("pStO")
                for q4 in range(4):
                    sc.dma("pool", W2m[:, q4 * 8:(q4 + 1) * 8, :],
                           w_m2.ap()[q4 * 1024:(q4 + 1) * 1024, :].rearrange("(c p) n -> p c n", p=128), w=["W2m"])
                outv = out.ap().rearrange("(c p) t -> p c t", p=128)
                NFB = 2048 // EB

                def loadF(fb):
                    sc.dma("sp", hT[fb % 3][:], ht_d.ap()[:, :, fb * EB:(fb + 1) * EB], r=["ht_d"], w=[("hT", fb % 3)])

                def normF(fb):
                    s = fb % 2
                    norm_cols(hT[fb % 3], ("hT", fb % 3), hsq, "hsq", pSt, "pStF", rsF, "rsF", u2[s], ("u2", s), 2, EB,
                              split=(fb > 0))

                def hiddenF(fb):
                    s = fb % 2
                    for hc in range(32):
                        b = hc % 3
                        for kc in range(8):
                            sc.op("pe", "matmul", pH[b][:, 0:EB], W1m[:, kc, hc * 128:(hc + 1) * 128], u2[s][:, kc, :],
                                  start=(kc == 0), stop=(kc == 7), r=["W1m", ("u2", s)], w=[("pH", b)])
                        sc.op("act", "activation", rl[hc % 2][:], pH[b][:, 0:EB], AF.Relu, r=[("pH", b)], w=[("rl", hc % 2)])
                        sc.op("pool" if (fb > 0 and hc % 2 == 1) else "dve", "tensor_tensor", hid[:, hc, :], rl[hc % 2][:],
                              rl[hc % 2][:], ALU.mult, r=[("rl", hc % 2)], w=["hid"])

                def secondF(fb):
                    for fo in range(8):
                        b = fo % 2
                        for hc in range(32):
                            sc.op("pe", "matmul", pF[b][:, 0:EB], W2m[:, hc, fo * 128:(fo + 1) * 128], hid[:, hc, :],
                                  start=(hc == 0), stop=(hc == 31), r=["W2m", "hid"], w=[("pF", b)])
                        sc.op("act", "activation", fT[:, fo, :], pF[b][:, 0:EB], AF.Copy, r=[("pF", b)], w=["fT"])

                def outF(fb):
                    s = fb % 2
                    cs = slice(fb * EB, (fb + 1) * EB)
                    norm_cols(fT, "fT", fsq, "fsq", pSo, "pStO", rsO, "rsO", None, None, 3, EB)
                    for fo in range(8):
                        b = fo % 2
                        sc.op("dve", "scalar_tensor_tensor", t1[b][:], fT[:, fo, :], gn[:, 3, fo:fo + 1], rsO[:],
                              ALU.mult, ALU.mult, r=["fT", "rsO", "gn"], w=[("t1F", b)])
                        sc.op("dve" if fo % 2 == 0 else "pool", "tensor_tensor", hT[fb % 3][:, fo, :], t1[b][:],
                              hT[fb % 3][:, fo, :], ALU.add, r=[("t1F", b), ("hT", fb % 3)], w=[("hT", fb % 3)])
                    sc.dma("act", outv[:, :, cs], hT[fb % 3][:], r=[("hT", fb % 3)], w=["out"])

                loadF(0)
                loadF(1)
                loadF(2)
                normF(0)
                hiddenF(0)
                for fb in range(NFB):
                    if fb + 1 < NFB:
                        normF(fb + 1)
                    secondF(fb)
                    if fb + 1 < NFB:
                        hiddenF(fb + 1)
                    outF(fb)
                    if fb + 3 < NFB:
                        loadF(fb + 3)

        sc.wait_all("sp", ["out"] + list(dbo.keys()))
    print("instructions:", sc.ninst, "sems:", sc.nsem)
    return nc


IN_SIZES = (512,) + (128,) * 6 + (24, 512, 128, 128, 1024, 1024)
OFF = np.concatenate([[0], np.cumsum(IN_SIZES)]).astype(int)


def _bias_table(relx, delta, heads, lo, hi):
    p = np.arange(128)[:, None]
    q = np.arange(128)[None, :]
    dist = 128 * delta + q - p
    valid = (dist >= lo) & (dist < hi)
    b = np.where(valid, t5_bucket_np(dist), 32)
    out = np.stack([relx[b, h] for h in heads], axis=1)
    return np.ascontiguousarray(out.reshape(128, 512))


def prep_inputs(inputs):
    f = lambda k: np.asarray(inputs[k], np.float32)
    x = f("x")[0]
    w_in = f("w_in")[0]
    xT = np.ascontiguousarray(x.T)
    seg = lambda i: w_in[:, OFF[i]:OFF[i + 1]]
    cat = lambda l: np.ascontiguousarray(np.concatenate(l, axis=1))
    gains = np.zeros((128, 4, 8), np.float32)
    for gi, nm in enumerate(["norm_mix_pre", "norm_mix_post", "norm_mlp_pre", "norm_mlp_post"]):
        gains[:, gi, :] = f(nm)[0].reshape(8, 128).T
    indic = np.zeros((63, S), np.float32)
    blk = np.arange(S) // 64
    indic[blk % 62, np.arange(S)] = 1.0
    indic[62, :] = 1.0
    rel = f("rel_bias")
    relx = np.concatenate([rel, np.full((1, 16), NEG, np.float32)], axis=0)
    b31row = np.zeros((1, 2, 512), np.float32)
    for g in range(2):
        for h in range(4):
            b31row[0, g, h * 128:(h + 1) * 128] = rel[31, 4 * g + h]
    pcol = np.arange(128)[:, None]
    cend = (16 * (np.arange(8)[None, :] * 128 + pcol) + 31).astype(np.float32)
    n = (np.arange(8)[None, :, None] * 128 + pcol[:, :, None])
    jb = np.arange(256)[None, None, :]
    band = ((n >= 4 * jb - 1) & (n <= 4 * jb + 3) & (n <= 1022)).astype(np.float32)
    band[:, :, 0] = 1.0
    nt_win = np.stack([np.stack([_bias_table(relx, 4 - i, [4 * g + h for h in range(4)], 0, 512) for g in range(2)])
                       for i in range(5)])
    nt_swa = np.stack([np.stack([_bias_table(relx, 1 - i, [8 + 4 * g + h for h in range(4)], 0, 128) for g in range(2)])
                       for i in range(2)])
    common = dict(
        xT_all=xT,
        w_A=cat([seg(1), seg(2), seg(3), seg(4)]), w_Q=cat([seg(0), seg(8)]),
        w_ga=np.ascontiguousarray(seg(11)), w_gb=np.ascontiguousarray(seg(12)),
        w_kv2=cat([seg(5), seg(6), seg(9), seg(10)]), w_gn=np.ascontiguousarray(seg(7)),
        gains=gains, indic=indic.astype(ml_dtypes.bfloat16),
        w1k=f("cmp_w1_k")[0], w1v=f("cmp_w1_v")[0], w2k=f("cmp_w2_k")[0], w2v=f("cmp_w2_v")[0],
        posk=np.ascontiguousarray(f("cmp_pos_k")[0].reshape(16, 128).T),
        posv=np.ascontiguousarray(f("cmp_pos_v")[0].reshape(16, 128).T),
        nt_win=nt_win, nt_swa=nt_swa, b31row=b31row,
        sinks=np.ascontiguousarray(np.tile(f("attn_sinks")[0][None, :], (128, 1))),
        cend=cend, band=band.astype(ml_dtypes.bfloat16),
        ident=np.eye(128, dtype=np.float32).astype(ml_dtypes.bfloat16), identf=np.eye(128, dtype=np.float32),
        w_upA=f("w_up_nsa")[0], w_upB=f("w_up_swa")[0], w_out=f("w_out")[0],
        w_m1=f("w_mlp_in")[0], w_m2=f("w_mlp_out")[0],
    )
    in_maps = []
    for c in range(NCORES):
        own = own_chunks(c)
        m = dict(common)
        m["xT_own"] = np.ascontiguousarray(np.concatenate([xT[:, ci * 128:(ci + 1) * 128] for ci in own], axis=1))
        kv2 = np.zeros((D, 16, 640), np.float32)
        hv = np.zeros((128, 16, 5), np.float32)
        atab = np.zeros((128, 16, 310), np.float32)
        trow = np.zeros((128, 16, 128), np.float32)
        bl = np.arange(310)[None, :]
        for j, ci in enumerate(own):
            for i in range(5):
                cc = ci - 4 + i
                if cc >= 0:
                    kv2[:, j, i * 128:(i + 1) * 128] = xT[:, cc * 128:(cc + 1) * 128]
                    hv[:, j, i] = 1.0
            t = 128 * ci + np.arange(128)[:, None]
            cur = t // 64
            valid = (bl <= cur) & (bl < 256)
            forced = (bl == 0) | (bl == cur) | (bl == cur - 1)
            atab[:, j, :] = np.where(valid, np.where(forced, 8192.0, 0.0), -8192.0)
            trow[:, j, :] = 128 * ci + np.arange(128)[None, :]
        m["xT_kv2"] = kv2
        m["hv"] = hv
        m["atab"] = atab.astype(ml_dtypes.bfloat16)
        m["trow"] = trow
        nts = np.zeros((2, 15, 2, 128, 512), np.float32)
        for s in range(15):
            for g in range(2):
                heads = [4 * g + h for h in range(4)]
                nts[0, s, g] = _bias_table(relx, c + 7 - s, heads, 0, 1 << 30)
                nts[1, s, g] = _bias_table(relx, 14 - c - s, heads, 0, 1 << 30)
        m["nt_sel"] = nts
        in_maps.append(m)
    return in_maps


def kernel(**inputs):
    dbg = set(os.environ.get("KDBG", "").split(",")) - {""}
    in_maps = prep_inputs(inputs)
    nc = build(dbg)
    res = run_bass_kernel_spmd(nc, in_maps, core_ids=list(range(NCORES)))
    if dbg:
        kernel.last = res
        kernel.in_maps = in_maps
    outT = np.zeros((D, S), np.float32)
    for c in range(NCORES):
        o = np.asarray(res.results[c]["out"])
        for j, ci in enumerate(own_chunks(c)):
            outT[:, ci * 128:(ci + 1) * 128] = o[:, j * 128:(j + 1) * 128]
    return np.ascontiguousarray(outT.T)[None].astype(np.float32)
```
